# Optimizing a Trainium2 kernel written in Bass

```python
import jax, jax.numpy as jnp
from jax import lax
import numpy as np

D_MODEL = 1024
BATCH = 8
SEQ = 4096
DEPTH = 2

CTX_LEN = 256
GRID_W = 64
N_MIXERS = 2
N_POOL_LAYERS = (DEPTH + 1) // 2
N_LRU_LAYERS = DEPTH // 2
POOL_WINDOWS = (2, 4, 8, 16)
N_POOL_GROUPS = 4
POOL_GROUP = D_MODEL // N_POOL_GROUPS
D_RNN = D_MODEL
N_LRU_BLOCKS = 4
LRU_BLOCK = D_RNN // N_LRU_BLOCKS
CONV_WIDTH = 4
CONV_LEFT = 2
LRU_C = 8.0
N_EXPERTS = 32
TOP_K = 4
D_FF = D_MODEL
SWIGLU_LIMIT = 7.0
SWIGLU_ALPHA = 1.702
EXPERT_BLOCK = 128
NORM_EPS = 1e-6

kernel_name = 'hybrid_pool_rglru_moe_prefix_dit'


def rms_norm(x, g):
    xf = x.astype(jnp.float32)
    y = xf * lax.rsqrt(jnp.mean(xf * xf, axis=-1, keepdims=True) + NORM_EPS)
    return (y * g.astype(jnp.float32)).astype(x.dtype)


def modulate(x, shift, scale):
    return x * (1.0 + scale) + shift


def window_bounds(n, w):
    idx = jnp.arange(n)
    return jnp.clip(idx - w // 2, 0, n), jnp.clip(idx + w // 2, 0, n)


def pool_grid(h, w):
    B, T, C = h.shape
    rows = T // GRID_W
    g = h.astype(jnp.float32).reshape(B, rows, GRID_W, C)
    sat = jnp.pad(jnp.cumsum(jnp.cumsum(g, axis=1), axis=2), ((0, 0), (1, 0), (1, 0), (0, 0)))
    r0, r1 = window_bounds(rows, w)
    c0, c1 = window_bounds(GRID_W, w)

    def corner(ri, ci):
        return jnp.take(jnp.take(sat, ri, axis=1), ci, axis=2)

    total = corner(r1, c1) - corner(r0, c1) - corner(r1, c0) + corner(r0, c0)
    count = ((r1 - r0)[:, None] * (c1 - c0)[None, :]).astype(jnp.float32)
    return (total / count[None, :, :, None]).reshape(B, T, C).astype(h.dtype)


def pool_seq(h, w):
    T = h.shape[1]
    cs = jnp.pad(jnp.cumsum(h.astype(jnp.float32), axis=1), ((0, 0), (1, 0), (0, 0)))
    lo, hi = window_bounds(T, w)
    total = jnp.take(cs, hi, axis=1) - jnp.take(cs, lo, axis=1)
    count = (hi - lo).astype(jnp.float32)
    return (total / count[None, :, None]).astype(h.dtype)


def pool_mixer(h, w_grp, scale, pool_fn):
    B, T, D = h.shape
    diffs = []
    for gi, w in enumerate(POOL_WINDOWS):
        hg = h[..., gi * POOL_GROUP:(gi + 1) * POOL_GROUP]
        diffs.append(pool_fn(hg, w) - hg)
    d = jnp.stack(diffs, axis=2)
    y = jnp.einsum('btgc,gcd->btgd', d, w_grp).reshape(B, T, D)
    return y * scale


def depthwise_conv(x, w, b):
    T = x.shape[1]
    xp = jnp.pad(x, ((0, 0), (CONV_LEFT, CONV_WIDTH - 1 - CONV_LEFT), (0, 0)))
    y = xp[:, 0:T] * w[0]
    for k in range(1, CONV_WIDTH):
        y = y + xp[:, k:k + T] * w[k]
    return y + b


def rglru_coeffs(u, w_r, b_r, w_i, b_i, lam):
    B, T, _ = u.shape
    ub = u.reshape(B, T, N_LRU_BLOCKS, LRU_BLOCK)
    r = jax.nn.sigmoid(jnp.einsum('btnd,nde->btne', ub, w_r).reshape(B, T, D_RNN) + b_r)
    i = jax.nn.sigmoid(jnp.einsum('btnd,nde->btne', ub, w_i).reshape(B, T, D_RNN) + b_i)
    log_a = -LRU_C * r.astype(jnp.float32) * jax.nn.softplus(-lam.astype(jnp.float32))
    a = jnp.exp(log_a)
    mult = jnp.sqrt(-jnp.expm1(2.0 * log_a))
    return a, mult * (i * u).astype(jnp.float32)


def linear_scan(a, b, h0, reverse):
    if h0 is not None:
        edge = -1 if reverse else 0
        b = b.at[:, edge].add(a[:, edge] * h0)

    def combine(p, q):
        a1, b1 = p
        a2, b2 = q
        return a1 * a2, a2 * b1 + b2

    _, h = lax.associative_scan(combine, (a, b), axis=1, reverse=reverse)
    return h


def lru_mixer(hl, hc, w_in, conv_w, conv_b, w_r, b_r, w_i, b_i, lam, w_out, need_ctx_out):
    gate_l, u_l = jnp.split(hl @ w_in, 2, axis=-1)
    u_l = depthwise_conv(u_l, conv_w, conv_b)
    u_c = depthwise_conv(hc @ w_in[:, D_RNN:], conv_w, conv_b)
    outs_l, outs_c = [], []
    for d, rev in enumerate((False, True)):
        a_c, b_c = rglru_coeffs(u_c, w_r[d], b_r[d], w_i[d], b_i[d], lam[d])
        h_c = linear_scan(a_c, b_c, None, rev)
        h0 = h_c[:, 0] if rev else h_c[:, -1]
        a_l, b_l = rglru_coeffs(u_l, w_r[d], b_r[d], w_i[d], b_i[d], lam[d])
        outs_l.append(linear_scan(a_l, b_l, h0, rev))
        outs_c.append(h_c)
    rec_l = (outs_l[0] + outs_l[1]).astype(hl.dtype)
    y_l = (jax.nn.gelu(gate_l) * rec_l) @ w_out
    if not need_ctx_out:
        return y_l, None
    rec_c = (outs_c[0] + outs_c[1]).astype(hc.dtype)
    y_c = (jax.nn.gelu(hc @ w_in[:, :D_RNN]) * rec_c) @ w_out
    return y_l, y_c


def moe(h, w_router, b_router, w_gu, b_gu, w_down, b_down):
    N, D = h.shape
    logits = (h @ w_router + b_router).astype(jnp.float32)
    top_val, top_idx = lax.top_k(logits, TOP_K)
    gate = jax.nn.softmax(top_val, axis=-1).astype(h.dtype)
    NK = N * TOP_K
    flat_e = top_idx.reshape(NK)
    flat_tok = jnp.arange(NK, dtype=jnp.int32) // TOP_K
    flat_w = gate.reshape(NK)
    order = jnp.argsort(flat_e)
    e_sorted, tok_sorted, w_sorted = flat_e[order], flat_tok[order], flat_w[order]
    counts = jnp.bincount(flat_e, length=N_EXPERTS)
    padded = (counts + EXPERT_BLOCK - 1) // EXPERT_BLOCK * EXPERT_BLOCK
    pad_end = jnp.cumsum(padded)
    pad_start = pad_end - padded
    start = jnp.cumsum(counts) - counts
    dest = pad_start[e_sorted] + jnp.arange(NK, dtype=jnp.int32) - start[e_sorted]
    n_blocks = (NK + N_EXPERTS * (EXPERT_BLOCK - 1) + EXPERT_BLOCK - 1) // EXPERT_BLOCK
    n_rows = n_blocks * EXPERT_BLOCK
    row_tok = jnp.full((n_rows,), N, dtype=jnp.int32).at[dest].set(tok_sorted)
    h_pad = jnp.concatenate([h, jnp.zeros((1, D), h.dtype)], axis=0)
    xb = h_pad[row_tok].reshape(n_blocks, EXPERT_BLOCK, D)
    block_start = jnp.arange(n_blocks, dtype=jnp.int32) * EXPERT_BLOCK
    block_e = jnp.minimum(jnp.searchsorted(pad_end, block_start, side='right'), N_EXPERTS - 1)

    def expert_block(args):
        xblk, e = args
        gu = xblk @ w_gu[e] + b_gu[e]
        g, u = gu[:, :D_FF], gu[:, D_FF:]
        g = jnp.minimum(g, SWIGLU_LIMIT)
        u = jnp.clip(u, -SWIGLU_LIMIT, SWIGLU_LIMIT)
        act = (u + 1.0) * g * jax.nn.sigmoid(SWIGLU_ALPHA * g)
        return act @ w_down[e] + b_down[e]

    y = lax.map(expert_block, (xb, block_e)).reshape(n_rows, D)
    return jnp.zeros((N, D), h.dtype).at[tok_sorted].add(y[dest] * w_sorted[:, None])


def setup_inputs(seed: int = 0) -> dict:
    key = jax.random.key(seed)
    ks = iter(jax.random.split(key, 32))
    D, E, F = D_MODEL, N_EXPERTS, D_FF

    def nrm(shape, scale):
        return scale * jax.random.normal(next(ks), shape, jnp.float32)

    def gain(shape):
        return 1.0 + nrm(shape, 0.05)

    a0 = jax.random.uniform(next(ks), (N_LRU_LAYERS, 2, D_RNN), jnp.float32, 0.9, 0.999)
    return {
        'x': nrm((BATCH, SEQ, D), 1.0),
        'c': nrm((BATCH, D), 1.0),
        'ctx': nrm((BATCH, CTX_LEN, D), 1.0),
        'c_ctx': nrm((D,), 1.0),
        'ada_w': nrm((DEPTH, D, 6 * D), 0.5 * D ** -0.5),
        'ada_b': nrm((DEPTH, 6 * D), 0.01),
        'norm_mix': gain((DEPTH, D)),
        'norm_ffn': gain((DEPTH, D)),
        'pool_w': nrm((N_POOL_LAYERS, N_POOL_GROUPS, POOL_GROUP, POOL_GROUP), POOL_GROUP ** -0.5),
        'pool_scale': gain((N_POOL_LAYERS, D)),
        'lru_w_in': nrm((N_LRU_LAYERS, D, 2 * D_RNN), D ** -0.5),
        'lru_conv_w': nrm((N_LRU_LAYERS, CONV_WIDTH, D_RNN), CONV_WIDTH ** -0.5),
        'lru_conv_b': nrm((N_LRU_LAYERS, D_RNN), 0.01),
        'lru_w_r': nrm((N_LRU_LAYERS, 2, N_LRU_BLOCKS, LRU_BLOCK, LRU_BLOCK), LRU_BLOCK ** -0.5),
        'lru_b_r': nrm((N_LRU_LAYERS, 2, D_RNN), 0.01),
        'lru_w_i': nrm((N_LRU_LAYERS, 2, N_LRU_BLOCKS, LRU_BLOCK, LRU_BLOCK), LRU_BLOCK ** -0.5),
        'lru_b_i': nrm((N_LRU_LAYERS, 2, D_RNN), 0.01),
        'lru_lam': jnp.log(a0) - jnp.log1p(-a0),
        'lru_w_out': nrm((N_LRU_LAYERS, D_RNN, D), D_RNN ** -0.5),
        'router_w': nrm((DEPTH, D, E), D ** -0.5),
        'router_b': nrm((DEPTH, E), 0.01),
        'exp_w_gu': nrm((DEPTH, E, D, 2 * F), D ** -0.5),
        'exp_b_gu': nrm((DEPTH, E, 2 * F), 0.01),
        'exp_w_down': nrm((DEPTH, E, F, D), F ** -0.5),
        'exp_b_down': nrm((DEPTH, E, D), 0.01),
        'final_norm': gain((D,)),
    }


def reference(x, c, ctx, c_ctx, ada_w, ada_b, norm_mix, norm_ffn, pool_w, pool_scale,
              lru_w_in, lru_conv_w, lru_conv_b, lru_w_r, lru_b_r, lru_w_i, lru_b_i, lru_lam, lru_w_out,
              router_w, router_b, exp_w_gu, exp_b_gu, exp_w_down, exp_b_down, final_norm):
    B, T, D = x.shape
    lat, cx = x, ctx
    silu_c = jax.nn.silu(c)
    silu_cc = jax.nn.silu(c_ctx)
    for i in range(DEPTH):
        last = i == DEPTH - 1
        is_pool = i % N_MIXERS == 0
        j = i // N_MIXERS
        need_ctx_out = not last
        need_ctx_in = need_ctx_out or not is_pool
        sh1, sc1, g1, sh2, sc2, g2 = jnp.split((silu_c @ ada_w[i] + ada_b[i])[:, None, :], 6, axis=-1)
        csh1, csc1, cg1, csh2, csc2, cg2 = jnp.split(silu_cc @ ada_w[i] + ada_b[i], 6)

        hl = modulate(rms_norm(lat, norm_mix[i]), sh1, sc1)
        hc = modulate(rms_norm(cx, norm_mix[i]), csh1, csc1) if need_ctx_in else None
        if is_pool:
            yl = pool_mixer(hl, pool_w[j], pool_scale[j], pool_grid)
            yc = pool_mixer(hc, pool_w[j], pool_scale[j], pool_seq) if need_ctx_out else None
        else:
            yl, yc = lru_mixer(hl, hc, lru_w_in[j], lru_conv_w[j], lru_conv_b[j], lru_w_r[j], lru_b_r[j],
                               lru_w_i[j], lru_b_i[j], lru_lam[j], lru_w_out[j], need_ctx_out)
        lat = lat + g1 * yl

        hl = modulate(rms_norm(lat, norm_ffn[i]), sh2, sc2)
        if need_ctx_out:
            cx = cx + cg1 * yc
            hc = modulate(rms_norm(cx, norm_ffn[i]), csh2, csc2)
            tokens = jnp.concatenate([hl.reshape(-1, D), hc.reshape(-1, D)], axis=0)
            y = moe(tokens, router_w[i], router_b[i], exp_w_gu[i], exp_b_gu[i], exp_w_down[i], exp_b_down[i])
            lat = lat + g2 * y[:B * T].reshape(B, T, D)
            cx = cx + cg2 * y[B * T:].reshape(cx.shape)
        else:
            y = moe(hl.reshape(-1, D), router_w[i], router_b[i], exp_w_gu[i], exp_b_gu[i], exp_w_down[i], exp_b_down[i])
            lat = lat + g2 * y.reshape(B, T, D)
    return rms_norm(lat, final_norm)
```

```python
import numpy as np
from contextlib import ExitStack
import concourse.bass as bass
import concourse.mybir as mybir
from concourse.bass_utils import run_bass_kernel_spmd

F32 = mybir.dt.float32
BF16 = mybir.dt.bfloat16
ALU = mybir.AluOpType
AF = mybir.ActivationFunctionType

T_LAT = 4096
T_CTX = 256
NTOK = T_LAT + T_CTX
NDS = 16
EPS = 1e-6


class Tr:
    ENG = ("pe", "act", "dve", "pool", "sp")

    def __init__(self, nc, es):
        self.nc = nc
        self.sem = {k: es.enter_context(nc.semaphore("s_" + k)) for k in self.ENG}
        self.dsem = [es.enter_context(nc.semaphore("d%d" % i)) for i in range(NDS)]
        self.cnt = {k: 0 for k in self.ENG}
        self.dcnt = [0] * NDS
        self.dnext = {"sp": 0, "pool": 0}
        self.seen = {k: {} for k in self.ENG}
        self.lastw = {}
        self.readers = {}
        self.ops = {k: [] for k in self.ENG}

    def _semh(self, key):
        return self.sem[key] if isinstance(key, str) else self.dsem[key[1]]

    def _collect(self, e, reads, writes, extra=()):
        need = {}

        def add(t):
            if t is None:
                return
            k, v = t
            if need.get(k, 0) < v:
                need[k] = v
        for r in reads:
            for k, v in self.lastw.get(r, {}).items():
                add((k, v))
        for w in writes:
            for k, v in self.lastw.get(w, {}).items():
                add((k, v))
            for k, v in self.readers.get(w, {}).items():
                add((k, v))
        for t in extra:
            add(t)
        wl = []
        for k, v in need.items():
            if k == e and e == "pe":
                continue
            if self.seen[e].get(k, 0) >= v:
                continue
            self.seen[e][k] = v
            wl.append((self._semh(k), v))
        return wl

    def _commit(self, ticket, reads, writes):
        k, v = ticket
        for r in reads:
            d = self.readers.setdefault(r, {})
            if d.get(k, 0) < v:
                d[k] = v
        for w in writes:
            d = self.lastw.setdefault(w, {})
            if d.get(k, 0) < v:
                d[k] = v
            self.readers[w] = {}

    def op(self, e, fn, reads=(), writes=(), signal=True):
        if not signal:
            self.ops[e].append(((), fn, None, 0))
            return None
        wl = self._collect(e, reads, writes)
        self.cnt[e] += 1
        ticket = (e, self.cnt[e])
        self.ops[e].append((wl, fn, self.sem[e], 1))
        self._commit(ticket, reads, writes)
        return ticket

    def dma(self, out, in_, reads=(), writes=(), q="sp", fn=None):
        half = NDS // 2
        j = self.dnext[q] + (0 if q == "sp" else half)
        self.dnext[q] = (self.dnext[q] + 1) % half
        extra = []
        if self.dcnt[j] > 0:
            extra.append((("d", j), self.dcnt[j]))
        wl = self._collect(q, reads, writes, extra)
        self.dcnt[j] += 16
        ticket = (("d", j), self.dcnt[j])
        if fn is None:
            fn = lambda eng, out=out, in_=in_: eng.dma_start(out=out, in_=in_)
        self.ops[q].append((wl, fn, self.dsem[j], 16))
        self._commit(ticket, reads, writes)
        return ticket

    def flush(self):
        nc = self.nc
        wl = []
        for j in range(NDS):
            if self.dcnt[j] > 0 and self.seen["sp"].get(("d", j), 0) < self.dcnt[j]:
                self.seen["sp"][("d", j)] = self.dcnt[j]
                wl.append((self.dsem[j], self.dcnt[j]))
        if wl:
            self.ops["sp"].append((wl, None, None, 0))
        ops = self.ops
        if not any(ops[k] for k in self.ENG):
            return
        self.ops = {k: [] for k in self.ENG}
        self.lastw = {}
        self.readers = {}

        def replay(eng, lst):
            for wl, fn, semh, inc in lst:
                for (sh, v) in wl:
                    eng.wait_ge(sh, v)
                if fn is not None:
                    ins = fn(eng)
                    if semh is not None:
                        ins.then_inc(semh, inc)

        with nc.Block() as block:
            if ops["sp"]:
                @block.sync
                def _(eng):
                    replay(eng, ops["sp"])
            if ops["pe"]:
                @block.tensor
                def _(eng):
                    replay(eng, ops["pe"])
            if ops["act"]:
                @block.scalar
                def _(eng):
                    replay(eng, ops["act"])
            if ops["dve"]:
                @block.vector
                def _(eng):
                    replay(eng, ops["dve"])
            if ops["pool"]:
                @block.gpsimd
                def _(eng):
                    replay(eng, ops["pool"])


class K:
    def __init__(self, nexp=32, stop_after=99, sparse=True):
        self.sparse = sparse
        self.nexp = nexp
        self.stop_after = stop_after
        nc = self.nc = bass.Bass("TRN2", target_bir_lowering=False)

        def din(name, shape, dt=F32):
            return nc.dram_tensor(name, shape, dt, kind="ExternalInput").ap()

        def dint(name, shape, dt=F32):
            return nc.dram_tensor(name, shape, dt, kind="Internal").ap()
        self.xT = din("xT", [128, 8 * T_LAT])
        self.ctxT = din("ctxT", [128, 8 * T_CTX])
        self.cs = din("cs", [128, 16])
        self.ada_wp = din("ada_wp", [2 * 12 * 128, 4096])
        self.ada_bp = din("ada_bp", [128, 96])
        self.gains = din("gains", [128, 40])
        self.pscale = din("pscale", [128, 8])
        self.poolw = din("poolw", [128, 2048])
        self.invc2 = din("invc2", [4, 4096])
        self.invc1 = din("invc1", [4, 256])
        self.win_p = din("win_p", [4 * 128, 4096])
        self.wout_p = din("wout_p", [2 * 128, 4096])
        self.convw = din("convw", [128, 32])
        self.lruv = din("lruv", [128, 56])
        self.wr_p = din("wr_p", [128, 4096])
        self.wi_p = din("wi_p", [128, 4096])
        self.router_wp = din("router_wp", [128, 512])
        self.router_bp = din("router_bp", [2, 32])
        self.wexp = din("wexp", [2 * 32 * 6 * 128, 4096])
        self.bgu = din("bgu", [128, 2 * 32 * 16])
        self.bdn = din("bdn", [64, 1024])
        self.identd = din("identd", [128, 128])
        self.identbd = din("identbd", [128, 128], BF16)
        self.utrid = din("utrid", [128, 128], BF16)
        self.iotapd = din("iotapd", [128, 1])
        self.jposd = din("jposd", [1, 66])
        self.bgu_e = din("bgu_e", [64 * 128, 16])
        self.xs_d = dint("xs_d", [66 * 512, 1024], BF16)
        self.ys_d = dint("ys_d", [66 * 512, 1024])
        self.outT = nc.dram_tensor("outT", [128, 8 * T_LAT], F32, kind="ExternalOutput").ap()
        self.lat_d = dint("lat_d", [128, 8 * T_LAT])
        self.ctx_d = dint("ctx_d", [128, 8 * T_CTX])
        self.h_d = dint("h_d", [128, 8 * NTOK], BF16)
        self.gt_d = dint("gt_d", [32, NTOK])
        self.u_d = dint("u_d", [128, 8 * NTOK])
        self.gg_d = dint("gg_d", [128, 8 * T_LAT])
        self.z_d = dint("z_d", [128, 8 * T_LAT], BF16)

    def zero_step(self, zt, n):
        if not self.sparse:
            return
        z = getattr(self, "_zrow", 0)
        for _ in range(n):
            if z < 66 * 512:
                self.T.dma(self.xs_d[z:z + 512, :].rearrange("(p r) c -> p (r c)", p=128), zt[:], reads=["zt"])
                z += 512
        self._zrow = z

    def un(self, name):
        self._uid = getattr(self, "_uid", 0) + 1
        return "%s_u%d" % (name, self._uid)

    def v3(self, ap2, c):
        return ap2.rearrange("p (c t) -> p c t", c=c)

    def tt(self, e, out, in0, in1, op, reads, writes):
        self.T.op(e, lambda g: g.tensor_tensor(out=out, in0=in0, in1=in1, op=op), reads, writes)

    def ts(self, e, out, in0, s1, s2, op0, op1, reads, writes):
        if s2 is None:
            self.T.op(e, lambda g: g.tensor_scalar(out=out, in0=in0, scalar1=s1, scalar2=None, op0=op0), reads, writes)
        else:
            self.T.op(e, lambda g: g.tensor_scalar(out=out, in0=in0, scalar1=s1, scalar2=s2, op0=op0, op1=op1), reads, writes)

    def stt(self, out, in0, scalar, in1, op0, op1, reads, writes):
        self.T.op("dve", lambda g: g.scalar_tensor_tensor(out=out, in0=in0, scalar=scalar, in1=in1, op0=op0, op1=op1), reads, writes)

    def actf(self, out, in_, func, reads, writes, bias=None, scale=None):
        kw = {}
        if bias is not None:
            kw["bias"] = bias
        if scale is not None:
            kw["scale"] = scale
        self.T.op("act", lambda g: g.activation(out=out, in_=in_, func=func, **kw), reads, writes)

    def mm(self, out, lhsT, rhs, start, stop, reads=(), writes=(), signal=True):
        self.T.op("pe", lambda g: g.matmul(out, lhsT=lhsT, rhs=rhs, start=start, stop=stop), reads, writes, signal=signal)

    def mm_group(self, out, pairs, reads, writes):
        n = len(pairs)
        for i, (l, r) in enumerate(pairs):
            sig = (i == 0) or (i == n - 1)
            self.mm(out, l, r, i == 0, i == n - 1, reads if sig else (), writes if sig else (), signal=sig)

    def tab(self, l, who, n, kind):
        i = ((l * 2 + who) * 2 + n) * 3 + kind
        return i * 8

    def build(self):
        nc = self.nc
        with ExitStack() as es:
            self.T = Tr(nc, es)
            self.tabt = es.enter_context(nc.sbuf_tensor("tabt", [128, 24 * 8], F32))
            self.ones = es.enter_context(nc.sbuf_tensor("ones", [128, 128], F32))
            self.ident = es.enter_context(nc.sbuf_tensor("ident", [128, 128], F32))
            self.gains_t = es.enter_context(nc.sbuf_tensor("gains_t", [128, 40], F32))
            self.epsc = es.enter_context(nc.sbuf_tensor("epsc", [128, 1], F32))
            with ExitStack() as es0:
                gen = self.phase_pool(es0)
                left = [9]

                def hook():
                    if left[0] > 0:
                        left[0] -= 1
                        next(gen, None)
                self.phase_adaln(es0, hook=hook)
                for _ in gen:
                    pass
                self.T.flush()
            if self.stop_after >= 2:
                if self.sparse:
                    self.phase_moe_sparse(0)
                else:
                    self.phase_norm_router(0)
                    self.T.flush()
                    self.phase_moe(0)
                self.T.flush()
            if self.stop_after >= 3:
                self.phase_lru()
                self.T.flush()
            if self.stop_after >= 4:
                if self.sparse:
                    self.phase_moe_sparse(1)
                else:
                    self.phase_norm_router(1)
                    self.T.flush()
                    self.phase_moe(1)
                self.T.flush()
            if not (self.sparse and self.stop_after >= 5):
                self.phase_final(self.stop_after >= 5)
            self.T.flush()
        return nc

    def phase_adaln(self, es, hook=None):
        nc, T = self.nc, self.T
        if True:
            sb = lambda name, shape, dt=F32: es.enter_context(nc.sbuf_tensor(self.un(name), shape, dt))
            cs_t = sb("cs_t", [128, 16]); sv = sb("sv", [128, 16])
            adab = sb("adab", [128, 96]); psc = sb("psc", [128, 8])
            modt = sb("modt", [128, 2 * 2 * 48])
            stg = [sb("a_stg%d" % i, [128, 4096]) for i in range(2)]
            tmp8 = sb("tmp8", [128, 8])
            psm = es.enter_context(nc.psum_tensor(self.un("psm"), [128, 192], F32))
            T.dma(cs_t[:], self.cs[:, :], writes=["cs_t"])
            T.dma(adab[:], self.ada_bp[:, :], writes=["adab"])
            T.dma(psc[:], self.pscale[:, :], writes=["psc"])
            T.dma(self.gains_t[:], self.gains[:, :], writes=["gains"])
            T.dma(self.ident[:], self.identd[:, :], writes=["ident"])
            T.op("pool", lambda g: g.memset(self.ones[:], 1.0), writes=["ones"])
            T.op("pool", lambda g: g.memset(self.epsc[:], EPS), writes=["epsc"])
            self.actf(sv[:], cs_t[:], AF.Silu, ["cs_t"], ["sv"])
            sv2 = sv[:].rearrange("p (two k) -> p k two", two=2)
            for l in range(2):
                for j in range(12):
                    s = stg[j % 2]
                    key = "a_stg%d" % (j % 2)
                    r0 = (l * 12 + j) * 128
                    T.dma(s[:], self.ada_wp[r0:r0 + 128, :], writes=[key])
                    for m in range(4):
                        jj = j * 4 + m
                        col = l * 96 + jj * 2
                        pairs = [(s[:, kc * 512 + m * 128: kc * 512 + (m + 1) * 128], sv2[:, kc, :]) for kc in range(8)]
                        self.mm_group(psm[:, col:col + 2], pairs, [key, "sv"], ["psm"])
                    if hook is not None and j % 2 == 1:
                        hook()
            for l in range(2):
                for who in range(2):
                    src = psm[:, l * 96:(l + 1) * 96].rearrange("p (j t) -> p j t", t=2)[:, :, who]
                    o = (l * 2 + who) * 48
                    self.tt("dve", modt[:, o:o + 48], src, adab[:, l * 48:(l + 1) * 48], ALU.add, ["psm", "adab"], ["modt"])
            for l in range(2):
                for who in range(2):
                    o = (l * 2 + who) * 48
                    for n in range(2):
                        gcol = (2 * l + n) * 8
                        a = self.tab(l, who, n, 0); b = self.tab(l, who, n, 1); g = self.tab(l, who, n, 2)
                        sc = modt[:, o + (1 + 3 * n) * 8: o + (2 + 3 * n) * 8]
                        sh = modt[:, o + (3 * n) * 8: o + (3 * n + 1) * 8]
                        gg = modt[:, o + (2 + 3 * n) * 8: o + (3 + 3 * n) * 8]
                        self.ts("dve", tmp8[:], sc, 1.0, None, ALU.add, None, ["modt"], ["tmp8"])
                        self.tt("dve", self.tabt[:, a:a + 8], tmp8[:], self.gains_t[:, gcol:gcol + 8], ALU.mult, ["tmp8", "gains"], ["tab"])
                        self.T.op("dve", lambda e, b=b, sh=sh: e.tensor_copy(out=self.tabt[:, b:b + 8], in_=sh), ["modt"], ["tab"])
                        if l == 0 and n == 0:
                            self.tt("dve", self.tabt[:, g:g + 8], gg, psc[:], ALU.mult, ["modt", "psc"], ["tab"])
                        else:
                            self.T.op("dve", lambda e, g=g, gg=gg: e.tensor_copy(out=self.tabt[:, g:g + 8], in_=gg), ["modt"], ["tab"])

    def rstd(self, src2, n, dst, rkeys, wkeys, sq, sqkey, ps, pskey, tmp, tmpkey):
        self.actf(sq[:, 0:8 * n], src2, AF.Square, rkeys, [sqkey])
        pairs = [(self.ones[:], sq[:, c * n:(c + 1) * n]) for c in range(8)]
        self.mm_group(ps[:, 0:n], pairs, [sqkey, "ones"], [pskey])
        self.actf(tmp[:, 0:n], ps[:, 0:n], AF.Sqrt, [pskey, "epsc"], [tmpkey], bias=self.epsc[:, 0:1], scale=1.0 / 1024.0)
        self.T.op("dve", lambda e: e.reciprocal(out=dst, in_=tmp[:, 0:n]), [tmpkey], wkeys)

    def phase_pool(self, es):
        nc, T = self.nc, self.T
        if True:
            sb = lambda name, shape, dt=F32: es.enter_context(nc.sbuf_tensor(self.un(name), shape, dt))
            rs_l = sb("rs_l", [128, T_LAT]); rs_c = sb("rs_c", [128, T_CTX])
            Lg = [sb("Lg%d" % i, [128, 4096]) for i in range(2)]
            tmp = sb("tmpr", [128, 512])
            Hc = sb("Hc", [128, 4096])
            sq = Hc
            ztp = sb("ztp", [128, 4096], BF16)
            T.op("pool", lambda e: e.memset(ztp[:], 0.0), writes=["zt"])
            P = sb("Pp", [128, 6400]); Q = sb("Qp", [128, 6400])
            d16 = [sb("d16_%d" % i, [128, 4096], BF16) for i in range(2)]
            invc = sb("invc", [128, 4096])
            pw32 = sb("pw32", [128, 2048]); pw16 = sb("pw16", [128, 2048], BF16)
            ps = [es.enter_context(nc.psum_tensor(self.un("pps%d" % i), [128, 512], F32)) for i in range(2)]
            T.dma(pw32[:], self.poolw[:, :], writes=["pw32"])
            self.actf(pw16[:], pw32[:], AF.Copy, ["pw32"], ["pw16"])
            for g in range(8):
                L = Lg[g % 2]; key = "Lg%d" % (g % 2)
                T.dma(self.v3(L[:, :], 8), self.v3(self.xT, 8)[:, :, g * 512:(g + 1) * 512], writes=[key])
                self.rstd(L[:, :], 512, rs_l[:, g * 512:(g + 1) * 512], [key], ["rs_l"], sq, "Hc", ps[g % 2], "pps%d" % (g % 2), tmp, "tmpr")
                yield
            L = Lg[0]
            T.dma(self.v3(L[:, 0:2048], 8), self.v3(self.ctxT, 8), writes=["Lg0"])
            self.rstd(L[:, 0:2048], 256, rs_c[:, :], ["Lg0"], ["rs_c"], sq, "Hc", ps[0], "pps0", tmp, "tmpr")
            yield
            for who in range(2):
                ntok = T_LAT if who == 0 else T_CTX
                R, Cw = (64, 64) if who == 0 else (1, 256)
                Ra, N = (R + 16, Cw + 16) if who == 0 else (1, Cw + 16)
                r_lo = 8 if who == 0 else 0
                src_d = self.xT if who == 0 else self.ctxT
                dst_d = self.lat_d if who == 0 else self.ctx_d
                rs = rs_l if who == 0 else rs_c
                invd = self.invc2 if who == 0 else self.invc1
                a0 = self.tab(0, who, 0, 0); b0 = self.tab(0, who, 0, 1); g0 = self.tab(0, who, 0, 2)
                Pv = P[:, 0:Ra * N].rearrange("p (r c) -> p r c", c=N)
                Qv = Q[:, 0:Ra * N].rearrange("p (r c) -> p r c", c=N)
                Pint = Pv[:, r_lo:r_lo + R, 8:8 + Cw]
                Qint = Qv[:, r_lo:r_lo + R, 8:8 + Cw]
                for gi in range(4):
                    k = gi + 1
                    T.dma(invc[:, 0:ntok], invd[gi:gi + 1, :].partition_broadcast(128), writes=["invc"])
                    for ci in range(2):
                        c = 2 * gi + ci
                        L = Lg[ci]; key = "Lg%d" % ci
                        T.dma(L[:, 0:ntok], src_d[:, c * ntok:(c + 1) * ntok], writes=[key])
                        self.tt("dve", Hc[:, 0:ntok], L[:, 0:ntok], rs[:, 0:ntok], ALU.mult, [key, "rs_l" if who == 0 else "rs_c"], ["Hc"])
                        self.ts("dve", Hc[:, 0:ntok], Hc[:, 0:ntok], self.tabt[:, a0 + c:a0 + c + 1], self.tabt[:, b0 + c:b0 + c + 1], ALU.mult, ALU.add, ["Hc", "tab"], ["Hc"])
                        T.op("pool", lambda e, ap=P[:, 0:Ra * N]: e.memset(ap, 0.0), writes=["P"])
                        self.actf(Pint, Hc[:, 0:ntok].rearrange("p (r c) -> p r c", c=Cw), AF.Copy, ["Hc"], ["P"])
                        bufs = [(Pv, "P"), (Qv, "Q")]
                        step = 0
                        passes = [2] if who == 1 else [2, 1]
                        for axis in passes:
                            Nn = N if axis == 2 else Ra
                            for s in range(1, k + 1):
                                (sv_, sk), (dv_, dk) = bufs[step % 2], bufs[(step + 1) % 2]
                                lo = [1, 2, 4, 8][s - 1]; hi = Nn - [0, 1, 3, 7][s - 1]
                                if s == 1:
                                    a_lo, a_hi, b_lo, b_hi = lo - 1, hi - 1, lo, hi
                                else:
                                    sh = 2 ** (s - 2)
                                    a_lo, a_hi, b_lo, b_hi = lo - sh, hi - sh, lo + sh, hi + sh
                                if axis == 2:
                                    o_ = dv_[:, :, lo:hi]; i0 = sv_[:, :, a_lo:a_hi]; i1 = sv_[:, :, b_lo:b_hi]
                                else:
                                    o_ = dv_[:, lo:hi, 8:8 + Cw]; i0 = sv_[:, a_lo:a_hi, 8:8 + Cw]; i1 = sv_[:, b_lo:b_hi, 8:8 + Cw]
                                self.tt("dve", o_, i0, i1, ALU.add, [sk], [dk])
                                step += 1
                        res_int, res_key, oth_int, oth_key = (Pint, "P", Qint, "Q") if step % 2 == 0 else (Qint, "Q", Pint, "P")
                        self.tt("dve", oth_int, res_int, invc[:, 0:ntok].rearrange("p (r c) -> p r c", c=Cw), ALU.mult, [res_key, "invc"], [oth_key])
                        self.tt("dve", d16[ci][:, 0:ntok].rearrange("p (r c) -> p r c", c=Cw), oth_int, Hc[:, 0:ntok].rearrange("p (r c) -> p r c", c=Cw), ALU.subtract, [oth_key, "Hc"], ["d16_%d" % ci])
                    ngrp = 8 if who == 0 else 1
                    gsz = 512 if who == 0 else 256
                    for oc in range(2):
                        c = 2 * gi + oc
                        for g in range(ngrp):
                            u = oc * ngrp + g
                            pst = ps[u % 2]; pk = "pps%d" % (u % 2)
                            pairs = [(pw16[:, (gi * 2 + kc) * 256 + oc * 128:(gi * 2 + kc) * 256 + (oc + 1) * 128],
                                      d16[kc][:, g * gsz:(g + 1) * gsz]) for kc in range(2)]
                            self.mm_group(pst[:, 0:gsz], pairs, ["pw16", "d16_0", "d16_1"], [pk])
                            self.stt(Lg[oc][:, g * gsz:(g + 1) * gsz], pst[:, 0:gsz], self.tabt[:, g0 + c:g0 + c + 1],
                                     Lg[oc][:, g * gsz:(g + 1) * gsz], ALU.mult, ALU.add, [pk, "Lg%d" % oc, "tab"], ["Lg%d" % oc])
                        T.dma(dst_d[:, c * ntok:(c + 1) * ntok], Lg[oc][:, 0:ntok], reads=["Lg%d" % oc])
                        self.zero_step(ztp, 4)

    def groups(self, l):
        gs = [("lat", g * 512, 512, g * 512) for g in range(8)]
        if l == 0:
            gs.append(("ctx", 0, 256, T_LAT))
        return gs

    def phase_norm_router(self, l, pers=None):
        nc, T = self.nc, self.T
        with ExitStack() as es:
            sb = lambda name, shape, dt=F32: es.enter_context(nc.sbuf_tensor(self.un(name), shape, dt))
            Lg = [sb("nLg%d" % i, [128, 4096]) for i in range(2)]
            sq = sb("nsq", [128, 4096]); tmp = sb("ntmp", [128, 512]); rs = sb("nrs", [128, 512])
            h32 = [sb("h32_%d" % i, [128, 4096]) for i in range(2)]
            h16 = [sb("h16_%d" % i, [128, 4096], BF16) for i in range(2)]
            rw = sb("rw", [128, 256]); rb = sb("rb", [128, 32])
            lg = sb("lg", [128, 32]); top8 = sb("top8", [128, 8]); nmx = sb("nmx", [128, 1])
            msk = sb("msk", [128, 32]); ex = sb("ex", [128, 32]); ssum = sb("ssum", [128, 1]); rsum = sb("rsum", [128, 1])
            G = sb("G", [128, 32]); GT = [sb("GT%d" % i, [32, 512]) for i in range(2)]
            psr = es.enter_context(nc.psum_tensor(self.un("psr"), [128, 512], F32))
            psl = [es.enter_context(nc.psum_tensor(self.un("psl%d" % i), [128, 128], F32)) for i in range(2)]
            pst = [es.enter_context(nc.psum_tensor(self.un("pst%d" % i), [32, 512], F32)) for i in range(2)]
            rb4 = sb("rb4", [128, 128]); msk4 = sb("msk4", [128, 128]); ex4 = sb("ex4", [128, 128]); ssum4 = sb("ssum4", [128, 4]); rsum4 = sb("rsum4", [128, 4])
            if pers is None:
                NTT_ = sum(n_ for (_, _, n_, _) in self.groups(l)) // 128
                pers = {"lg": sb("q_lg", [128, NTT_ * 32]), "top8": sb("q_top8", [128, NTT_ * 8]),
                        "G": sb("q_G", [128, NTT_ * 32]), "M": sb("q_M", [128, NTT_ * 32], BF16)}
            zt = None
            zrow = [0]
            if l == 0 and self.sparse:
                zt = sb("zt", [128, 4096], BF16)
                T.op("pool", lambda e: e.memset(zt[:], 0.0), writes=["zt"])
            T.dma(rw[:], self.router_wp[:, l * 256:(l + 1) * 256], writes=["rw"])
            for t_ in range(4):
                T.dma(rb4[:, t_ * 32:(t_ + 1) * 32], self.router_bp[l:l + 1, :].partition_broadcast(128), writes=["rb4"])
            tcount = 0
            for gidx, (kind, off, n, hoff) in enumerate(self.groups(l)):
                who = 0 if kind == "lat" else 1
                src_d = self.lat_d if who == 0 else self.ctx_d
                ntok = T_LAT if who == 0 else T_CTX
                L = Lg[gidx % 2]; key = "nLg%d" % (gidx % 2)
                T.dma(self.v3(L[:, 0:8 * n], 8), self.v3(src_d, 8)[:, :, off:off + n], writes=[key])
                self.rstd(L[:, 0:8 * n], n, rs[:, 0:n], [key], ["nrs"], sq, "nsq", psr, "psr", tmp, "ntmp")
                a = self.tab(l, who, 1, 0); b = self.tab(l, who, 1, 1)
                H = h32[gidx % 2]; hk = "h32_%d" % (gidx % 2)
                H16 = h16[gidx % 2]; hk16 = "h16_%d" % (gidx % 2)
                for c in range(8):
                    self.tt("dve", H[:, c * n:(c + 1) * n], L[:, c * n:(c + 1) * n], rs[:, 0:n], ALU.mult, [key, "nrs"], [hk])
                    self.ts("dve", H[:, c * n:(c + 1) * n], H[:, c * n:(c + 1) * n], self.tabt[:, a + c:a + c + 1], self.tabt[:, b + c:b + c + 1], ALU.mult, ALU.add, [hk], [hk])
                self.actf(H16[:, 0:8 * n], H[:, 0:8 * n], AF.Copy, [hk], [hk16])
                T.dma(self.v3(self.h_d, 8)[:, :, hoff:hoff + n], self.v3(H16[:, 0:8 * n], 8), reads=[hk16])
                gt = GT[gidx % 2]; gk = "GT%d" % (gidx % 2)
                nt4 = n // 128
                tti0 = hoff // 128
                pl_ = psl[gidx % 2]; plk = "psl%d" % (gidx % 2)
                ptt = pst[gidx % 2]; ptk = "pst%d" % (gidx % 2)
                for t in range(nt4):
                    pairs = [(H[:, c * n + t * 128:c * n + (t + 1) * 128], rw[:, c * 32:(c + 1) * 32]) for c in range(8)]
                    self.mm_group(pl_[:, t * 32:(t + 1) * 32], pairs, [hk, "rw"], [plk])
                W4 = nt4 * 32
                lg4 = pers["lg"][:, tti0 * 32: tti0 * 32 + W4]
                G4 = pers["G"][:, tti0 * 32: tti0 * 32 + W4]
                M4 = pers["M"][:, tti0 * 32: tti0 * 32 + W4]
                t8 = pers["top8"]

                def v4(ap2):
                    return ap2.rearrange("p (t e) -> p t e", e=32)

                def bc(ap2, col0, tstride):
                    pstp = ap2.ap[0][0]
                    return bass.AP(ap2.tensor, ap2.offset + col0, [[pstp, 128], [tstride, nt4], [0, 32]])
                self.tt("dve", lg4, pl_[:, 0:W4], rb4[:, 0:W4], ALU.add, [plk, "rb4"], ["lg"])
                for t in range(nt4):
                    T.op("dve", lambda e, t=t, tti0=tti0: e.max(out=t8[:, (tti0 + t) * 8:(tti0 + t + 1) * 8], in_=pers["lg"][:, (tti0 + t) * 32:(tti0 + t + 1) * 32]), ["lg"], ["top8"])
                t8b = t8[:, :]
                self.tt("dve", v4(msk4[:, 0:W4]), v4(lg4), bc(t8b, tti0 * 8 + 3, 8), ALU.is_ge, ["lg", "top8"], ["msk"])
                T.op("dve", lambda e, M4=M4, W4=W4: e.tensor_copy(out=M4, in_=msk4[:, 0:W4]), ["msk"], ["Mall"])
                self.tt("dve", v4(ex4[:, 0:W4]), v4(lg4), bc(t8b, tti0 * 8 + 0, 8), ALU.subtract, ["lg", "top8"], ["ex"])
                self.actf(ex4[:, 0:W4], ex4[:, 0:W4], AF.Exp, ["ex"], ["ex"])
                self.tt("dve", ex4[:, 0:W4], ex4[:, 0:W4], msk4[:, 0:W4], ALU.mult, ["ex", "msk"], ["ex"])
                T.op("dve", lambda e, W4=W4, nt4=nt4: e.reduce_sum(out=ssum4[:, 0:nt4], in_=v4(ex4[:, 0:W4]), axis=mybir.AxisListType.X), ["ex"], ["ssum"])
                T.op("dve", lambda e, nt4=nt4: e.reciprocal(out=rsum4[:, 0:nt4], in_=ssum4[:, 0:nt4]), ["ssum"], ["rsum"])
                self.tt("dve", v4(G4), v4(ex4[:, 0:W4]), bc(rsum4[:, :], 0, 1), ALU.mult, ["ex", "rsum"], ["G"])
                for t in range(nt4):
                    sig = t in (0, nt4 - 1)
                    T.op("pe", lambda e, t=t, ptt=ptt, tti0=tti0: e.transpose(out=ptt[:, t * 128:(t + 1) * 128], in_=pers["G"][:, (tti0 + t) * 32:(tti0 + t + 1) * 32], identity=self.ident[:]),
                         ["G", "ident"] if sig else (), [ptk] if sig else (), signal=sig)
                self.actf(gt[:, 0:n], ptt[:, 0:n], AF.Copy, [ptk], [gk])
                T.dma(self.gt_d[:, hoff:hoff + n], gt[:, 0:n], reads=[gk])
                if zt is not None:
                    self.zero_step(zt, 4 if gidx < 8 else 66)
            T.flush()

    def phase_moe(self, l):
        nc, T = self.nc, self.T
        gs = self.groups(l)
        blocks = [gs[0:3], gs[3:6], gs[6:]]
        TB = 1536
        with ExitStack() as es:
            sb = lambda name, shape, dt=F32: es.enter_context(nc.sbuf_tensor(self.un(name), shape, dt))
            Lb = sb("Lb", [128, 8 * TB]); Hb = sb("Hb", [128, 8 * TB], BF16); Ab = sb("Ab", [128, 8 * TB], BF16)
            stg = [sb("stg%d" % i, [128, 4096]) for i in range(2)]
            NW = 4
            w16 = [sb("w16_%d" % i, [128, 4096], BF16) for i in range(NW)]
            tsg = [sb("tsg%d" % i, [128, 512]) for i in range(2)]
            tr1 = [sb("tr1%d" % i, [128, 512]) for i in range(2)]
            tr2 = [sb("tr2%d" % i, [128, 512]) for i in range(2)]
            tq = [sb("tq%d" % i, [128, 512]) for i in range(2)]
            g2n = sb("g2n", [128, 16])
            Gb = sb("Gb", [128, TB]); GTs = sb("GTs", [32, TB])
            bg = sb("bg", [128, 512]); bgs = sb("bgs", [128, 512]); bd = sb("bd", [32, 1024])
            psg = [es.enter_context(nc.psum_tensor(self.un("psg%d" % i), [128, 512], F32)) for i in range(2)]
            psu = [es.enter_context(nc.psum_tensor(self.un("psu%d" % i), [128, 512], F32)) for i in range(2)]
            psy = [es.enter_context(nc.psum_tensor(self.un("psy%d" % i), [128, 512], F32)) for i in range(2)]
            T.dma(bg[:], self.bgu[:, l * 512:(l + 1) * 512], writes=["bg"])
            T.dma(bd[:], self.bdn[l * 32:(l + 1) * 32, :], writes=["bd"])
            for who in range(2):
                g2c = self.tab(l, who, 1, 2)
                self.ts("dve", g2n[:, who * 8:(who + 1) * 8], self.tabt[:, g2c:g2c + 8], -1.0 / 1.702, None, ALU.mult, None, ["tab"], ["g2n"])
            bg3 = bg[:].rearrange("p (e j) -> p e j", j=16)
            bgs3 = bgs[:].rearrange("p (e j) -> p e j", j=16)
            self.ts("dve", bgs3[:, :, 0:8], bg3[:, :, 0:8], 1.702, None, ALU.mult, None, ["bg"], ["bgs"])
            self.ts("dve", bgs3[:, :, 8:16], bg3[:, :, 8:16], 7.0, None, ALU.add, None, ["bg"], ["bgs"])
            q_issue = [0]
            ucount = [0, 0]
            pend = [None]
            C1 = 11.914 / (1.0 + float(np.exp(-11.914)))
            c14 = sb("c14", [128, 1])
            T.op("pool", lambda e: e.memset(c14[:], 14.0), writes=["c14"])
            for bi, blk in enumerate(blocks):
                offs = []
                o = 0
                for (kind, off, n, hoff) in blk:
                    offs.append(o)
                    o += n
                nb = o
                jobs = []
                for e in range(self.nexp):
                    for p in range(6):
                        jobs.append((e, p))

                def issue(idx):
                    e, p = jobs[idx]
                    q = q_issue[0]
                    q_issue[0] += 1
                    s = stg[q % 2]; sk = "stg%d" % (q % 2)
                    w = w16[q % NW]; wk = "w16_%d" % (q % NW)
                    r0 = ((l * 32 + e) * 6 + p) * 128
                    T.dma(s[:], self.wexp[r0:r0 + 128, :], writes=[sk])
                    self.actf(w[:], s[:], AF.Copy, [sk], [wk])
                    return (w, wk)
                issued = {}
                nxt = 0
                for _ in range(min(4, len(jobs))):
                    issued[nxt] = issue(nxt)
                    nxt += 1
                for gi, (kind, off, n, hoff) in enumerate(blk):
                    src_d = self.lat_d if kind == "lat" else self.ctx_d
                    bo = offs[gi]
                    T.dma(self.v3(Lb[:, :], 8)[:, :, bo:bo + n], self.v3(src_d, 8)[:, :, off:off + n], writes=[("Lb", gi)])
                    T.dma(self.v3(Hb[:, :], 8)[:, :, bo:bo + n], self.v3(self.h_d, 8)[:, :, hoff:hoff + n], writes=["Hb"])
                    T.dma(GTs[:, bo:bo + n], self.gt_d[:, hoff:hoff + n], writes=["GTs"])
                for gi, (kind, off, n, hoff) in enumerate(blk):
                    who = 0 if kind == "lat" else 1
                    g2 = self.tab(l, who, 1, 2)
                    bo = offs[gi]
                    for dc in range(8):
                        par = ucount[1] % 2; ucount[1] += 1
                        self.mm(psy[par][:, 0:n], bd[:, dc * 128:(dc + 1) * 128], GTs[:, bo:bo + n], True, True, ["bd", "GTs"], ["psy%d" % par])
                        lsl = Lb[:, dc * TB + bo: dc * TB + bo + n]
                        self.stt(lsl, psy[par][:, 0:n], self.tabt[:, g2 + dc:g2 + dc + 1], lsl, ALU.mult, ALU.add, ["psy%d" % par, ("Lb", gi)], [("Lb", gi)])
                for e in range(self.nexp):
                    for gi, (kind, off, n, hoff) in enumerate(blk):
                        bo = offs[gi]
                        T.dma(Gb[:, bo:bo + n], self.gt_d[e:e + 1, hoff:hoff + n].partition_broadcast(128), writes=["Gb"])
                    base = e * 6
                    for half in range(2):
                        (wg, wgk) = issued.pop(base + 2 * half)
                        (wu, wuk) = issued.pop(base + 2 * half + 1)
                        for fl in range(4):
                            f = half * 4 + fl
                            for gi, (kind, off, n, hoff) in enumerate(blk):
                                bo = offs[gi]
                                par = ucount[0] % 2; ucount[0] += 1
                                pg, pu = psg[par], psu[par]
                                pgk, puk = "psg%d" % par, "psu%d" % par
                                pairs = [(wg[:, kc * 512 + fl * 128: kc * 512 + (fl + 1) * 128], Hb[:, kc * TB + bo: kc * TB + bo + n]) for kc in range(8)]
                                self.mm_group(pg[:, 0:n], pairs, [wgk, "Hb"], [pgk])
                                pairs = [(wu[:, kc * 512 + fl * 128: kc * 512 + (fl + 1) * 128], Hb[:, kc * TB + bo: kc * TB + bo + n]) for kc in range(8)]
                                self.mm_group(pu[:, 0:n], pairs, [wuk, "Hb"], [puk])
                                ks = "t%d" % par
                                self.actf(tr1[par][:, 0:n], pu[:, 0:n], AF.Relu, [puk, "bgs"], [ks + "r1"], bias=bgs[:, e * 16 + 8 + f:e * 16 + 8 + f + 1], scale=1.0)
                                self.actf(tsg[par][:, 0:n], pg[:, 0:n], AF.Silu, [pgk, "bgs"], [ks + "s"], bias=bgs[:, e * 16 + f:e * 16 + f + 1], scale=1.702)
                                self.actf(tr2[par][:, 0:n], tr1[par][:, 0:n], AF.Relu, [ks + "r1", "c14"], [ks + "r2"], bias=c14[:, 0:1], scale=-1.0)
                                self.stt(tq[par][:, 0:n], tsg[par][:, 0:n], C1, Gb[:, bo:bo + n], ALU.min, ALU.mult, [ks + "s", "Gb"], [ks + "q"])
                                if pend[0] is not None:
                                    pend[0]()
                                def d2(par=par, n=n, f=f, bo=bo, gi=gi, ks=ks):
                                    self.stt(Ab[:, f * TB + bo: f * TB + bo + n], tr2[par][:, 0:n], 8.0, tq[par][:, 0:n], ALU.subtract, ALU.mult, [ks + "r2", ks + "q"], [("Ab", gi)])
                                pend[0] = d2
                        if pend[0] is not None:
                            pend[0]()
                            pend[0] = None
                        for _ in range(2):
                            if nxt < len(jobs):
                                issued[nxt] = issue(nxt)
                                nxt += 1
                    for half in range(2):
                        (wd, wdk) = issued.pop(base + 4 + half)
                        for dl in range(4):
                            dc = half * 4 + dl
                            for gi, (kind, off, n, hoff) in enumerate(blk):
                                who = 0 if kind == "lat" else 1
                                g2 = self.tab(l, who, 1, 2)
                                bo = offs[gi]
                                par = ucount[1] % 2; ucount[1] += 1
                                py = psy[par]; pyk = "psy%d" % par
                                pairs = [(wd[:, fc * 512 + dl * 128: fc * 512 + (dl + 1) * 128], Ab[:, fc * TB + bo: fc * TB + bo + n]) for fc in range(8)]
                                self.mm_group(py[:, 0:n], pairs, [wdk, ("Ab", gi)], [pyk])
                                lsl = Lb[:, dc * TB + bo: dc * TB + bo + n]
                                self.stt(lsl, py[:, 0:n], g2n[:, who * 8 + dc:who * 8 + dc + 1], lsl, ALU.mult, ALU.add, [pyk, ("Lb", gi), "g2n"], [("Lb", gi)])
                        if nxt < len(jobs):
                            issued[nxt] = issue(nxt)
                            nxt += 1
                for gi, (kind, off, n, hoff) in enumerate(blk):
                    dst_d = self.lat_d if kind == "lat" else self.ctx_d
                    bo = offs[gi]
                    T.dma(self.v3(dst_d, 8)[:, :, off:off + n], self.v3(Lb[:, :], 8)[:, :, bo:bo + n], reads=[("Lb", gi)])
            T.flush()


    def phase_moe_sparse(self, l):
        nc, T = self.nc, self.T
        I32 = mybir.dt.int32
        X = mybir.AxisListType.X
        gs = self.groups(l)
        NTT = sum(n for (_, _, n, _) in gs) // 128
        NT = (4 * NTT * 128 + 32 * 511) // 512
        TS = 512
        with ExitStack() as pes:
            psb_ = lambda name, shape, dt=F32: pes.enter_context(nc.sbuf_tensor(self.un(name), shape, dt))
            pers = {"lg": psb_("p_lg", [128, NTT * 32]), "top8": psb_("p_top8", [128, NTT * 8]),
                    "G": psb_("p_G", [128, NTT * 32]), "M": psb_("p_M", [128, NTT * 32], BF16)}
            dest_i = psb_("dest_i", [128, NTT * 4], I32)
            gk = psb_("gk", [128, NTT * 4])
            widx_i = psb_("widx_i", [128, NT * 12], I32)
            bidx_i = psb_("bidx_i", [128, NT], I32)
            identb = psb_("identb", [128, 128], BF16)
            self.phase_norm_router(l, pers)
            T.flush()
            with ExitStack() as es:
                sb = lambda name, shape, dt=F32: es.enter_context(nc.sbuf_tensor(self.un(name), shape, dt))
                onesb = sb("onesb", [128, 128], BF16); utri = sb("utri", [128, 128], BF16)
                iotap = sb("iotap", [128, 1]); jpos = sb("jpos", [128, 66]); one32 = sb("one32", [128, 32])
                cnt = sb("cnt", [128, 32]); ntl = sb("ntl", [128, 32]); pc = sb("pc", [128, 32]); pend = sb("pend", [128, 32]); pstart = sb("pstart", [128, 32])
                cmp3 = sb("cmp3", [128, 66 * 32]); ej = sb("ej", [128, 66]); wix = sb("wix", [128, 66]); wix6 = sb("wix6", [128, 66 * 6]); bix = sb("bix", [128, 66])
                Dt = sb("Dt", [128, 32]); oh = sb("oh", [128, 32]); pr = sb("pr", [128, 32]); destf = sb("destf", [128, NTT * 4]); gkf = sb("gkf", [128, NTT * 4])
                ps_c = es.enter_context(nc.psum_tensor(self.un("ps_c"), [128, 32], F32))
                ps_r = [es.enter_context(nc.psum_tensor(self.un("ps_r%d" % i), [128, 32], F32)) for i in range(2)]
                T.dma(utri[:], self.utrid[:, :], writes=["utri"])
                T.dma(identb[:], self.identbd[:, :], writes=["identb"])
                T.dma(iotap[:], self.iotapd[:, :], writes=["iotap"])
                T.dma(jpos[:], self.jposd[0:1, :].partition_broadcast(128), writes=["jpos"])
                T.op("pool", lambda e: e.memset(onesb[:], 1.0), writes=["onesb"])
                T.op("pool", lambda e: e.memset(one32[:], 1.0), writes=["one32"])
                M = pers["M"]
                pairs = [(onesb[:], M[:, tt * 32:(tt + 1) * 32]) for tt in range(NTT)]
                self.mm_group(ps_c[:, :], pairs, ["onesb", "Mall"], ["ps_c"])
                T.op("dve", lambda e: e.tensor_copy(out=cnt[:], in_=ps_c[:, :]), ["ps_c"], ["cnt"])
                self.ts("dve", ntl[:], cnt[:], 0.0, None, ALU.is_gt, None, ["cnt"], ["ntl"])
                for m in range(1, 9):
                    self.stt(ntl[:], cnt[:], 512.0 * m, ntl[:], ALU.is_gt, ALU.add, ["cnt", "ntl"], ["ntl"])
                self.ts("dve", pc[:], ntl[:], 512.0, None, ALU.mult, None, ["ntl"], ["pc"])
                T.op("dve", lambda e: e.tensor_tensor_scan(out=pend[:], data0=one32[:], data1=pc[:], initial=0.0, op0=ALU.mult, op1=ALU.add), ["one32", "pc"], ["pend"])
                self.tt("dve", pstart[:], pend[:], pc[:], ALU.subtract, ["pend", "pc"], ["pstart"])
                a0 = pend[:, :]; pstp = a0.ap[0][0]
                in0 = bass.AP(a0.tensor, a0.offset, [[pstp, 128], [0, NT], [1, 32]])
                a1 = jpos[:, :]; pstp1 = a1.ap[0][0]
                in1 = bass.AP(a1.tensor, a1.offset, [[pstp1, 128], [1, NT], [0, 32]])
                c3 = cmp3[:, 0:NT * 32].rearrange("p (j e) -> p j e", e=32)
                T.op("dve", lambda e: e.tensor_tensor(out=c3, in0=in0, in1=in1, op=ALU.is_le), ["pend", "jpos"], ["cmp3"])
                T.op("dve", lambda e: e.reduce_sum(out=ej[:, 0:NT], in_=c3, axis=X), ["cmp3"], ["ej"])
                self.ts("dve", ej[:, 0:NT], ej[:, 0:NT], 31.0, 32.0 * l, ALU.min, ALU.add, ["ej"], ["ej"])
                self.ts("dve", wix[:, 0:NT], ej[:, 0:NT], 768.0, None, ALU.mult, None, ["ej"], ["wix"])
                w6 = wix6[:, 0:NT * 6].rearrange("p (j s) -> p j s", s=6)
                for p_ in range(6):
                    self.ts("dve", w6[:, :, p_], wix[:, 0:NT], iotap[:, 0:1], 128.0 * p_, ALU.add, ALU.add, ["wix", "iotap"], ["wix6"])
                wix12 = sb("wix12", [128, 66 * 12])
                w12 = wix12[:, 0:NT * 12].rearrange("p (q h) -> p q h", h=2)
                for h_ in range(2):
                    self.ts("dve", w12[:, :, h_], wix6[:, 0:NT * 6], 2.0, float(h_), ALU.mult, ALU.add, ["wix6"], ["wix12"])
                T.op("dve", lambda e: e.tensor_copy(out=widx_i[:, :], in_=wix12[:, 0:NT * 12]), ["wix12"], ["widx_i"])
                self.ts("dve", bix[:, 0:NT], ej[:, 0:NT], 128.0, iotap[:, 0:1], ALU.mult, ALU.add, ["ej", "iotap"], ["bix"])
                T.op("dve", lambda e: e.tensor_copy(out=bidx_i[:, :], in_=bix[:, 0:NT]), ["bix"], ["bidx_i"])
                D4 = sb("D4", [128, 128]); oh4 = sb("oh4", [128, 128]); pr4 = sb("pr4", [128, 128])
                ps_r4 = [es.enter_context(nc.psum_tensor(self.un("ps_r4_%d" % i), [128, 128], F32)) for i in range(2)]
                pstart4 = sb("pstart4", [128, 128])
                for t_ in range(4):
                    T.op("dve", lambda e, t_=t_: e.tensor_copy(out=pstart4[:, t_ * 32:(t_ + 1) * 32], in_=pstart[:]), ["pstart"], ["pstart4"])

                def v4(ap2):
                    return ap2.rearrange("p (t e) -> p t e", e=32)
                for b0 in range(0, NTT, 4):
                    nb4 = min(4, NTT - b0)
                    W4 = nb4 * 32
                    pr_ = ps_r4[(b0 // 4) % 2]; prk = "ps_r4_%d" % ((b0 // 4) % 2)
                    for t_ in range(nb4):
                        tt = b0 + t_
                        pairs = [(utri[:], M[:, tt * 32:(tt + 1) * 32])] + [(onesb[:], M[:, t2 * 32:(t2 + 1) * 32]) for t2 in range(tt)]
                        o_ = pr_[:, t_ * 32:(t_ + 1) * 32]
                        if len(pairs) == 1:
                            self.mm(o_, pairs[0][0], pairs[0][1], True, True, ["utri", "Mall", "onesb"], [prk])
                        else:
                            self.mm_group(o_, pairs, ["utri", "Mall", "onesb"], [prk])
                    self.tt("dve", D4[:, 0:W4], pr_[:, 0:W4], pstart4[:, 0:W4], ALU.add, [prk, "pstart4"], ["D4"])
                    lg4 = pers["lg"][:, b0 * 32: b0 * 32 + W4]; G4 = pers["G"][:, b0 * 32: b0 * 32 + W4]
                    t8b = pers["top8"][:, :]
                    for k in range(4):
                        pstp = t8b.ap[0][0]
                        bck = bass.AP(t8b.tensor, t8b.offset + b0 * 8 + k, [[pstp, 128], [8, nb4], [0, 32]])
                        self.tt("dve", v4(oh4[:, 0:W4]), v4(lg4), bck, ALU.is_equal, [], ["oh4"])
                        self.tt("dve", pr4[:, 0:W4], oh4[:, 0:W4], D4[:, 0:W4], ALU.mult, ["oh4", "D4"], ["pr4"])
                        c0 = b0 * 4 + k * nb4
                        T.op("dve", lambda e, c0=c0, nb4=nb4, W4=W4: e.reduce_sum(out=destf[:, c0:c0 + nb4], in_=v4(pr4[:, 0:W4]), axis=X), ["pr4"], ["destf"])
                        self.tt("dve", pr4[:, 0:W4], oh4[:, 0:W4], G4, ALU.mult, ["oh4"], ["pr4"])
                        T.op("dve", lambda e, c0=c0, nb4=nb4, W4=W4: e.reduce_sum(out=gkf[:, c0:c0 + nb4], in_=v4(pr4[:, 0:W4]), axis=X), ["pr4"], ["gkf"])
                T.op("dve", lambda e: e.tensor_copy(out=dest_i[:, :], in_=destf[:, :]), ["destf"], ["dest_i"])
                self.ts("dve", gk[:, :], gkf[:, :], -1.0 / 1.702, None, ALU.mult, None, ["gkf"], ["gk"])
                T.flush()
            def dcol(tt, k):
                b0 = (tt // 4) * 4
                nb4 = min(4, NTT - b0)
                return b0 * 4 + k * nb4 + (tt - b0)
            XW = 1024
            with ExitStack() as es:
                sb = lambda name, shape, dt=F32: es.enter_context(nc.sbuf_tensor(self.un(name), shape, dt))
                hf = [sb("hf%d" % i, [128, 1024], BF16) for i in range(4)]
                XT = [sb("XT_%d" % i, [128, XW], BF16) for i in range(4)]
                psX = [es.enter_context(nc.psum_tensor(self.un("psX%d" % i), [128, 1024], BF16)) for i in range(2)]
                for tt in range(NTT):
                    par = tt % 4
                    pp = tt % 2
                    T.dma(self.v3(hf[par][:, :], 8), self.v3(self.h_d, 8)[:, :, tt * 128:(tt + 1) * 128], writes=["hf%d" % par])
                    for c in range(8):
                        sig = c in (0, 7)
                        T.op("pe", lambda e, c=c, par=par, pp=pp: e.transpose(out=psX[pp][:, c * 128:(c + 1) * 128], in_=hf[par][:, c * 128:(c + 1) * 128], identity=identb[:]),
                             ["hf%d" % par, "identb"] if sig else (), ["psX%d" % pp] if sig else (), signal=sig)
                    if tt % 2 == 0:
                        self.actf(XT[par][:, :], psX[pp][:, :], AF.Copy, ["psX%d" % pp], [("XT", par)])
                    else:
                        T.op("dve", lambda e, par=par, pp=pp: e.tensor_copy(out=XT[par][:, :], in_=psX[pp][:, :]), ["psX%d" % pp], [("XT", par)])
                    for k in range(4):
                        def scat(eng, par=par, dc_=dcol(tt, k)):
                            return eng.indirect_dma_start(out=self.xs_d, out_offset=bass.IndirectOffsetOnAxis(ap=dest_i[:, dc_:dc_ + 1], axis=0),
                                                          in_=XT[par][:, :], in_offset=None)
                        T.dma(None, None, reads=[("XT", par)], writes=["xs_d"], q="pool", fn=scat)
                T.flush()
            with ExitStack() as es:
                sb = lambda name, shape, dt=F32: es.enter_context(nc.sbuf_tensor(self.un(name), shape, dt))
                NW = 8
                w16 = [sb("s_w16_%d" % i, [128, 4096], BF16) for i in range(NW)]
                Xs = [sb("Xs%d" % i, [128, XW], BF16) for i in range(4)]
                Hb = [sb("sHb%d" % i, [128, 8 * TS], BF16) for i in range(2)]
                Ab = [sb("sAb%d" % i, [128, 8 * TS], BF16) for i in range(2)]
                gc = [sb("gc%d" % i, [128, 4]) for i in range(2)]
                bgt = [sb("bgt%d" % i, [128, 16]) for i in range(2)]
                bgst = [sb("bgst%d" % i, [128, 16]) for i in range(2)]
                tsg = [sb("s_tsg%d" % i, [128, 512]) for i in range(2)]
                tr1 = [sb("s_tr1%d" % i, [128, 512]) for i in range(2)]
                tr2 = [sb("s_tr2%d" % i, [128, 512]) for i in range(2)]
                tq = [sb("s_tq%d" % i, [128, 512]) for i in range(2)]
                Ys = [sb("Ys%d" % i, [128, 1024]) for i in range(2)]
                c14 = sb("s_c14", [128, 1])
                psT = [es.enter_context(nc.psum_tensor(self.un("psT%d" % i), [128, 512], BF16)) for i in range(2)]
                psg = [es.enter_context(nc.psum_tensor(self.un("spsg%d" % i), [128, 512], F32)) for i in range(2)]
                psu = [es.enter_context(nc.psum_tensor(self.un("spsu%d" % i), [128, 512], F32)) for i in range(2)]
                psy = [es.enter_context(nc.psum_tensor(self.un("spsy%d" % i), [128, 512], F32)) for i in range(2)]
                T.op("pool", lambda e: e.memset(c14[:], 14.0), writes=["c14"])
                C1 = 11.914 / (1.0 + float(np.exp(-11.914)))
                q_issue = [0]
                jobs = [(j, p) for j in range(NT) for p in range(6)]

                wexp2 = self.wexp.rearrange("r (h c) -> (r h) c", h=2)

                def issue(idx):
                    j, p = jobs[idx]
                    q = q_issue[0]; q_issue[0] += 1
                    w = w16[q % NW]; wk = "s_w16_%d" % (q % NW)
                    for h_ in range(2):
                        def gat(eng, w=w, j=j, p=p, h_=h_):
                            col = (j * 6 + p) * 2 + h_
                            return eng.indirect_dma_start(out=w[:, h_ * 2048:(h_ + 1) * 2048], out_offset=None, in_=wexp2,
                                                          in_offset=bass.IndirectOffsetOnAxis(ap=widx_i[:, col:col + 1], axis=0))
                        T.dma(None, None, reads=[], writes=[wk], q="pool", fn=gat)
                    return (w, wk)
                issued = {}
                nxt = 0
                for _ in range(NW):
                    issued[nxt] = issue(nxt); nxt += 1
                uc = [0, 0, 0]
                pend = [None]
                def prep_load(j):
                    par = j % 2
                    def bgat(eng, par=par, j=j):
                        return eng.indirect_dma_start(out=bgt[par][:], out_offset=None, in_=self.bgu_e,
                                                      in_offset=bass.IndirectOffsetOnAxis(ap=bidx_i[:, j:j + 1], axis=0))
                    T.dma(None, None, reads=[], writes=["bgt%d" % par], q="pool", fn=bgat)
                    self.ts("dve", bgst[par][:, 0:8], bgt[par][:, 0:8], 1.702, None, ALU.mult, None, ["bgt%d" % par], ["bgst%d" % par])
                    self.ts("dve", bgst[par][:, 8:16], bgt[par][:, 8:16], 7.0, None, ALU.add, None, ["bgt%d" % par], ["bgst%d" % par])
                    for s4 in range(4):
                        xs = Xs[s4]; xk = "Xs%d" % s4
                        r0 = j * TS + s4 * 128
                        T.dma(xs[:], self.xs_d[r0:r0 + 128, :], writes=[xk])

                def prep_tr(j):
                    par = j % 2
                    hb = Hb[par]; hbk = "sHb%d" % par
                    for c in range(8):
                        tp = uc[2] % 2; uc[2] += 1
                        for s4 in range(4):
                            sig = s4 in (0, 3)
                            T.op("pe", lambda e, c=c, s4=s4, tp=tp: e.transpose(out=psT[tp][:, s4 * 128:(s4 + 1) * 128], in_=Xs[s4][:, c * 128:(c + 1) * 128], identity=identb[:]),
                                 ["Xs0", "Xs1", "Xs2", "Xs3", "identb"] if sig else (), ["psT%d" % tp] if sig else (), signal=sig)
                        if c % 2 == 0:
                            self.actf(hb[:, c * TS:(c + 1) * TS], psT[tp][:, :], AF.Copy, ["psT%d" % tp], [hbk])
                        else:
                            T.op("dve", lambda e, c=c, tp=tp, hb=hb: e.tensor_copy(out=hb[:, c * TS:(c + 1) * TS], in_=psT[tp][:, :]), ["psT%d" % tp], [hbk])
                prep_load(0)
                prep_tr(0)
                for j in range(NT):
                    par = j % 2
                    hb = Hb[par]; hbk = "sHb%d" % par
                    ab = Ab[par]; abk = "sAb%d" % par
                    if j + 1 < NT:
                        prep_load(j + 1)
                    base = j * 6
                    for half in range(2):
                        (wg, wgk) = issued.pop(base + 2 * half)
                        (wu, wuk) = issued.pop(base + 2 * half + 1)
                        for fl in range(4):
                            f = half * 4 + fl
                            up = uc[0] % 2; uc[0] += 1
                            pg, pu = psg[up], psu[up]
                            pgk, puk = "spsg%d" % up, "spsu%d" % up
                            pairs = [(wg[:, kc * 512 + fl * 128: kc * 512 + (fl + 1) * 128], hb[:, kc * TS:(kc + 1) * TS]) for kc in range(8)]
                            self.mm_group(pg[:, :], pairs, [wgk, hbk], [pgk])
                            pairs = [(wu[:, kc * 512 + fl * 128: kc * 512 + (fl + 1) * 128], hb[:, kc * TS:(kc + 1) * TS]) for kc in range(8)]
                            self.mm_group(pu[:, :], pairs, [wuk, hbk], [puk])
                            ks = "st%d" % up
                            bk = "bgst%d" % par
                            self.actf(tr1[up][:, :], pu[:, :], AF.Relu, [puk, bk], [ks + "r1"], bias=bgst[par][:, 8 + f:9 + f], scale=1.0)
                            self.actf(tsg[up][:, :], pg[:, :], AF.Silu, [pgk, bk], [ks + "s"], bias=bgst[par][:, f:f + 1], scale=1.702)
                            self.actf(tr2[up][:, :], tr1[up][:, :], AF.Relu, [ks + "r1", "c14"], [ks + "r2"], bias=c14[:, 0:1], scale=-1.0)
                            self.ts("dve", tq[up][:, :], tsg[up][:, :], C1, None, ALU.min, None, [ks + "s"], [ks + "q"])
                            if pend[0] is not None:
                                pend[0]()
                            def d2(up=up, f=f, ab=ab, abk=abk, ks=ks):
                                self.stt(ab[:, f * TS:(f + 1) * TS], tr2[up][:, :], 8.0, tq[up][:, :], ALU.subtract, ALU.mult, [ks + "r2", ks + "q"], [abk])
                            pend[0] = d2
                        if pend[0] is not None:
                            pend[0](); pend[0] = None
                        if half == 0:
                            for _ in range(2):
                                if nxt < len(jobs):
                                    issued[nxt] = issue(nxt); nxt += 1
                    if j + 1 < NT:
                        prep_tr(j + 1)
                    for _ in range(2):
                        if nxt < len(jobs):
                            issued[nxt] = issue(nxt); nxt += 1
                    wd = [issued.pop(base + 4), issued.pop(base + 5)]
                    for s4 in range(4):
                        ysb = Ys[s4 % 2]; yk = "Ys%d" % (s4 % 2)
                        for half in range(2):
                            (w_, wk_) = wd[half]
                            yp = uc[1] % 2; uc[1] += 1
                            pairs = [(ab[:, fc * TS + s4 * 128: fc * TS + (s4 + 1) * 128], w_[:, fc * 512:(fc + 1) * 512]) for fc in range(8)]
                            self.mm_group(psy[yp][:, :], pairs, [wk_, abk], ["spsy%d" % yp])
                            self.actf(ysb[:, half * 512:(half + 1) * 512], psy[yp][:, :], AF.Copy, ["spsy%d" % yp], [yk])
                        r0 = j * TS + s4 * 128
                        T.dma(self.ys_d[r0:r0 + 128, :], ysb[:, :], reads=[yk], writes=["ys_d"])
                    for _ in range(2):
                        if nxt < len(jobs):
                            issued[nxt] = issue(nxt); nxt += 1
                T.flush()
            with ExitStack() as es:
                sb = lambda name, shape, dt=F32: es.enter_context(nc.sbuf_tensor(self.un(name), shape, dt))
                Y4 = [sb("Y4_%d" % i, [128, 4 * 1024]) for i in range(4)]
                S4 = sb("S4", [128, 4 * 1024])
                GTg = [sb("GTg%d" % i, [32, 512]) for i in range(2)]
                bd = sb("e_bd", [32, 1024])
                Lg = [sb("eL%d" % i, [128, 4096]) for i in range(2)]
                psb2 = [es.enter_context(nc.psum_tensor(self.un("psb2_%d" % i), [128, 512], F32)) for i in range(2)]
                psl2 = [es.enter_context(nc.psum_tensor(self.un("psl2_%d" % i), [128, 512], F32)) for i in range(2)]
                fuse_final = (l == 1 and self.stop_after >= 5)
                if fuse_final:
                    fsq = sb("fsq", [128, 4096]); ftmp = sb("ftmp", [128, 512]); frs = sb("frs", [128, 512])
                    fpsr = es.enter_context(nc.psum_tensor(self.un("fpsr"), [128, 512], F32))
                T.dma(bd[:], self.bdn[l * 32:(l + 1) * 32, :], writes=["e_bd"])
                uc2 = 0; uc3 = 0
                for gidx, (kind, off, n, hoff) in enumerate(gs):
                    who = 0 if kind == "lat" else 1
                    src_d = self.lat_d if who == 0 else self.ctx_d
                    g2 = self.tab(l, who, 1, 2)
                    L = Lg[gidx % 2]; lk = "eL%d" % (gidx % 2)
                    gt = GTg[gidx % 2]; gtk = "GTg%d" % (gidx % 2)
                    T.dma(self.v3(L[:, 0:8 * n], 8), self.v3(src_d, 8)[:, :, off:off + n], writes=[lk])
                    T.dma(gt[:, 0:n], self.gt_d[:, hoff:hoff + n], writes=[gtk])
                    nt4 = n // 128
                    for t in range(nt4):
                        tt = hoff // 128 + t
                        y4 = Y4[tt % 4]; yk = ("Y4", tt % 4)
                        for k in range(4):
                            def gat(eng, y4=y4, k=k, dc_=dcol(tt, k)):
                                return eng.indirect_dma_start(out=y4[:, k * 1024:(k + 1) * 1024], out_offset=None, in_=self.ys_d,
                                                              in_offset=bass.IndirectOffsetOnAxis(ap=dest_i[:, dc_:dc_ + 1], axis=0))
                            T.dma(None, None, reads=["ys_d"], writes=[yk], q="pool", fn=gat)
                        st = S4[:, t * 1024:(t + 1) * 1024]
                        g_ = [gk[:, dcol(tt, k):dcol(tt, k) + 1] for k in range(4)]
                        self.ts("dve", y4[:, 0:1024], y4[:, 0:1024], g_[0], None, ALU.mult, None, [yk], [yk])
                        self.stt(y4[:, 0:1024], y4[:, 1024:2048], g_[1], y4[:, 0:1024], ALU.mult, ALU.add, [yk], [yk])
                        self.stt(y4[:, 0:1024], y4[:, 2048:3072], g_[2], y4[:, 0:1024], ALU.mult, ALU.add, [yk], [yk])
                        self.stt(st, y4[:, 3072:4096], g_[3], y4[:, 0:1024], ALU.mult, ALU.add, [yk], [("S4", t)])
                        for half in range(2):
                            bp = uc2 % 2; uc2 += 1
                            self.mm(psb2[bp][:, :], gt[:, t * 128:(t + 1) * 128], bd[:, half * 512:(half + 1) * 512], True, True, [gtk, "e_bd"], ["psb2_%d" % bp])
                            sl = S4[:, t * 1024 + half * 512: t * 1024 + (half + 1) * 512]
                            self.tt("dve", sl, sl, psb2[bp][:, :], ALU.add, [("S4", t), "psb2_%d" % bp], [("S4", t)])
                    for c in range(8):
                        lp = uc3 % 2; uc3 += 1
                        for t in range(nt4):
                            sig = t in (0, nt4 - 1)
                            T.op("pe", lambda e, c=c, t=t, lp=lp: e.transpose(out=psl2[lp][:, t * 128:(t + 1) * 128], in_=S4[:, t * 1024 + c * 128: t * 1024 + (c + 1) * 128], identity=self.ident[:]),
                                 [("S4", t_) for t_ in range(nt4)] + ["ident"] if sig else (), ["psl2_%d" % lp] if sig else (), signal=sig)
                        sl = L[:, c * n:(c + 1) * n]
                        self.stt(sl, psl2[lp][:, 0:n], self.tabt[:, g2 + c:g2 + c + 1], sl, ALU.mult, ALU.add, ["psl2_%d" % lp, lk], [lk])
                    if fuse_final:
                        self.rstd(L[:, 0:8 * n], n, frs[:, 0:n], [lk], ["frs"], fsq, "fsq", fpsr, "fpsr", ftmp, "ftmp")
                        for c in range(8):
                            sl = L[:, c * n:(c + 1) * n]
                            self.stt(sl, sl, self.gains_t[:, 32 + c:33 + c], frs[:, 0:n], ALU.mult, ALU.mult, [lk, "frs"], [lk])
                        T.dma(self.v3(self.outT, 8)[:, :, off:off + n], self.v3(L[:, 0:8 * n], 8), reads=[lk])
                    else:
                        T.dma(self.v3(src_d, 8)[:, :, off:off + n], self.v3(L[:, 0:8 * n], 8), reads=[lk])
                T.flush()

    def phase_lru(self):
        nc, T = self.nc, self.T
        l = 1
        with ExitStack() as es:
            sb = lambda name, shape, dt=F32: es.enter_context(nc.sbuf_tensor(self.un(name), shape, dt))
            Lg = [sb("lLg%d" % i, [128, 4096]) for i in range(2)]
            sq = sb("lsq", [128, 4096]); tmp = sb("ltmp", [128, 512]); rs = sb("lrs", [128, 512])
            Ht = [sb("lHt%d" % i, [128, 512]) for i in range(2)]
            H16 = [sb("lH16_%d" % i, [128, 4096], BF16) for i in range(2)]
            win = [sb("win%d" % i, [128, 4096], BF16) for i in range(4)]
            xs = [sb("bxs%d" % i, [128, 512]) for i in range(2)]
            x2 = [sb("bx2%d" % i, [128, 512]) for i in range(2)]
            sg = [sb("bsg%d" % i, [128, 512]) for i in range(2)]
            GG = [sb("bGG%d" % i, [128, 4096]) for i in range(2)]
            UU = [sb("bUU%d" % i, [128, 4096]) for i in range(2)]
            psr = es.enter_context(nc.psum_tensor(self.un("lpsr"), [128, 512], F32))
            ps = [es.enter_context(nc.psum_tensor(self.un("bps%d" % i), [128, 512], F32)) for i in range(2)]
            for j in range(4):
                T.dma(Lg[j % 2][:], self.win_p[j * 128:(j + 1) * 128, :], writes=["lLg%d" % (j % 2)])
                self.actf(win[j][:], Lg[j % 2][:], AF.Copy, ["lLg%d" % (j % 2)], ["win%d" % j])
            gs = [("lat", g * 512, 512, g * 512) for g in range(8)] + [("ctx", 0, 256, T_LAT)]
            ucb = [0]

            def norm(gidx):
                (kind, off, n, hoff) = gs[gidx]
                who = 0 if kind == "lat" else 1
                src_d = self.lat_d if who == 0 else self.ctx_d
                L = Lg[gidx % 2]; key = "lLg%d" % (gidx % 2)
                T.dma(self.v3(L[:, 0:8 * n], 8), self.v3(src_d, 8)[:, :, off:off + n], writes=[key])
                self.rstd(L[:, 0:8 * n], n, rs[:, 0:n], [key], ["lrs"], sq, "lsq", psr, "lpsr", tmp, "ltmp")
                a = self.tab(l, who, 0, 0); b = self.tab(l, who, 0, 1)
                hh16 = H16[gidx % 2]; hk16 = "lH16_%d" % (gidx % 2)
                for c in range(8):
                    ht = Ht[c % 2]; htk = "lHt%d" % (c % 2)
                    self.tt("dve", ht[:, 0:n], L[:, c * n:(c + 1) * n], rs[:, 0:n], ALU.mult, [key, "lrs"], [htk])
                    self.ts("dve", hh16[:, c * n:(c + 1) * n], ht[:, 0:n], self.tabt[:, a + c:a + c + 1], self.tabt[:, b + c:b + c + 1], ALU.mult, ALU.add, [htk], [hk16])

            def proj(gidx):
                (kind, off, n, hoff) = gs[gidx]
                hg = H16[gidx % 2]; hk = "lH16_%d" % (gidx % 2)
                gg = GG[gidx % 2]; ggk = "bGG%d" % (gidx % 2)
                uu = UU[gidx % 2]; uuk = "bUU%d" % (gidx % 2)
                for oc in range(16):
                    if kind == "ctx" and oc < 8:
                        continue
                    par = ucb[0] % 2; ucb[0] += 1
                    p_ = ps[par]; pk = "bps%d" % par
                    w = win[oc // 4]; wk = "win%d" % (oc // 4)
                    ol = oc % 4
                    pairs = [(w[:, kc * 512 + ol * 128: kc * 512 + (ol + 1) * 128], hg[:, kc * n:(kc + 1) * n]) for kc in range(8)]
                    self.mm_group(p_[:, 0:n], pairs, [wk, hk], [pk])
                    if oc < 8:
                        k_ = "bx%d" % par
                        self.actf(xs[par][:, 0:n], p_[:, 0:n], AF.Copy, [pk], [k_ + "s"])
                        self.tt("dve", x2[par][:, 0:n], xs[par][:, 0:n], xs[par][:, 0:n], ALU.mult, [k_ + "s"], [k_ + "2"])
                        self.ts("dve", x2[par][:, 0:n], x2[par][:, 0:n], 0.044715, 1.0, ALU.mult, ALU.add, [k_ + "2"], [k_ + "2"])
                        self.tt("dve", x2[par][:, 0:n], x2[par][:, 0:n], xs[par][:, 0:n], ALU.mult, [k_ + "2", k_ + "s"], [k_ + "2"])
                        self.actf(sg[par][:, 0:n], x2[par][:, 0:n], AF.Sigmoid, [k_ + "2"], [k_ + "g"], scale=1.5957691216057308)
                        self.tt("dve", gg[:, oc * n:(oc + 1) * n], xs[par][:, 0:n], sg[par][:, 0:n], ALU.mult, [k_ + "s", k_ + "g"], [ggk])
                    else:
                        self.actf(uu[:, (oc - 8) * n:(oc - 7) * n], p_[:, 0:n], AF.Copy, [pk], [uuk])
                if kind == "lat":
                    T.dma(self.v3(self.gg_d, 8)[:, :, off:off + n], self.v3(gg[:, 0:8 * n], 8), reads=[ggk])
                    T.dma(self.v3(self.u_d, 8)[:, :, T_CTX + off:T_CTX + off + n], self.v3(uu[:, 0:8 * n], 8), reads=[uuk])
                else:
                    T.dma(self.v3(self.u_d, 8)[:, :, 0:T_CTX], self.v3(uu[:, 0:8 * n], 8), reads=[uuk])
            norm(0)
            for gidx in range(len(gs)):
                if gidx + 1 < len(gs):
                    norm(gidx + 1)
                proj(gidx)
            T.flush()
        with ExitStack() as es:
            sb = lambda name, shape, dt=F32: es.enter_context(nc.sbuf_tensor(self.un(name), shape, dt))
            cw = sb("cw", [128, 32]); lv = sb("lv", [128, 56]); cA = sb("cA", [128, 16]); tmp16 = sb("tmp16", [128, 16])
            w32 = sb("cw32", [128, 4096]); wr16 = sb("wr16", [128, 4096], BF16); wi16 = sb("wi16", [128, 4096], BF16)
            U = sb("cU", [128, NTOK])
            UC32 = [sb("UC32_%d" % i, [128, NTOK]) for i in range(2)]
            UC16 = [sb("UC16_%d" % i, [128, NTOK], BF16) for i in range(2)]
            Aa = sb("Aa", [128, NTOK]); Bb = sb("Bb", [128, NTOK]); Hs = sb("Hs", [128, NTOK])
            REC = sb("REC", [128, T_LAT]); Z16 = sb("Z16", [128, T_LAT], BF16)
            tr = [sb("ctr%d" % i, [128, 512]) for i in range(2)]
            ti = [sb("cti%d" % i, [128, 512]) for i in range(2)]
            tm = [sb("ctm%d" % i, [128, 512]) for i in range(2)]
            psr = [es.enter_context(nc.psum_tensor(self.un("cpr%d" % i), [128, 512], F32)) for i in range(2)]
            psi = [es.enter_context(nc.psum_tensor(self.un("cpi%d" % i), [128, 512], F32)) for i in range(2)]
            T.dma(cw[:], self.convw[:, :], writes=["cw"])
            T.dma(lv[:], self.lruv[:, :], writes=["lv"])
            T.dma(w32[:], self.wr_p[:, :], writes=["cw32"])
            self.actf(wr16[:], w32[:], AF.Copy, ["cw32"], ["wr16"])
            T.dma(w32[:], self.wi_p[:, :], writes=["cw32"])
            self.actf(wi16[:], w32[:], AF.Copy, ["cw32"], ["wi16"])
            self.actf(tmp16[:], lv[:, 40:56], AF.Exp, ["lv"], ["tmp16"], scale=-1.0)
            self.actf(tmp16[:], tmp16[:], AF.Ln, ["tmp16"], ["tmp16"], bias=1.0, scale=1.0)
            self.ts("dve", cA[:], tmp16[:], -8.0, None, ALU.mult, None, ["tmp16"], ["cA"])
            segs = [(0, T_CTX), (T_CTX, NTOK)]
            gs = [(s, min(512, NTOK - s)) for s in range(0, NTOK, 512)]
            uc = 0
            g1 = self.tab(1, 0, 0, 2)
            for nb in range(4):
                for ci in range(2):
                    c = 2 * nb + ci
                    T.dma(U[:], self.u_d[:, c * NTOK:(c + 1) * NTOK], writes=["cU"])
                    uc32 = UC32[ci]; k32 = "UC32_%d" % ci
                    for (s0, s1) in segs:
                        self.actf(uc32[:, s0:s1], U[:, s0:s1], AF.Identity, ["cU", "cw", "lv"], [k32], bias=lv[:, c:c + 1], scale=cw[:, 2 * 8 + c:2 * 8 + c + 1])
                        self.stt(uc32[:, s0 + 2:s1], U[:, s0:s1 - 2], cw[:, 0 * 8 + c:0 * 8 + c + 1], uc32[:, s0 + 2:s1], ALU.mult, ALU.add, ["cU", k32], [k32])
                        self.stt(uc32[:, s0 + 1:s1], U[:, s0:s1 - 1], cw[:, 1 * 8 + c:1 * 8 + c + 1], uc32[:, s0 + 1:s1], ALU.mult, ALU.add, ["cU", k32], [k32])
                        self.stt(uc32[:, s0:s1 - 1], U[:, s0 + 1:s1], cw[:, 3 * 8 + c:3 * 8 + c + 1], uc32[:, s0:s1 - 1], ALU.mult, ALU.add, ["cU", k32], [k32])
                    self.actf(UC16[ci][:], uc32[:], AF.Copy, [k32], ["UC16_%d" % ci])
                for oc in range(2):
                    c = 2 * nb + oc
                    for d in range(2):
                        for (s, n) in gs:
                            par = uc % 2; uc += 1
                            wbase = ((d * 4 + nb) * 2) * 256
                            pairs = [(wr16[:, wbase + kc * 256 + oc * 128: wbase + kc * 256 + (oc + 1) * 128], UC16[kc][:, s:s + n]) for kc in range(2)]
                            self.mm_group(psr[par][:, 0:n], pairs, ["wr16", "UC16_0", "UC16_1"], ["cpr%d" % par])
                            pairs = [(wi16[:, wbase + kc * 256 + oc * 128: wbase + kc * 256 + (oc + 1) * 128], UC16[kc][:, s:s + n]) for kc in range(2)]
                            self.mm_group(psi[par][:, 0:n], pairs, ["wi16", "UC16_0", "UC16_1"], ["cpi%d" % par])
                            self.actf(Aa[:, s:s + n], psr[par][:, 0:n], AF.Sigmoid, ["cpr%d" % par, "lv"], ["Aa"], bias=lv[:, 8 + d * 8 + c:8 + d * 8 + c + 1], scale=1.0)
                            self.actf(Bb[:, s:s + n], psi[par][:, 0:n], AF.Sigmoid, ["cpi%d" % par, "lv"], ["Bb"], bias=lv[:, 24 + d * 8 + c:24 + d * 8 + c + 1], scale=1.0)
                        self.actf(Aa[:, :], Aa[:, :], AF.Exp, ["Aa", "cA"], ["Aa"], scale=cA[:, d * 8 + c:d * 8 + c + 1])
                        self.actf(Hs[:, :], Aa[:, :], AF.Square, ["Aa"], ["Hs"])
                        self.actf(Hs[:, :], Hs[:, :], AF.Sqrt, ["Hs"], ["Hs"], bias=1.0, scale=-1.0)
                        self.tt("dve", Bb[:, :], Bb[:, :], UC32[oc][:, :], ALU.mult, ["Bb", "UC32_%d" % oc], ["Bb"])
                        self.tt("dve", Bb[:, :], Bb[:, :], Hs[:, :], ALU.mult, ["Bb", "Hs"], ["Bb"])
                        if d == 0:
                            T.op("dve", lambda e: e.tensor_tensor_scan(out=Hs[:, 0:T_CTX], data0=Aa[:, 0:T_CTX], data1=Bb[:, 0:T_CTX], initial=0.0, op0=ALU.mult, op1=ALU.add), ["Aa", "Bb"], ["Hs"])
                            T.op("dve", lambda e: e.tensor_tensor_scan(out=REC[:, :], data0=Aa[:, T_CTX:NTOK], data1=Bb[:, T_CTX:NTOK], initial=Hs[:, T_CTX - 1:T_CTX], op0=ALU.mult, op1=ALU.add), ["Aa", "Bb", "Hs"], ["REC"])
                        else:
                            def rev(t, a, b):
                                apx = t[:, a:b]
                                pstep = apx.ap[0][0]
                                return bass.AP(apx.tensor, apx.offset + (b - a - 1), [[pstep, 128], [-1, b - a]])
                            T.op("dve", lambda e: e.tensor_tensor_scan(out=rev(Hs, 0, T_CTX), data0=rev(Aa, 0, T_CTX), data1=rev(Bb, 0, T_CTX), initial=0.0, op0=ALU.mult, op1=ALU.add), ["Aa", "Bb"], ["Hs"])
                            T.op("dve", lambda e: e.tensor_tensor_scan(out=rev(Hs, T_CTX, NTOK), data0=rev(Aa, T_CTX, NTOK), data1=rev(Bb, T_CTX, NTOK), initial=Hs[:, 0:1], op0=ALU.mult, op1=ALU.add), ["Aa", "Bb", "Hs"], ["Hs"])
                            self.tt("dve", REC[:, :], REC[:, :], Hs[:, T_CTX:NTOK], ALU.add, ["REC", "Hs"], ["REC"])
                    T.dma(U[:, 0:T_LAT], self.gg_d[:, c * T_LAT:(c + 1) * T_LAT], writes=["cU"])
                    self.tt("dve", Z16[:, :], REC[:, :], U[:, 0:T_LAT], ALU.mult, ["REC", "cU"], ["Z16"])
                    T.dma(self.z_d[:, c * T_LAT:(c + 1) * T_LAT], Z16[:, :], reads=["Z16"])
            T.flush()
        with ExitStack() as es:
            sb = lambda name, shape, dt=F32: es.enter_context(nc.sbuf_tensor(self.un(name), shape, dt))
            stg = [sb("dstg%d" % i, [128, 4096]) for i in range(2)]
            wo = [sb("wo%d" % i, [128, 4096], BF16) for i in range(2)]
            Zg = [sb("dZ%d" % i, [128, 4096], BF16) for i in range(2)]
            Lg = [sb("dL%d" % i, [128, 4096]) for i in range(2)]
            ps = [es.enter_context(nc.psum_tensor(self.un("dps%d" % i), [128, 512], F32)) for i in range(2)]
            for j in range(2):
                T.dma(stg[j][:], self.wout_p[j * 128:(j + 1) * 128, :], writes=["dstg%d" % j])
                self.actf(wo[j][:], stg[j][:], AF.Copy, ["dstg%d" % j], ["wo%d" % j])
            g1 = self.tab(1, 0, 0, 2)
            uc = 0
            for g in range(8):
                z = Zg[g % 2]; zk = "dZ%d" % (g % 2)
                L = Lg[g % 2]; lk = "dL%d" % (g % 2)
                T.dma(self.v3(z[:, :], 8), self.v3(self.z_d, 8)[:, :, g * 512:(g + 1) * 512], writes=[zk])
                T.dma(self.v3(L[:, :], 8), self.v3(self.lat_d, 8)[:, :, g * 512:(g + 1) * 512], writes=[lk])
                for dc in range(8):
                    par = uc % 2; uc += 1
                    w = wo[dc // 4]; dl = dc % 4
                    pairs = [(w[:, kc * 512 + dl * 128: kc * 512 + (dl + 1) * 128], z[:, kc * 512:(kc + 1) * 512]) for kc in range(8)]
                    self.mm_group(ps[par][:, :], pairs, ["wo%d" % (dc // 4), zk], ["dps%d" % par])
                    sl = L[:, dc * 512:(dc + 1) * 512]
                    self.stt(sl, ps[par][:, :], self.tabt[:, g1 + dc:g1 + dc + 1], sl, ALU.mult, ALU.add, ["dps%d" % par, lk], [lk])
                T.dma(self.v3(self.lat_d, 8)[:, :, g * 512:(g + 1) * 512], self.v3(L[:, :], 8), reads=[lk])
            T.flush()

    def phase_final(self, do_norm):
        nc, T = self.nc, self.T
        with ExitStack() as es:
            sb = lambda name, shape, dt=F32: es.enter_context(nc.sbuf_tensor(self.un(name), shape, dt))
            Lg = [sb("fL%d" % i, [128, 4096]) for i in range(2)]
            sq = sb("fsq", [128, 4096]); tmp = sb("ftmp", [128, 512]); rs = sb("frs", [128, 512])
            psr = es.enter_context(nc.psum_tensor(self.un("fpsr"), [128, 512], F32))
            for g in range(8):
                L = Lg[g % 2]; key = "fL%d" % (g % 2)
                T.dma(self.v3(L[:, :], 8), self.v3(self.lat_d, 8)[:, :, g * 512:(g + 1) * 512], writes=[key])
                if do_norm:
                    self.rstd(L[:, :], 512, rs[:, :], [key], ["frs"], sq, "fsq", psr, "fpsr", tmp, "ftmp")
                    for c in range(8):
                        sl = L[:, c * 512:(c + 1) * 512]
                        self.stt(sl, sl, self.gains_t[:, 32 + c:33 + c], rs[:, :], ALU.mult, ALU.mult, [key, "frs"], [key])
                T.dma(self.v3(self.outT, 8)[:, :, g * 512:(g + 1) * 512], self.v3(L[:, :], 8), reads=[key])
            T.flush()


def _col(v, n):
    return np.ascontiguousarray(np.asarray(v, np.float32).reshape(n, 128).T)


def _pieces(w, ncol_pieces):
    w = np.asarray(w, np.float32)
    return np.ascontiguousarray(w.reshape(8, 128, ncol_pieces, 512).transpose(2, 1, 0, 3)).reshape(ncol_pieces, 128, 4096)


def _inv_counts():
    def bounds(n, w):
        idx = np.arange(n)
        return np.clip(idx - w // 2, 0, n), np.clip(idx + w // 2, 0, n)
    i2 = np.zeros((4, 4096), np.float32)
    i1 = np.zeros((4, 256), np.float32)
    for gi, w in enumerate((2, 4, 8, 16)):
        r0, r1 = bounds(64, w)
        cnt = ((r1 - r0)[:, None] * (r1 - r0)[None, :]).astype(np.float32)
        i2[gi] = (np.float32(1.0) / cnt).reshape(-1)
        l0, l1 = bounds(256, w)
        i1[gi] = np.float32(1.0) / (l1 - l0).astype(np.float32)
    return i2, i1


def prep_shared(inp, nexp=32):
    f = lambda k: np.asarray(inp[k], np.float32)
    sh = {}
    ada_w = f("ada_w")
    sh["ada_wp"] = np.concatenate([_pieces(ada_w[l], 12) for l in range(2)], 0).reshape(2 * 12 * 128, 4096)
    sh["ada_bp"] = np.concatenate([_col(f("ada_b")[l], 48) for l in range(2)], 1)
    sh["gains"] = np.concatenate([_col(f("norm_mix")[0], 8), _col(f("norm_ffn")[0], 8), _col(f("norm_mix")[1], 8),
                                  _col(f("norm_ffn")[1], 8), _col(f("final_norm"), 8)], 1)
    sh["pscale"] = _col(f("pool_scale")[0], 8)
    sh["poolw"] = np.ascontiguousarray(f("pool_w")[0].reshape(4, 2, 128, 256).transpose(2, 0, 1, 3)).reshape(128, 2048)
    sh["invc2"], sh["invc1"] = _inv_counts()
    sh["win_p"] = _pieces(f("lru_w_in")[0], 4).reshape(4 * 128, 4096)
    sh["wout_p"] = _pieces(f("lru_w_out")[0], 2).reshape(2 * 128, 4096)
    sh["convw"] = np.concatenate([_col(f("lru_conv_w")[0][k], 8) for k in range(4)], 1)
    sh["lruv"] = np.concatenate([_col(f("lru_conv_b")[0], 8)] + [_col(f("lru_b_r")[0][d], 8) for d in range(2)] +
                                [_col(f("lru_b_i")[0][d], 8) for d in range(2)] + [_col(f("lru_lam")[0][d], 8) for d in range(2)], 1)
    for nm, key in (("wr_p", "lru_w_r"), ("wi_p", "lru_w_i")):
        w = f(key)[0]
        sh[nm] = np.ascontiguousarray(w.reshape(2, 4, 2, 128, 256).transpose(3, 0, 1, 2, 4)).reshape(128, 4096)
    rw = f("router_w")
    sh["router_wp"] = np.ascontiguousarray(rw.reshape(2, 8, 128, 32).transpose(2, 0, 1, 3)).reshape(128, 512)
    sh["router_bp"] = np.ascontiguousarray(f("router_b"))
    wgu = f("exp_w_gu"); wdn = f("exp_w_down")
    wexp = np.empty((2, 32, 6, 128, 4096), np.float32)
    for l in range(2):
        for e in range(nexp):
            pg = _pieces(wgu[l, e], 4)
            pd = _pieces(wdn[l, e], 2)
            wexp[l, e, 0] = pg[0]; wexp[l, e, 1] = pg[2]; wexp[l, e, 2] = pg[1]; wexp[l, e, 3] = pg[3]
            wexp[l, e, 4] = pd[0]; wexp[l, e, 5] = pd[1]
    sh["wexp"] = wexp.reshape(2 * 32 * 6 * 128, 4096)
    bgu = f("exp_b_gu")
    sh["bgu"] = np.ascontiguousarray(bgu.reshape(2, 32, 16, 128).transpose(3, 0, 1, 2)).reshape(128, 1024)
    sh["bdn"] = np.ascontiguousarray(f("exp_b_down").reshape(64, 1024))
    sh["identd"] = np.eye(128, dtype=np.float32)
    import ml_dtypes
    sh["identbd"] = np.eye(128).astype(ml_dtypes.bfloat16)
    sh["utrid"] = np.triu(np.ones((128, 128), np.float32), 1).astype(ml_dtypes.bfloat16)
    sh["iotapd"] = np.arange(128, dtype=np.float32).reshape(128, 1)
    sh["jposd"] = (np.arange(66, dtype=np.float32) * 512.0).reshape(1, 66)
    sh["bgu_e"] = np.ascontiguousarray(bgu.reshape(2, 32, 16, 128).transpose(0, 1, 3, 2)).reshape(64 * 128, 16)
    return sh


def prep_core(inp, b):
    x = np.asarray(inp["x"][b], np.float32)
    ctx = np.asarray(inp["ctx"][b], np.float32)
    d = {}
    d["xT"] = np.ascontiguousarray(x.T.reshape(8, 128, T_LAT).transpose(1, 0, 2)).reshape(128, 8 * T_LAT)
    d["ctxT"] = np.ascontiguousarray(ctx.T.reshape(8, 128, T_CTX).transpose(1, 0, 2)).reshape(128, 8 * T_CTX)
    d["cs"] = np.concatenate([_col(inp["c"][b], 8), _col(inp["c_ctx"], 8)], 1)
    return d


def unpack_out(o):
    return np.ascontiguousarray(o.reshape(128, 8, T_LAT).transpose(1, 0, 2).reshape(1024, T_LAT).T)


_CACHE = {}


def kernel(**inputs):
    if "nc" not in _CACHE:
        _CACHE["nc"] = K().build()
    nc = _CACHE["nc"]
    sh = prep_shared(inputs)
    in_maps = []
    for b in range(8):
        m = dict(sh)
        m.update(prep_core(inputs, b))
        in_maps.append(m)
    res = run_bass_kernel_spmd(nc, in_maps, core_ids=list(range(8)))
    out = np.stack([unpack_out(res.results[b]["outT"]) for b in range(8)], 0)
    return out.astype(np.float32)
```

```python
import numpy as np
from contextlib import ExitStack
import concourse.bass as bass
import concourse.mybir as mybir
from concourse.bass_utils import run_bass_kernel_spmd

F32 = mybir.dt.float32
BF16 = mybir.dt.bfloat16
ALU = mybir.AluOpType
AF = mybir.ActivationFunctionType

T_LAT = 4096
T_CTX = 256
NTOK = T_LAT + T_CTX
NDS = 16
EPS = 1e-6


class Tr:
    ENG = ("pe", "act", "dve", "pool", "sp")

    def __init__(self, nc, es):
        self.nc = nc
        self.sem = {k: es.enter_context(nc.semaphore("s_" + k)) for k in self.ENG}
        self.dsem = [es.enter_context(nc.semaphore("d%d" % i)) for i in range(NDS)]
        self.cnt = {k: 0 for k in self.ENG}
        self.dcnt = [0] * NDS
        self.dnext = {"sp": 0, "pool": 0}
        self.seen = {k: {} for k in self.ENG}
        self.lastw = {}
        self.readers = {}
        self.ops = {k: [] for k in self.ENG}

    def _semh(self, key):
        return self.sem[key] if isinstance(key, str) else self.dsem[key[1]]

    def _collect(self, e, reads, writes, extra=()):
        need = {}

        def add(t):
            if t is None:
                return
            k, v = t
            if need.get(k, 0) < v:
                need[k] = v
        for r in reads:
            for k, v in self.lastw.get(r, {}).items():
                add((k, v))
        for w in writes:
            for k, v in self.lastw.get(w, {}).items():
                add((k, v))
            for k, v in self.readers.get(w, {}).items():
                add((k, v))
        for t in extra:
            add(t)
        wl = []
        for k, v in need.items():
            if k == e and e == "pe":
                continue
            if self.seen[e].get(k, 0) >= v:
                continue
            self.seen[e][k] = v
            wl.append((self._semh(k), v))
        return wl

    def _commit(self, ticket, reads, writes):
        k, v = ticket
        for r in reads:
            d = self.readers.setdefault(r, {})
            if d.get(k, 0) < v:
                d[k] = v
        for w in writes:
            d = self.lastw.setdefault(w, {})
            if d.get(k, 0) < v:
                d[k] = v
            self.readers[w] = {}

    def op(self, e, fn, reads=(), writes=(), signal=True):
        if not signal:
            self.ops[e].append(((), fn, None, 0))
            return None
        wl = self._collect(e, reads, writes)
        self.cnt[e] += 1
        ticket = (e, self.cnt[e])
        self.ops[e].append((wl, fn, self.sem[e], 1))
        self._commit(ticket, reads, writes)
        return ticket

    def dma(self, out, in_, reads=(), writes=(), q="sp", fn=None):
        half = NDS // 2
        j = self.dnext[q] + (0 if q == "sp" else half)
        self.dnext[q] = (self.dnext[q] + 1) % half
        extra = []
        if self.dcnt[j] > 0:
            extra.append((("d", j), self.dcnt[j]))
        wl = self._collect(q, reads, writes, extra)
        self.dcnt[j] += 16
        ticket = (("d", j), self.dcnt[j])
        if fn is None:
            fn = lambda eng, out=out, in_=in_: eng.dma_start(out=out, in_=in_)
        self.ops[q].append((wl, fn, self.dsem[j], 16))
        self._commit(ticket, reads, writes)
        return ticket

    def flush(self):
        nc = self.nc
        wl = []
        for j in range(NDS):
            if self.dcnt[j] > 0 and self.seen["sp"].get(("d", j), 0) < self.dcnt[j]:
                self.seen["sp"][("d", j)] = self.dcnt[j]
                wl.append((self.dsem[j], self.dcnt[j]))
        if wl:
            self.ops["sp"].append((wl, None, None, 0))
        ops = self.ops
        if not any(ops[k] for k in self.ENG):
            return
        self.ops = {k: [] for k in self.ENG}
        self.lastw = {}
        self.readers = {}

        def replay(eng, lst):
            for wl, fn, semh, inc in lst:
                for (sh, v) in wl:
                    eng.wait_ge(sh, v)
                if fn is not None:
                    ins = fn(eng)
                    if semh is not None:
                        ins.then_inc(semh, inc)

        with nc.Block() as block:
            if ops["sp"]:
                @block.sync
                def _(eng):
                    replay(eng, ops["sp"])
            if ops["pe"]:
                @block.tensor
                def _(eng):
                    replay(eng, ops["pe"])
            if ops["act"]:
                @block.scalar
                def _(eng):
                    replay(eng, ops["act"])
            if ops["dve"]:
                @block.vector
                def _(eng):
                    replay(eng, ops["dve"])
            if ops["pool"]:
                @block.gpsimd
                def _(eng):
                    replay(eng, ops["pool"])


class K:
    def __init__(self, nexp=32, stop_after=99, sparse=True):
        self.sparse = sparse
        self.nexp = nexp
        self.stop_after = stop_after
        nc = self.nc = bass.Bass("TRN2", target_bir_lowering=False)

        def din(name, shape, dt=F32):
            return nc.dram_tensor(name, shape, dt, kind="ExternalInput").ap()

        def dint(name, shape, dt=F32):
            return nc.dram_tensor(name, shape, dt, kind="Internal").ap()
        self.xT = din("xT", [128, 8 * T_LAT])
        self.ctxT = din("ctxT", [128, 8 * T_CTX])
        self.cs = din("cs", [128, 16])
        self.ada_wp = din("ada_wp", [2 * 12 * 128, 4096])
        self.ada_bp = din("ada_bp", [128, 96])
        self.gains = din("gains", [128, 40])
        self.pscale = din("pscale", [128, 8])
        self.poolw = din("poolw", [128, 2048])
        self.invc2 = din("invc2", [4, 4096])
        self.invc1 = din("invc1", [4, 256])
        self.win_p = din("win_p", [4 * 128, 4096])
        self.wout_p = din("wout_p", [2 * 128, 4096])
        self.convw = din("convw", [128, 32])
        self.lruv = din("lruv", [128, 56])
        self.wr_p = din("wr_p", [128, 4096])
        self.wi_p = din("wi_p", [128, 4096])
        self.router_wp = din("router_wp", [128, 512])
        self.router_bp = din("router_bp", [2, 32])
        self.wexp = din("wexp", [2 * 32 * 6 * 128, 4096])
        self.bgu = din("bgu", [128, 2 * 32 * 16])
        self.bdn = din("bdn", [64, 1024])
        self.identd = din("identd", [128, 128])
        self.identbd = din("identbd", [128, 128], BF16)
        self.utrid = din("utrid", [128, 128], BF16)
        self.iotapd = din("iotapd", [128, 1])
        self.jposd = din("jposd", [1, 66])
        self.bgu_e = din("bgu_e", [64 * 128, 16])
        self.xs_d = dint("xs_d", [66 * 512, 1024], BF16)
        self.ys_d = dint("ys_d", [66 * 512, 1024])
        self.outT = nc.dram_tensor("outT", [128, 8 * T_LAT], F32, kind="ExternalOutput").ap()
        self.lat_d = dint("lat_d", [128, 8 * T_LAT])
        self.ctx_d = dint("ctx_d", [128, 8 * T_CTX])
        self.h_d = dint("h_d", [128, 8 * NTOK], BF16)
        self.gt_d = dint("gt_d", [32, NTOK])
        self.u_d = dint("u_d", [128, 8 * NTOK])
        self.gg_d = dint("gg_d", [128, 8 * T_LAT])
        self.z_d = dint("z_d", [128, 8 * T_LAT], BF16)

    def zero_step(self, zt, n):
        if not self.sparse:
            return
        z = getattr(self, "_zrow", 0)
        for _ in range(n):
            if z < 66 * 512:
                self.T.dma(self.xs_d[z:z + 512, :].rearrange("(p r) c -> p (r c)", p=128), zt[:], reads=["zt"])
                z += 512
        self._zrow = z

    def un(self, name):
        self._uid = getattr(self, "_uid", 0) + 1
        return "%s_u%d" % (name, self._uid)

    def v3(self, ap2, c):
        return ap2.rearrange("p (c t) -> p c t", c=c)

    def tt(self, e, out, in0, in1, op, reads, writes):
        self.T.op(e, lambda g: g.tensor_tensor(out=out, in0=in0, in1=in1, op=op), reads, writes)

    def ts(self, e, out, in0, s1, s2, op0, op1, reads, writes):
        if s2 is None:
            self.T.op(e, lambda g: g.tensor_scalar(out=out, in0=in0, scalar1=s1, scalar2=None, op0=op0), reads, writes)
        else:
            self.T.op(e, lambda g: g.tensor_scalar(out=out, in0=in0, scalar1=s1, scalar2=s2, op0=op0, op1=op1), reads, writes)

    def stt(self, out, in0, scalar, in1, op0, op1, reads, writes):
        self.T.op("dve", lambda g: g.scalar_tensor_tensor(out=out, in0=in0, scalar=scalar, in1=in1, op0=op0, op1=op1), reads, writes)

    def actf(self, out, in_, func, reads, writes, bias=None, scale=None):
        kw = {}
        if bias is not None:
            kw["bias"] = bias
        if scale is not None:
            kw["scale"] = scale
        self.T.op("act", lambda g: g.activation(out=out, in_=in_, func=func, **kw), reads, writes)

    def mm(self, out, lhsT, rhs, start, stop, reads=(), writes=(), signal=True):
        self.T.op("pe", lambda g: g.matmul(out, lhsT=lhsT, rhs=rhs, start=start, stop=stop), reads, writes, signal=signal)

    def mm_group(self, out, pairs, reads, writes):
        n = len(pairs)
        for i, (l, r) in enumerate(pairs):
            sig = (i == 0) or (i == n - 1)
            self.mm(out, l, r, i == 0, i == n - 1, reads if sig else (), writes if sig else (), signal=sig)

    def tab(self, l, who, n, kind):
        i = ((l * 2 + who) * 2 + n) * 3 + kind
        return i * 8

    def build(self):
        nc = self.nc
        with ExitStack() as es:
            self.T = Tr(nc, es)
            self.tabt = es.enter_context(nc.sbuf_tensor("tabt", [128, 24 * 8], F32))
            self.ones = es.enter_context(nc.sbuf_tensor("ones", [128, 128], F32))
            self.ident = es.enter_context(nc.sbuf_tensor("ident", [128, 128], F32))
            self.gains_t = es.enter_context(nc.sbuf_tensor("gains_t", [128, 40], F32))
            self.epsc = es.enter_context(nc.sbuf_tensor("epsc", [128, 1], F32))
            with ExitStack() as es0:
                gen = self.phase_pool(es0)
                left = [9]

                def hook():
                    if left[0] > 0:
                        left[0] -= 1
                        next(gen, None)
                self.phase_adaln(es0, hook=hook)
                for _ in gen:
                    pass
                self.T.flush()
            if self.stop_after >= 2:
                if self.sparse:
                    self.phase_moe_sparse(0)
                else:
                    self.phase_norm_router(0)
                    self.T.flush()
                    self.phase_moe(0)
                self.T.flush()
            if self.stop_after >= 3:
                self.phase_lru()
                self.T.flush()
            if self.stop_after >= 4:
                if self.sparse:
                    self.phase_moe_sparse(1)
                else:
                    self.phase_norm_router(1)
                    self.T.flush()
                    self.phase_moe(1)
                self.T.flush()
            if not (self.sparse and self.stop_after >= 5):
                self.phase_final(self.stop_after >= 5)
            self.T.flush()
        return nc

    def phase_adaln(self, es, hook=None):
        nc, T = self.nc, self.T
        if True:
            sb = lambda name, shape, dt=F32: es.enter_context(nc.sbuf_tensor(self.un(name), shape, dt))
            cs_t = sb("cs_t", [128, 16]); sv = sb("sv", [128, 16])
            adab = sb("adab", [128, 96]); psc = sb("psc", [128, 8])
            modt = sb("modt", [128, 2 * 2 * 48])
            stg = [sb("a_stg%d" % i, [128, 4096]) for i in range(2)]
            tmp8 = sb("tmp8", [128, 8])
            psm = es.enter_context(nc.psum_tensor(self.un("psm"), [128, 192], F32))
            T.dma(cs_t[:], self.cs[:, :], writes=["cs_t"])
            T.dma(adab[:], self.ada_bp[:, :], writes=["adab"])
            T.dma(psc[:], self.pscale[:, :], writes=["psc"])
            T.dma(self.gains_t[:], self.gains[:, :], writes=["gains"])
            T.dma(self.ident[:], self.identd[:, :], writes=["ident"])
            T.op("pool", lambda g: g.memset(self.ones[:], 1.0), writes=["ones"])
            T.op("pool", lambda g: g.memset(self.epsc[:], EPS), writes=["epsc"])
            self.actf(sv[:], cs_t[:], AF.Silu, ["cs_t"], ["sv"])
            sv2 = sv[:].rearrange("p (two k) -> p k two", two=2)
            for l in range(2):
                for j in range(12):
                    s = stg[j % 2]
                    key = "a_stg%d" % (j % 2)
                    r0 = (l * 12 + j) * 128
                    T.dma(s[:], self.ada_wp[r0:r0 + 128, :], writes=[key])
                    for m in range(4):
                        jj = j * 4 + m
                        col = l * 96 + jj * 2
                        pairs = [(s[:, kc * 512 + m * 128: kc * 512 + (m + 1) * 128], sv2[:, kc, :]) for kc in range(8)]
                        self.mm_group(psm[:, col:col + 2], pairs, [key, "sv"], ["psm"])
                    if hook is not None and j % 2 == 1:
                        hook()
            for l in range(2):
                for who in range(2):
                    src = psm[:, l * 96:(l + 1) * 96].rearrange("p (j t) -> p j t", t=2)[:, :, who]
                    o = (l * 2 + who) * 48
                    self.tt("dve", modt[:, o:o + 48], src, adab[:, l * 48:(l + 1) * 48], ALU.add, ["psm", "adab"], ["modt"])
            for l in range(2):
                for who in range(2):
                    o = (l * 2 + who) * 48
                    for n in range(2):
                        gcol = (2 * l + n) * 8
                        a = self.tab(l, who, n, 0); b = self.tab(l, who, n, 1); g = self.tab(l, who, n, 2)
                        sc = modt[:, o + (1 + 3 * n) * 8: o + (2 + 3 * n) * 8]
                        sh = modt[:, o + (3 * n) * 8: o + (3 * n + 1) * 8]
                        gg = modt[:, o + (2 + 3 * n) * 8: o + (3 + 3 * n) * 8]
                        self.ts("dve", tmp8[:], sc, 1.0, None, ALU.add, None, ["modt"], ["tmp8"])
                        self.tt("dve", self.tabt[:, a:a + 8], tmp8[:], self.gains_t[:, gcol:gcol + 8], ALU.mult, ["tmp8", "gains"], ["tab"])
                        self.T.op("dve", lambda e, b=b, sh=sh: e.tensor_copy(out=self.tabt[:, b:b + 8], in_=sh), ["modt"], ["tab"])
                        if l == 0 and n == 0:
                            self.tt("dve", self.tabt[:, g:g + 8], gg, psc[:], ALU.mult, ["modt", "psc"], ["tab"])
                        else:
                            self.T.op("dve", lambda e, g=g, gg=gg: e.tensor_copy(out=self.tabt[:, g:g + 8], in_=gg), ["modt"], ["tab"])

    def rstd(self, src2, n, dst, rkeys, wkeys, sq, sqkey, ps, pskey, tmp, tmpkey):
        self.actf(sq[:, 0:8 * n], src2, AF.Square, rkeys, [sqkey])
        pairs = [(self.ones[:], sq[:, c * n:(c + 1) * n]) for c in range(8)]
        self.mm_group(ps[:, 0:n], pairs, [sqkey, "ones"], [pskey])
        self.actf(tmp[:, 0:n], ps[:, 0:n], AF.Sqrt, [pskey, "epsc"], [tmpkey], bias=self.epsc[:, 0:1], scale=1.0 / 1024.0)
        self.T.op("dve", lambda e: e.reciprocal(out=dst, in_=tmp[:, 0:n]), [tmpkey], wkeys)

    def phase_pool(self, es):
        nc, T = self.nc, self.T
        if True:
            sb = lambda name, shape, dt=F32: es.enter_context(nc.sbuf_tensor(self.un(name), shape, dt))
            rs_l = sb("rs_l", [128, T_LAT]); rs_c = sb("rs_c", [128, T_CTX])
            Lg = [sb("Lg%d" % i, [128, 4096]) for i in range(2)]
            tmp = sb("tmpr", [128, 512])
            Hc = sb("Hc", [128, 4096])
            sq = Hc
            ztp = sb("ztp", [128, 4096], BF16)
            T.op("pool", lambda e: e.memset(ztp[:], 0.0), writes=["zt"])
            P = sb("Pp", [128, 6400]); Q = sb("Qp", [128, 6400])
            d16 = [sb("d16_%d" % i, [128, 4096], BF16) for i in range(2)]
            invc = sb("invc", [128, 4096])
            pw32 = sb("pw32", [128, 2048]); pw16 = sb("pw16", [128, 2048], BF16)
            ps = [es.enter_context(nc.psum_tensor(self.un("pps%d" % i), [128, 512], F32)) for i in range(2)]
            T.dma(pw32[:], self.poolw[:, :], writes=["pw32"])
            self.actf(pw16[:], pw32[:], AF.Copy, ["pw32"], ["pw16"])
            for g in range(8):
                L = Lg[g % 2]; key = "Lg%d" % (g % 2)
                T.dma(self.v3(L[:, :], 8), self.v3(self.xT, 8)[:, :, g * 512:(g + 1) * 512], writes=[key])
                self.rstd(L[:, :], 512, rs_l[:, g * 512:(g + 1) * 512], [key], ["rs_l"], sq, "Hc", ps[g % 2], "pps%d" % (g % 2), tmp, "tmpr")
                yield
            L = Lg[0]
            T.dma(self.v3(L[:, 0:2048], 8), self.v3(self.ctxT, 8), writes=["Lg0"])
            self.rstd(L[:, 0:2048], 256, rs_c[:, :], ["Lg0"], ["rs_c"], sq, "Hc", ps[0], "pps0", tmp, "tmpr")
            yield
            for who in range(2):
                ntok = T_LAT if who == 0 else T_CTX
                R, Cw = (64, 64) if who == 0 else (1, 256)
                Ra, N = (R + 16, Cw + 16) if who == 0 else (1, Cw + 16)
                r_lo = 8 if who == 0 else 0
                src_d = self.xT if who == 0 else self.ctxT
                dst_d = self.lat_d if who == 0 else self.ctx_d
                rs = rs_l if who == 0 else rs_c
                invd = self.invc2 if who == 0 else self.invc1
                a0 = self.tab(0, who, 0, 0); b0 = self.tab(0, who, 0, 1); g0 = self.tab(0, who, 0, 2)
                Pv = P[:, 0:Ra * N].rearrange("p (r c) -> p r c", c=N)
                Qv = Q[:, 0:Ra * N].rearrange("p (r c) -> p r c", c=N)
                Pint = Pv[:, r_lo:r_lo + R, 8:8 + Cw]
                Qint = Qv[:, r_lo:r_lo + R, 8:8 + Cw]
                for gi in range(4):
                    k = gi + 1
                    T.dma(invc[:, 0:ntok], invd[gi:gi + 1, :].partition_broadcast(128), writes=["invc"])
                    for ci in range(2):
                        c = 2 * gi + ci
                        L = Lg[ci]; key = "Lg%d" % ci
                        T.dma(L[:, 0:ntok], src_d[:, c * ntok:(c + 1) * ntok], writes=[key])
                        self.tt("dve", Hc[:, 0:ntok], L[:, 0:ntok], rs[:, 0:ntok], ALU.mult, [key, "rs_l" if who == 0 else "rs_c"], ["Hc"])
                        self.ts("dve", Hc[:, 0:ntok], Hc[:, 0:ntok], self.tabt[:, a0 + c:a0 + c + 1], self.tabt[:, b0 + c:b0 + c + 1], ALU.mult, ALU.add, ["Hc", "tab"], ["Hc"])
                        T.op("pool", lambda e, ap=P[:, 0:Ra * N]: e.memset(ap, 0.0), writes=["P"])
                        self.actf(Pint, Hc[:, 0:ntok].rearrange("p (r c) -> p r c", c=Cw), AF.Copy, ["Hc"], ["P"])
                        bufs = [(Pv, "P"), (Qv, "Q")]
                        step = 0
                        passes = [2] if who == 1 else [2, 1]
                        for axis in passes:
                            Nn = N if axis == 2 else Ra
                            for s in range(1, k + 1):
                                (sv_, sk), (dv_, dk) = bufs[step % 2], bufs[(step + 1) % 2]
                                lo = [1, 2, 4, 8][s - 1]; hi = Nn - [0, 1, 3, 7][s - 1]
                                if s == 1:
                                    a_lo, a_hi, b_lo, b_hi = lo - 1, hi - 1, lo, hi
                                else:
                                    sh = 2 ** (s - 2)
                                    a_lo, a_hi, b_lo, b_hi = lo - sh, hi - sh, lo + sh, hi + sh
                                if axis == 2:
                                    o_ = dv_[:, :, lo:hi]; i0 = sv_[:, :, a_lo:a_hi]; i1 = sv_[:, :, b_lo:b_hi]
                                else:
                                    o_ = dv_[:, lo:hi, 8:8 + Cw]; i0 = sv_[:, a_lo:a_hi, 8:8 + Cw]; i1 = sv_[:, b_lo:b_hi, 8:8 + Cw]
                                self.tt("dve", o_, i0, i1, ALU.add, [sk], [dk])
                                step += 1
                        res_int, res_key, oth_int, oth_key = (Pint, "P", Qint, "Q") if step % 2 == 0 else (Qint, "Q", Pint, "P")
                        self.tt("dve", oth_int, res_int, invc[:, 0:ntok].rearrange("p (r c) -> p r c", c=Cw), ALU.mult, [res_key, "invc"], [oth_key])
                        self.tt("dve", d16[ci][:, 0:ntok].rearrange("p (r c) -> p r c", c=Cw), oth_int, Hc[:, 0:ntok].rearrange("p (r c) -> p r c", c=Cw), ALU.subtract, [oth_key, "Hc"], ["d16_%d" % ci])
                    ngrp = 8 if who == 0 else 1
                    gsz = 512 if who == 0 else 256
                    for oc in range(2):
                        c = 2 * gi + oc
                        for g in range(ngrp):
                            u = oc * ngrp + g
                            pst = ps[u % 2]; pk = "pps%d" % (u % 2)
                            pairs = [(pw16[:, (gi * 2 + kc) * 256 + oc * 128:(gi * 2 + kc) * 256 + (oc + 1) * 128],
                                      d16[kc][:, g * gsz:(g + 1) * gsz]) for kc in range(2)]
                            self.mm_group(pst[:, 0:gsz], pairs, ["pw16", "d16_0", "d16_1"], [pk])
                            self.stt(Lg[oc][:, g * gsz:(g + 1) * gsz], pst[:, 0:gsz], self.tabt[:, g0 + c:g0 + c + 1],
                                     Lg[oc][:, g * gsz:(g + 1) * gsz], ALU.mult, ALU.add, [pk, "Lg%d" % oc, "tab"], ["Lg%d" % oc])
                        T.dma(dst_d[:, c * ntok:(c + 1) * ntok], Lg[oc][:, 0:ntok], reads=["Lg%d" % oc])
                        self.zero_step(ztp, 4)

    def groups(self, l):
        gs = [("lat", g * 512, 512, g * 512) for g in range(8)]
        if l == 0:
            gs.append(("ctx", 0, 256, T_LAT))
        return gs

    def phase_norm_router(self, l, pers=None):
        nc, T = self.nc, self.T
        with ExitStack() as es:
            sb = lambda name, shape, dt=F32: es.enter_context(nc.sbuf_tensor(self.un(name), shape, dt))
            Lg = [sb("nLg%d" % i, [128, 4096]) for i in range(2)]
            sq = sb("nsq", [128, 4096]); tmp = sb("ntmp", [128, 512]); rs = sb("nrs", [128, 512])
            h32 = [sb("h32_%d" % i, [128, 4096]) for i in range(2)]
            h16 = [sb("h16_%d" % i, [128, 4096], BF16) for i in range(2)]
            rw = sb("rw", [128, 256]); rb = sb("rb", [128, 32])
            lg = sb("lg", [128, 32]); top8 = sb("top8", [128, 8]); nmx = sb("nmx", [128, 1])
            msk = sb("msk", [128, 32]); ex = sb("ex", [128, 32]); ssum = sb("ssum", [128, 1]); rsum = sb("rsum", [128, 1])
            G = sb("G", [128, 32]); GT = [sb("GT%d" % i, [32, 512]) for i in range(2)]
            psr = es.enter_context(nc.psum_tensor(self.un("psr"), [128, 512], F32))
            psl = [es.enter_context(nc.psum_tensor(self.un("psl%d" % i), [128, 128], F32)) for i in range(2)]
            pst = [es.enter_context(nc.psum_tensor(self.un("pst%d" % i), [32, 512], F32)) for i in range(2)]
            rb4 = sb("rb4", [128, 128]); msk4 = sb("msk4", [128, 128]); ex4 = sb("ex4", [128, 128]); ssum4 = sb("ssum4", [128, 4]); rsum4 = sb("rsum4", [128, 4])
            if pers is None:
                NTT_ = sum(n_ for (_, _, n_, _) in self.groups(l)) // 128
                pers = {"lg": sb("q_lg", [128, NTT_ * 32]), "top8": sb("q_top8", [128, NTT_ * 8]),
                        "G": sb("q_G", [128, NTT_ * 32]), "M": sb("q_M", [128, NTT_ * 32], BF16)}
            zt = None
            zrow = [0]
            if l == 0 and self.sparse:
                zt = sb("zt", [128, 4096], BF16)
                T.op("pool", lambda e: e.memset(zt[:], 0.0), writes=["zt"])
            T.dma(rw[:], self.router_wp[:, l * 256:(l + 1) * 256], writes=["rw"])
            for t_ in range(4):
                T.dma(rb4[:, t_ * 32:(t_ + 1) * 32], self.router_bp[l:l + 1, :].partition_broadcast(128), writes=["rb4"])
            tcount = 0
            for gidx, (kind, off, n, hoff) in enumerate(self.groups(l)):
                who = 0 if kind == "lat" else 1
                src_d = self.lat_d if who == 0 else self.ctx_d
                ntok = T_LAT if who == 0 else T_CTX
                L = Lg[gidx % 2]; key = "nLg%d" % (gidx % 2)
                T.dma(self.v3(L[:, 0:8 * n], 8), self.v3(src_d, 8)[:, :, off:off + n], writes=[key])
                self.rstd(L[:, 0:8 * n], n, rs[:, 0:n], [key], ["nrs"], sq, "nsq", psr, "psr", tmp, "ntmp")
                a = self.tab(l, who, 1, 0); b = self.tab(l, who, 1, 1)
                H = h32[gidx % 2]; hk = "h32_%d" % (gidx % 2)
                H16 = h16[gidx % 2]; hk16 = "h16_%d" % (gidx % 2)
                for c in range(8):
                    self.tt("dve", H[:, c * n:(c + 1) * n], L[:, c * n:(c + 1) * n], rs[:, 0:n], ALU.mult, [key, "nrs"], [hk])
                    self.ts("dve", H[:, c * n:(c + 1) * n], H[:, c * n:(c + 1) * n], self.tabt[:, a + c:a + c + 1], self.tabt[:, b + c:b + c + 1], ALU.mult, ALU.add, [hk], [hk])
                self.actf(H16[:, 0:8 * n], H[:, 0:8 * n], AF.Copy, [hk], [hk16])
                T.dma(self.v3(self.h_d, 8)[:, :, hoff:hoff + n], self.v3(H16[:, 0:8 * n], 8), reads=[hk16])
                gt = GT[gidx % 2]; gk = "GT%d" % (gidx % 2)
                nt4 = n // 128
                tti0 = hoff // 128
                pl_ = psl[gidx % 2]; plk = "psl%d" % (gidx % 2)
                ptt = pst[gidx % 2]; ptk = "pst%d" % (gidx % 2)
                for t in range(nt4):
                    pairs = [(H[:, c * n + t * 128:c * n + (t + 1) * 128], rw[:, c * 32:(c + 1) * 32]) for c in range(8)]
                    self.mm_group(pl_[:, t * 32:(t + 1) * 32], pairs, [hk, "rw"], [plk])
                W4 = nt4 * 32
                lg4 = pers["lg"][:, tti0 * 32: tti0 * 32 + W4]
                G4 = pers["G"][:, tti0 * 32: tti0 * 32 + W4]
                M4 = pers["M"][:, tti0 * 32: tti0 * 32 + W4]
                t8 = pers["top8"]

                def v4(ap2):
                    return ap2.rearrange("p (t e) -> p t e", e=32)

                def bc(ap2, col0, tstride):
                    pstp = ap2.ap[0][0]
                    return bass.AP(ap2.tensor, ap2.offset + col0, [[pstp, 128], [tstride, nt4], [0, 32]])
                self.tt("dve", lg4, pl_[:, 0:W4], rb4[:, 0:W4], ALU.add, [plk, "rb4"], ["lg"])
                for t in range(nt4):
                    T.op("dve", lambda e, t=t, tti0=tti0: e.max(out=t8[:, (tti0 + t) * 8:(tti0 + t + 1) * 8], in_=pers["lg"][:, (tti0 + t) * 32:(tti0 + t + 1) * 32]), ["lg"], ["top8"])
                t8b = t8[:, :]
                self.tt("dve", v4(msk4[:, 0:W4]), v4(lg4), bc(t8b, tti0 * 8 + 3, 8), ALU.is_ge, ["lg", "top8"], ["msk"])
                T.op("dve", lambda e, M4=M4, W4=W4: e.tensor_copy(out=M4, in_=msk4[:, 0:W4]), ["msk"], ["Mall"])
                self.tt("dve", v4(ex4[:, 0:W4]), v4(lg4), bc(t8b, tti0 * 8 + 0, 8), ALU.subtract, ["lg", "top8"], ["ex"])
                self.actf(ex4[:, 0:W4], ex4[:, 0:W4], AF.Exp, ["ex"], ["ex"])
                self.tt("dve", ex4[:, 0:W4], ex4[:, 0:W4], msk4[:, 0:W4], ALU.mult, ["ex", "msk"], ["ex"])
                T.op("dve", lambda e, W4=W4, nt4=nt4: e.reduce_sum(out=ssum4[:, 0:nt4], in_=v4(ex4[:, 0:W4]), axis=mybir.AxisListType.X), ["ex"], ["ssum"])
                T.op("dve", lambda e, nt4=nt4: e.reciprocal(out=rsum4[:, 0:nt4], in_=ssum4[:, 0:nt4]), ["ssum"], ["rsum"])
                self.tt("dve", v4(G4), v4(ex4[:, 0:W4]), bc(rsum4[:, :], 0, 1), ALU.mult, ["ex", "rsum"], ["G"])
                for t in range(nt4):
                    sig = t in (0, nt4 - 1)
                    T.op("pe", lambda e, t=t, ptt=ptt, tti0=tti0: e.transpose(out=ptt[:, t * 128:(t + 1) * 128], in_=pers["G"][:, (tti0 + t) * 32:(tti0 + t + 1) * 32], identity=self.ident[:]),
                         ["G", "ident"] if sig else (), [ptk] if sig else (), signal=sig)
                self.actf(gt[:, 0:n], ptt[:, 0:n], AF.Copy, [ptk], [gk])
                T.dma(self.gt_d[:, hoff:hoff + n], gt[:, 0:n], reads=[gk])
                if zt is not None:
                    self.zero_step(zt, 4 if gidx < 8 else 66)
            T.flush()

    def phase_moe(self, l):
        nc, T = self.nc, self.T
        gs = self.groups(l)
        blocks = [gs[0:3], gs[3:6], gs[6:]]
        TB = 1536
        with ExitStack() as es:
            sb = lambda name, shape, dt=F32: es.enter_context(nc.sbuf_tensor(self.un(name), shape, dt))
            Lb = sb("Lb", [128, 8 * TB]); Hb = sb("Hb", [128, 8 * TB], BF16); Ab = sb("Ab", [128, 8 * TB], BF16)
            stg = [sb("stg%d" % i, [128, 4096]) for i in range(2)]
            NW = 4
            w16 = [sb("w16_%d" % i, [128, 4096], BF16) for i in range(NW)]
            tsg = [sb("tsg%d" % i, [128, 512]) for i in range(2)]
            tr1 = [sb("tr1%d" % i, [128, 512]) for i in range(2)]
            tr2 = [sb("tr2%d" % i, [128, 512]) for i in range(2)]
            tq = [sb("tq%d" % i, [128, 512]) for i in range(2)]
            g2n = sb("g2n", [128, 16])
            Gb = sb("Gb", [128, TB]); GTs = sb("GTs", [32, TB])
            bg = sb("bg", [128, 512]); bgs = sb("bgs", [128, 512]); bd = sb("bd", [32, 1024])
            psg = [es.enter_context(nc.psum_tensor(self.un("psg%d" % i), [128, 512], F32)) for i in range(2)]
            psu = [es.enter_context(nc.psum_tensor(self.un("psu%d" % i), [128, 512], F32)) for i in range(2)]
            psy = [es.enter_context(nc.psum_tensor(self.un("psy%d" % i), [128, 512], F32)) for i in range(2)]
            T.dma(bg[:], self.bgu[:, l * 512:(l + 1) * 512], writes=["bg"])
            T.dma(bd[:], self.bdn[l * 32:(l + 1) * 32, :], writes=["bd"])
            for who in range(2):
                g2c = self.tab(l, who, 1, 2)
                self.ts("dve", g2n[:, who * 8:(who + 1) * 8], self.tabt[:, g2c:g2c + 8], -1.0 / 1.702, None, ALU.mult, None, ["tab"], ["g2n"])
            bg3 = bg[:].rearrange("p (e j) -> p e j", j=16)
            bgs3 = bgs[:].rearrange("p (e j) -> p e j", j=16)
            self.ts("dve", bgs3[:, :, 0:8], bg3[:, :, 0:8], 1.702, None, ALU.mult, None, ["bg"], ["bgs"])
            self.ts("dve", bgs3[:, :, 8:16], bg3[:, :, 8:16], 7.0, None, ALU.add, None, ["bg"], ["bgs"])
            q_issue = [0]
            ucount = [0, 0]
            pend = [None]
            C1 = 11.914 / (1.0 + float(np.exp(-11.914)))
            c14 = sb("c14", [128, 1])
            T.op("pool", lambda e: e.memset(c14[:], 14.0), writes=["c14"])
            for bi, blk in enumerate(blocks):
                offs = []
                o = 0
                for (kind, off, n, hoff) in blk:
                    offs.append(o)
                    o += n
                nb = o
                jobs = []
                for e in range(self.nexp):
                    for p in range(6):
                        jobs.append((e, p))

                def issue(idx):
                    e, p = jobs[idx]
                    q = q_issue[0]
                    q_issue[0] += 1
                    s = stg[q % 2]; sk = "stg%d" % (q % 2)
                    w = w16[q % NW]; wk = "w16_%d" % (q % NW)
                    r0 = ((l * 32 + e) * 6 + p) * 128
                    T.dma(s[:], self.wexp[r0:r0 + 128, :], writes=[sk])
                    self.actf(w[:], s[:], AF.Copy, [sk], [wk])
                    return (w, wk)
                issued = {}
                nxt = 0
                for _ in range(min(4, len(jobs))):
                    issued[nxt] = issue(nxt)
                    nxt += 1
                for gi, (kind, off, n, hoff) in enumerate(blk):
                    src_d = self.lat_d if kind == "lat" else self.ctx_d
                    bo = offs[gi]
                    T.dma(self.v3(Lb[:, :], 8)[:, :, bo:bo + n], self.v3(src_d, 8)[:, :, off:off + n], writes=[("Lb", gi)])
                    T.dma(self.v3(Hb[:, :], 8)[:, :, bo:bo + n], self.v3(self.h_d, 8)[:, :, hoff:hoff + n], writes=["Hb"])
                    T.dma(GTs[:, bo:bo + n], self.gt_d[:, hoff:hoff + n], writes=["GTs"])
                for gi, (kind, off, n, hoff) in enumerate(blk):
                    who = 0 if kind == "lat" else 1
                    g2 = self.tab(l, who, 1, 2)
                    bo = offs[gi]
                    for dc in range(8):
                        par = ucount[1] % 2; ucount[1] += 1
                        self.mm(psy[par][:, 0:n], bd[:, dc * 128:(dc + 1) * 128], GTs[:, bo:bo + n], True, True, ["bd", "GTs"], ["psy%d" % par])
                        lsl = Lb[:, dc * TB + bo: dc * TB + bo + n]
                        self.stt(lsl, psy[par][:, 0:n], self.tabt[:, g2 + dc:g2 + dc + 1], lsl, ALU.mult, ALU.add, ["psy%d" % par, ("Lb", gi)], [("Lb", gi)])
                for e in range(self.nexp):
                    for gi, (kind, off, n, hoff) in enumerate(blk):
                        bo = offs[gi]
                        T.dma(Gb[:, bo:bo + n], self.gt_d[e:e + 1, hoff:hoff + n].partition_broadcast(128), writes=["Gb"])
                    base = e * 6
                    for half in range(2):
                        (wg, wgk) = issued.pop(base + 2 * half)
                        (wu, wuk) = issued.pop(base + 2 * half + 1)
                        for fl in range(4):
                            f = half * 4 + fl
                            for gi, (kind, off, n, hoff) in enumerate(blk):
                                bo = offs[gi]
                                par = ucount[0] % 2; ucount[0] += 1
                                pg, pu = psg[par], psu[par]
                                pgk, puk = "psg%d" % par, "psu%d" % par
                                pairs = [(wg[:, kc * 512 + fl * 128: kc * 512 + (fl + 1) * 128], Hb[:, kc * TB + bo: kc * TB + bo + n]) for kc in range(8)]
                                self.mm_group(pg[:, 0:n], pairs, [wgk, "Hb"], [pgk])
                                pairs = [(wu[:, kc * 512 + fl * 128: kc * 512 + (fl + 1) * 128], Hb[:, kc * TB + bo: kc * TB + bo + n]) for kc in range(8)]
                                self.mm_group(pu[:, 0:n], pairs, [wuk, "Hb"], [puk])
                                ks = "t%d" % par
                                self.actf(tr1[par][:, 0:n], pu[:, 0:n], AF.Relu, [puk, "bgs"], [ks + "r1"], bias=bgs[:, e * 16 + 8 + f:e * 16 + 8 + f + 1], scale=1.0)
                                self.actf(tsg[par][:, 0:n], pg[:, 0:n], AF.Silu, [pgk, "bgs"], [ks + "s"], bias=bgs[:, e * 16 + f:e * 16 + f + 1], scale=1.702)
                                self.actf(tr2[par][:, 0:n], tr1[par][:, 0:n], AF.Relu, [ks + "r1", "c14"], [ks + "r2"], bias=c14[:, 0:1], scale=-1.0)
                                self.stt(tq[par][:, 0:n], tsg[par][:, 0:n], C1, Gb[:, bo:bo + n], ALU.min, ALU.mult, [ks + "s", "Gb"], [ks + "q"])
                                if pend[0] is not None:
                                    pend[0]()
                                def d2(par=par, n=n, f=f, bo=bo, gi=gi, ks=ks):
                                    self.stt(Ab[:, f * TB + bo: f * TB + bo + n], tr2[par][:, 0:n], 8.0, tq[par][:, 0:n], ALU.subtract, ALU.mult, [ks + "r2", ks + "q"], [("Ab", gi)])
                                pend[0] = d2
                        if pend[0] is not None:
                            pend[0]()
                            pend[0] = None
                        for _ in range(2):
                            if nxt < len(jobs):
                                issued[nxt] = issue(nxt)
                                nxt += 1
                    for half in range(2):
                        (wd, wdk) = issued.pop(base + 4 + half)
                        for dl in range(4):
                            dc = half * 4 + dl
                            for gi, (kind, off, n, hoff) in enumerate(blk):
                                who = 0 if kind == "lat" else 1
                                g2 = self.tab(l, who, 1, 2)
                                bo = offs[gi]
                                par = ucount[1] % 2; ucount[1] += 1
                                py = psy[par]; pyk = "psy%d" % par
                                pairs = [(wd[:, fc * 512 + dl * 128: fc * 512 + (dl + 1) * 128], Ab[:, fc * TB + bo: fc * TB + bo + n]) for fc in range(8)]
                                self.mm_group(py[:, 0:n], pairs, [wdk, ("Ab", gi)], [pyk])
                                lsl = Lb[:, dc * TB + bo: dc * TB + bo + n]
                                self.stt(lsl, py[:, 0:n], g2n[:, who * 8 + dc:who * 8 + dc + 1], lsl, ALU.mult, ALU.add, [pyk, ("Lb", gi), "g2n"], [("Lb", gi)])
                        if nxt < len(jobs):
                            issued[nxt] = issue(nxt)
                            nxt += 1
                for gi, (kind, off, n, hoff) in enumerate(blk):
                    dst_d = self.lat_d if kind == "lat" else self.ctx_d
                    bo = offs[gi]
                    T.dma(self.v3(dst_d, 8)[:, :, off:off + n], self.v3(Lb[:, :], 8)[:, :, bo:bo + n], reads=[("Lb", gi)])
            T.flush()


    def phase_moe_sparse(self, l):
        nc, T = self.nc, self.T
        I32 = mybir.dt.int32
        X = mybir.AxisListType.X
        gs = self.groups(l)
        NTT = sum(n for (_, _, n, _) in gs) // 128
        NT = (4 * NTT * 128 + 32 * 511) // 512
        TS = 512
        with ExitStack() as pes:
            psb_ = lambda name, shape, dt=F32: pes.enter_context(nc.sbuf_tensor(self.un(name), shape, dt))
            pers = {"lg": psb_("p_lg", [128, NTT * 32]), "top8": psb_("p_top8", [128, NTT * 8]),
                    "G": psb_("p_G", [128, NTT * 32]), "M": psb_("p_M", [128, NTT * 32], BF16)}
            dest_i = psb_("dest_i", [128, NTT * 4], I32)
            gk = psb_("gk", [128, NTT * 4])
            widx_i = psb_("widx_i", [128, NT * 12], I32)
            bidx_i = psb_("bidx_i", [128, NT], I32)
            identb = psb_("identb", [128, 128], BF16)
            self.phase_norm_router(l, pers)
            T.flush()
            with ExitStack() as es:
                sb = lambda name, shape, dt=F32: es.enter_context(nc.sbuf_tensor(self.un(name), shape, dt))
                onesb = sb("onesb", [128, 128], BF16); utri = sb("utri", [128, 128], BF16)
                iotap = sb("iotap", [128, 1]); jpos = sb("jpos", [128, 66]); one32 = sb("one32", [128, 32])
                cnt = sb("cnt", [128, 32]); ntl = sb("ntl", [128, 32]); pc = sb("pc", [128, 32]); pend = sb("pend", [128, 32]); pstart = sb("pstart", [128, 32])
                cmp3 = sb("cmp3", [128, 66 * 32]); ej = sb("ej", [128, 66]); wix = sb("wix", [128, 66]); wix6 = sb("wix6", [128, 66 * 6]); bix = sb("bix", [128, 66])
                Dt = sb("Dt", [128, 32]); oh = sb("oh", [128, 32]); pr = sb("pr", [128, 32]); destf = sb("destf", [128, NTT * 4]); gkf = sb("gkf", [128, NTT * 4])
                ps_c = es.enter_context(nc.psum_tensor(self.un("ps_c"), [128, 32], F32))
                ps_r = [es.enter_context(nc.psum_tensor(self.un("ps_r%d" % i), [128, 32], F32)) for i in range(2)]
                T.dma(utri[:], self.utrid[:, :], writes=["utri"])
                T.dma(identb[:], self.identbd[:, :], writes=["identb"])
                T.dma(iotap[:], self.iotapd[:, :], writes=["iotap"])
                T.dma(jpos[:], self.jposd[0:1, :].partition_broadcast(128), writes=["jpos"])
                T.op("pool", lambda e: e.memset(onesb[:], 1.0), writes=["onesb"])
                T.op("pool", lambda e: e.memset(one32[:], 1.0), writes=["one32"])
                M = pers["M"]
                pairs = [(onesb[:], M[:, tt * 32:(tt + 1) * 32]) for tt in range(NTT)]
                self.mm_group(ps_c[:, :], pairs, ["onesb", "Mall"], ["ps_c"])
                T.op("dve", lambda e: e.tensor_copy(out=cnt[:], in_=ps_c[:, :]), ["ps_c"], ["cnt"])
                self.ts("dve", ntl[:], cnt[:], 0.0, None, ALU.is_gt, None, ["cnt"], ["ntl"])
                for m in range(1, 9):
                    self.stt(ntl[:], cnt[:], 512.0 * m, ntl[:], ALU.is_gt, ALU.add, ["cnt", "ntl"], ["ntl"])
                self.ts("dve", pc[:], ntl[:], 512.0, None, ALU.mult, None, ["ntl"], ["pc"])
                T.op("dve", lambda e: e.tensor_tensor_scan(out=pend[:], data0=one32[:], data1=pc[:], initial=0.0, op0=ALU.mult, op1=ALU.add), ["one32", "pc"], ["pend"])
                self.tt("dve", pstart[:], pend[:], pc[:], ALU.subtract, ["pend", "pc"], ["pstart"])
                a0 = pend[:, :]; pstp = a0.ap[0][0]
                in0 = bass.AP(a0.tensor, a0.offset, [[pstp, 128], [0, NT], [1, 32]])
                a1 = jpos[:, :]; pstp1 = a1.ap[0][0]
                in1 = bass.AP(a1.tensor, a1.offset, [[pstp1, 128], [1, NT], [0, 32]])
                c3 = cmp3[:, 0:NT * 32].rearrange("p (j e) -> p j e", e=32)
                T.op("dve", lambda e: e.tensor_tensor(out=c3, in0=in0, in1=in1, op=ALU.is_le), ["pend", "jpos"], ["cmp3"])
                T.op("dve", lambda e: e.reduce_sum(out=ej[:, 0:NT], in_=c3, axis=X), ["cmp3"], ["ej"])
                tailf = sb("tailf", [128, 66])
                self.ts("dve", tailf[:, 0:NT], ej[:, 0:NT], 32.0, float(2 ** 20), ALU.is_ge, ALU.mult, ["ej"], ["tailf"])
                self.ts("dve", ej[:, 0:NT], ej[:, 0:NT], 31.0, 32.0 * l, ALU.min, ALU.add, ["ej"], ["ej"])
                self.ts("dve", wix[:, 0:NT], ej[:, 0:NT], 768.0, None, ALU.mult, None, ["ej"], ["wix"])
                self.tt("dve", wix[:, 0:NT], wix[:, 0:NT], tailf[:, 0:NT], ALU.add, ["wix", "tailf"], ["wix"])
                w6 = wix6[:, 0:NT * 6].rearrange("p (j s) -> p j s", s=6)
                for p_ in range(6):
                    self.ts("dve", w6[:, :, p_], wix[:, 0:NT], iotap[:, 0:1], 128.0 * p_, ALU.add, ALU.add, ["wix", "iotap"], ["wix6"])
                wix12 = sb("wix12", [128, 66 * 12])
                w12 = wix12[:, 0:NT * 12].rearrange("p (q h) -> p q h", h=2)
                for h_ in range(2):
                    self.ts("dve", w12[:, :, h_], wix6[:, 0:NT * 6], 2.0, float(h_), ALU.mult, ALU.add, ["wix6"], ["wix12"])
                T.op("dve", lambda e: e.tensor_copy(out=widx_i[:, :], in_=wix12[:, 0:NT * 12]), ["wix12"], ["widx_i"])
                self.ts("dve", bix[:, 0:NT], ej[:, 0:NT], 128.0, iotap[:, 0:1], ALU.mult, ALU.add, ["ej", "iotap"], ["bix"])
                T.op("dve", lambda e: e.tensor_copy(out=bidx_i[:, :], in_=bix[:, 0:NT]), ["bix"], ["bidx_i"])
                D4 = sb("D4", [128, 128]); oh4 = sb("oh4", [128, 128]); pr4 = sb("pr4", [128, 128])
                ps_r4 = [es.enter_context(nc.psum_tensor(self.un("ps_r4_%d" % i), [128, 128], F32)) for i in range(2)]
                pstart4 = sb("pstart4", [128, 128])
                for t_ in range(4):
                    T.op("dve", lambda e, t_=t_: e.tensor_copy(out=pstart4[:, t_ * 32:(t_ + 1) * 32], in_=pstart[:]), ["pstart"], ["pstart4"])

                def v4(ap2):
                    return ap2.rearrange("p (t e) -> p t e", e=32)
                for b0 in range(0, NTT, 4):
                    nb4 = min(4, NTT - b0)
                    W4 = nb4 * 32
                    pr_ = ps_r4[(b0 // 4) % 2]; prk = "ps_r4_%d" % ((b0 // 4) % 2)
                    for t_ in range(nb4):
                        tt = b0 + t_
                        pairs = [(utri[:], M[:, tt * 32:(tt + 1) * 32])] + [(onesb[:], M[:, t2 * 32:(t2 + 1) * 32]) for t2 in range(tt)]
                        o_ = pr_[:, t_ * 32:(t_ + 1) * 32]
                        if len(pairs) == 1:
                            self.mm(o_, pairs[0][0], pairs[0][1], True, True, ["utri", "Mall", "onesb"], [prk])
                        else:
                            self.mm_group(o_, pairs, ["utri", "Mall", "onesb"], [prk])
                    self.tt("dve", D4[:, 0:W4], pr_[:, 0:W4], pstart4[:, 0:W4], ALU.add, [prk, "pstart4"], ["D4"])
                    lg4 = pers["lg"][:, b0 * 32: b0 * 32 + W4]; G4 = pers["G"][:, b0 * 32: b0 * 32 + W4]
                    t8b = pers["top8"][:, :]
                    for k in range(4):
                        pstp = t8b.ap[0][0]
                        bck = bass.AP(t8b.tensor, t8b.offset + b0 * 8 + k, [[pstp, 128], [8, nb4], [0, 32]])
                        self.tt("dve", v4(oh4[:, 0:W4]), v4(lg4), bck, ALU.is_equal, [], ["oh4"])
                        self.tt("dve", pr4[:, 0:W4], oh4[:, 0:W4], D4[:, 0:W4], ALU.mult, ["oh4", "D4"], ["pr4"])
                        c0 = b0 * 4 + k * nb4
                        T.op("dve", lambda e, c0=c0, nb4=nb4, W4=W4: e.reduce_sum(out=destf[:, c0:c0 + nb4], in_=v4(pr4[:, 0:W4]), axis=X), ["pr4"], ["destf"])
                        self.tt("dve", pr4[:, 0:W4], oh4[:, 0:W4], G4, ALU.mult, ["oh4"], ["pr4"])
                        T.op("dve", lambda e, c0=c0, nb4=nb4, W4=W4: e.reduce_sum(out=gkf[:, c0:c0 + nb4], in_=v4(pr4[:, 0:W4]), axis=X), ["pr4"], ["gkf"])
                T.op("dve", lambda e: e.tensor_copy(out=dest_i[:, :], in_=destf[:, :]), ["destf"], ["dest_i"])
                self.ts("dve", gk[:, :], gkf[:, :], -1.0 / 1.702, None, ALU.mult, None, ["gkf"], ["gk"])
                T.flush()
            def dcol(tt, k):
                b0 = (tt // 4) * 4
                nb4 = min(4, NTT - b0)
                return b0 * 4 + k * nb4 + (tt - b0)
            XW = 1024
            with ExitStack() as es:
                sb = lambda name, shape, dt=F32: es.enter_context(nc.sbuf_tensor(self.un(name), shape, dt))
                hf = [sb("hf%d" % i, [128, 1024], BF16) for i in range(4)]
                XT = [sb("XT_%d" % i, [128, XW], BF16) for i in range(4)]
                psX = [es.enter_context(nc.psum_tensor(self.un("psX%d" % i), [128, 1024], BF16)) for i in range(2)]
                for tt in range(NTT):
                    par = tt % 4
                    pp = tt % 2
                    T.dma(self.v3(hf[par][:, :], 8), self.v3(self.h_d, 8)[:, :, tt * 128:(tt + 1) * 128], writes=["hf%d" % par])
                    for c in range(8):
                        sig = c in (0, 7)
                        T.op("pe", lambda e, c=c, par=par, pp=pp: e.transpose(out=psX[pp][:, c * 128:(c + 1) * 128], in_=hf[par][:, c * 128:(c + 1) * 128], identity=identb[:]),
                             ["hf%d" % par, "identb"] if sig else (), ["psX%d" % pp] if sig else (), signal=sig)
                    if tt % 2 == 0:
                        self.actf(XT[par][:, :], psX[pp][:, :], AF.Copy, ["psX%d" % pp], [("XT", par)])
                    else:
                        T.op("dve", lambda e, par=par, pp=pp: e.tensor_copy(out=XT[par][:, :], in_=psX[pp][:, :]), ["psX%d" % pp], [("XT", par)])
                    for k in range(4):
                        def scat(eng, par=par, dc_=dcol(tt, k)):
                            return eng.indirect_dma_start(out=self.xs_d, out_offset=bass.IndirectOffsetOnAxis(ap=dest_i[:, dc_:dc_ + 1], axis=0),
                                                          in_=XT[par][:, :], in_offset=None)
                        T.dma(None, None, reads=[("XT", par)], writes=["xs_d"], q="pool", fn=scat)
                T.flush()
            with ExitStack() as es:
                sb = lambda name, shape, dt=F32: es.enter_context(nc.sbuf_tensor(self.un(name), shape, dt))
                NW = 8
                w16 = [sb("s_w16_%d" % i, [128, 4096], BF16) for i in range(NW)]
                Xs = [sb("Xs%d" % i, [128, XW], BF16) for i in range(4)]
                Hb = [sb("sHb%d" % i, [128, 8 * TS], BF16) for i in range(2)]
                Ab = [sb("sAb%d" % i, [128, 8 * TS], BF16) for i in range(2)]
                gc = [sb("gc%d" % i, [128, 4]) for i in range(2)]
                bgt = [sb("bgt%d" % i, [128, 16]) for i in range(2)]
                bgst = [sb("bgst%d" % i, [128, 16]) for i in range(2)]
                tsg = [sb("s_tsg%d" % i, [128, 512]) for i in range(2)]
                tr1 = [sb("s_tr1%d" % i, [128, 512]) for i in range(2)]
                tr2 = [sb("s_tr2%d" % i, [128, 512]) for i in range(2)]
                tq = [sb("s_tq%d" % i, [128, 512]) for i in range(2)]
                Ys = [sb("Ys%d" % i, [128, 1024]) for i in range(2)]
                c14 = sb("s_c14", [128, 1])
                psT = [es.enter_context(nc.psum_tensor(self.un("psT%d" % i), [128, 512], BF16)) for i in range(2)]
                psg = [es.enter_context(nc.psum_tensor(self.un("spsg%d" % i), [128, 512], F32)) for i in range(2)]
                psu = [es.enter_context(nc.psum_tensor(self.un("spsu%d" % i), [128, 512], F32)) for i in range(2)]
                psy = [es.enter_context(nc.psum_tensor(self.un("spsy%d" % i), [128, 512], F32)) for i in range(2)]
                T.op("pool", lambda e: e.memset(c14[:], 14.0), writes=["c14"])
                C1 = 11.914 / (1.0 + float(np.exp(-11.914)))
                q_issue = [0]
                jobs = [(j, p) for j in range(NT) for p in range(6)]

                wexp2 = self.wexp.rearrange("r (h c) -> (r h) c", h=2)
                bc_reg = {}

                def issue(idx):
                    j, p = jobs[idx]
                    q = q_issue[0]; q_issue[0] += 1
                    w = w16[q % NW]; wk = "s_w16_%d" % (q % NW)
                    for h_ in range(2):
                        def gat(eng, w=w, j=j, p=p, h_=h_):
                            col = (j * 6 + p) * 2 + h_
                            if "r" not in bc_reg:
                                bc_reg["r"] = eng.to_reg(2 * 2 * 32 * 6 * 128 - 1)
                            return eng.indirect_dma_start(out=w[:, h_ * 2048:(h_ + 1) * 2048], out_offset=None, in_=wexp2,
                                                          in_offset=bass.IndirectOffsetOnAxis(ap=widx_i[:, col:col + 1], axis=0),
                                                          bounds_check=bc_reg["r"], oob_is_err=False)
                        T.dma(None, None, reads=[], writes=[wk], q="pool", fn=gat)
                    return (w, wk)
                issued = {}
                nxt = 0
                for _ in range(NW):
                    issued[nxt] = issue(nxt); nxt += 1
                uc = [0, 0, 0]
                pend = [None]
                def prep_load(j):
                    par = j % 2
                    def bgat(eng, par=par, j=j):
                        return eng.indirect_dma_start(out=bgt[par][:], out_offset=None, in_=self.bgu_e,
                                                      in_offset=bass.IndirectOffsetOnAxis(ap=bidx_i[:, j:j + 1], axis=0))
                    T.dma(None, None, reads=[], writes=["bgt%d" % par], q="pool", fn=bgat)
                    self.ts("dve", bgst[par][:, 0:8], bgt[par][:, 0:8], 1.702, None, ALU.mult, None, ["bgt%d" % par], ["bgst%d" % par])
                    self.ts("dve", bgst[par][:, 8:16], bgt[par][:, 8:16], 7.0, None, ALU.add, None, ["bgt%d" % par], ["bgst%d" % par])
                    for s4 in range(4):
                        xs = Xs[s4]; xk = "Xs%d" % s4
                        r0 = j * TS + s4 * 128
                        T.dma(xs[:], self.xs_d[r0:r0 + 128, :], writes=[xk])

                def prep_tr(j):
                    par = j % 2
                    hb = Hb[par]; hbk = "sHb%d" % par
                    for c in range(8):
                        tp = uc[2] % 2; uc[2] += 1
                        for s4 in range(4):
                            sig = s4 in (0, 3)
                            T.op("pe", lambda e, c=c, s4=s4, tp=tp: e.transpose(out=psT[tp][:, s4 * 128:(s4 + 1) * 128], in_=Xs[s4][:, c * 128:(c + 1) * 128], identity=identb[:]),
                                 ["Xs0", "Xs1", "Xs2", "Xs3", "identb"] if sig else (), ["psT%d" % tp] if sig else (), signal=sig)
                        if c % 2 == 0:
                            self.actf(hb[:, c * TS:(c + 1) * TS], psT[tp][:, :], AF.Copy, ["psT%d" % tp], [hbk])
                        else:
                            T.op("dve", lambda e, c=c, tp=tp, hb=hb: e.tensor_copy(out=hb[:, c * TS:(c + 1) * TS], in_=psT[tp][:, :]), ["psT%d" % tp], [hbk])
                prep_load(0)
                prep_tr(0)
                for j in range(NT):
                    par = j % 2
                    hb = Hb[par]; hbk = "sHb%d" % par
                    ab = Ab[par]; abk = "sAb%d" % par
                    if j + 1 < NT:
                        prep_load(j + 1)
                    base = j * 6
                    for half in range(2):
                        (wg, wgk) = issued.pop(base + 2 * half)
                        (wu, wuk) = issued.pop(base + 2 * half + 1)
                        for fl in range(4):
                            f = half * 4 + fl
                            up = uc[0] % 2; uc[0] += 1
                            pg, pu = psg[up], psu[up]
                            pgk, puk = "spsg%d" % up, "spsu%d" % up
                            pairs = [(wg[:, kc * 512 + fl * 128: kc * 512 + (fl + 1) * 128], hb[:, kc * TS:(kc + 1) * TS]) for kc in range(8)]
                            self.mm_group(pg[:, :], pairs, [wgk, hbk], [pgk])
                            pairs = [(wu[:, kc * 512 + fl * 128: kc * 512 + (fl + 1) * 128], hb[:, kc * TS:(kc + 1) * TS]) for kc in range(8)]
                            self.mm_group(pu[:, :], pairs, [wuk, hbk], [puk])
                            ks = "st%d" % up
                            bk = "bgst%d" % par
                            self.actf(tr1[up][:, :], pu[:, :], AF.Relu, [puk, bk], [ks + "r1"], bias=bgst[par][:, 8 + f:9 + f], scale=1.0)
                            self.actf(tsg[up][:, :], pg[:, :], AF.Silu, [pgk, bk], [ks + "s"], bias=bgst[par][:, f:f + 1], scale=1.702)
                            self.actf(tr2[up][:, :], tr1[up][:, :], AF.Relu, [ks + "r1", "c14"], [ks + "r2"], bias=c14[:, 0:1], scale=-1.0)
                            self.ts("dve", tq[up][:, :], tsg[up][:, :], C1, None, ALU.min, None, [ks + "s"], [ks + "q"])
                            if pend[0] is not None:
                                pend[0]()
                            def d2(up=up, f=f, ab=ab, abk=abk, ks=ks):
                                self.stt(ab[:, f * TS:(f + 1) * TS], tr2[up][:, :], 8.0, tq[up][:, :], ALU.subtract, ALU.mult, [ks + "r2", ks + "q"], [abk])
                            pend[0] = d2
                        if pend[0] is not None:
                            pend[0](); pend[0] = None
                        if half == 0:
                            for _ in range(2):
                                if nxt < len(jobs):
                                    issued[nxt] = issue(nxt); nxt += 1
                    if j + 1 < NT:
                        prep_tr(j + 1)
                    for _ in range(2):
                        if nxt < len(jobs):
                            issued[nxt] = issue(nxt); nxt += 1
                    wd = [issued.pop(base + 4), issued.pop(base + 5)]
                    for s4 in range(4):
                        ysb = Ys[s4 % 2]; yk = "Ys%d" % (s4 % 2)
                        for half in range(2):
                            (w_, wk_) = wd[half]
                            yp = uc[1] % 2; uc[1] += 1
                            pairs = [(ab[:, fc * TS + s4 * 128: fc * TS + (s4 + 1) * 128], w_[:, fc * 512:(fc + 1) * 512]) for fc in range(8)]
                            self.mm_group(psy[yp][:, :], pairs, [wk_, abk], ["spsy%d" % yp])
                            self.actf(ysb[:, half * 512:(half + 1) * 512], psy[yp][:, :], AF.Copy, ["spsy%d" % yp], [yk])
                        r0 = j * TS + s4 * 128
                        T.dma(self.ys_d[r0:r0 + 128, :], ysb[:, :], reads=[yk], writes=["ys_d"])
                    for _ in range(2):
                        if nxt < len(jobs):
                            issued[nxt] = issue(nxt); nxt += 1
                T.flush()
            with ExitStack() as es:
                sb = lambda name, shape, dt=F32: es.enter_context(nc.sbuf_tensor(self.un(name), shape, dt))
                Y4 = [sb("Y4_%d" % i, [128, 4 * 1024]) for i in range(4)]
                S4 = sb("S4", [128, 4 * 1024])
                GTg = [sb("GTg%d" % i, [32, 512]) for i in range(2)]
                bd = sb("e_bd", [32, 1024])
                Lg = [sb("eL%d" % i, [128, 4096]) for i in range(2)]
                psb2 = [es.enter_context(nc.psum_tensor(self.un("psb2_%d" % i), [128, 512], F32)) for i in range(2)]
                psl2 = [es.enter_context(nc.psum_tensor(self.un("psl2_%d" % i), [128, 512], F32)) for i in range(2)]
                fuse_final = (l == 1 and self.stop_after >= 5)
                if fuse_final:
                    fsq = sb("fsq", [128, 4096]); ftmp = sb("ftmp", [128, 512]); frs = sb("frs", [128, 512])
                    fpsr = es.enter_context(nc.psum_tensor(self.un("fpsr"), [128, 512], F32))
                T.dma(bd[:], self.bdn[l * 32:(l + 1) * 32, :], writes=["e_bd"])
                uc2 = 0; uc3 = 0
                for gidx, (kind, off, n, hoff) in enumerate(gs):
                    who = 0 if kind == "lat" else 1
                    src_d = self.lat_d if who == 0 else self.ctx_d
                    g2 = self.tab(l, who, 1, 2)
                    L = Lg[gidx % 2]; lk = "eL%d" % (gidx % 2)
                    gt = GTg[gidx % 2]; gtk = "GTg%d" % (gidx % 2)
                    T.dma(self.v3(L[:, 0:8 * n], 8), self.v3(src_d, 8)[:, :, off:off + n], writes=[lk])
                    T.dma(gt[:, 0:n], self.gt_d[:, hoff:hoff + n], writes=[gtk])
                    nt4 = n // 128
                    for t in range(nt4):
                        tt = hoff // 128 + t
                        y4 = Y4[tt % 4]; yk = ("Y4", tt % 4)
                        for k in range(4):
                            def gat(eng, y4=y4, k=k, dc_=dcol(tt, k)):
                                return eng.indirect_dma_start(out=y4[:, k * 1024:(k + 1) * 1024], out_offset=None, in_=self.ys_d,
                                                              in_offset=bass.IndirectOffsetOnAxis(ap=dest_i[:, dc_:dc_ + 1], axis=0))
                            T.dma(None, None, reads=["ys_d"], writes=[yk], q="pool", fn=gat)
                        st = S4[:, t * 1024:(t + 1) * 1024]
                        g_ = [gk[:, dcol(tt, k):dcol(tt, k) + 1] for k in range(4)]
                        self.ts("dve", y4[:, 0:1024], y4[:, 0:1024], g_[0], None, ALU.mult, None, [yk], [yk])
                        self.stt(y4[:, 0:1024], y4[:, 1024:2048], g_[1], y4[:, 0:1024], ALU.mult, ALU.add, [yk], [yk])
                        self.stt(y4[:, 0:1024], y4[:, 2048:3072], g_[2], y4[:, 0:1024], ALU.mult, ALU.add, [yk], [yk])
                        self.stt(st, y4[:, 3072:4096], g_[3], y4[:, 0:1024], ALU.mult, ALU.add, [yk], [("S4", t)])
                        for half in range(2):
                            bp = uc2 % 2; uc2 += 1
                            self.mm(psb2[bp][:, :], gt[:, t * 128:(t + 1) * 128], bd[:, half * 512:(half + 1) * 512], True, True, [gtk, "e_bd"], ["psb2_%d" % bp])
                            sl = S4[:, t * 1024 + half * 512: t * 1024 + (half + 1) * 512]
                            self.tt("dve", sl, sl, psb2[bp][:, :], ALU.add, [("S4", t), "psb2_%d" % bp], [("S4", t)])
                    for c in range(8):
                        lp = uc3 % 2; uc3 += 1
                        for t in range(nt4):
                            sig = t in (0, nt4 - 1)
                            T.op("pe", lambda e, c=c, t=t, lp=lp: e.transpose(out=psl2[lp][:, t * 128:(t + 1) * 128], in_=S4[:, t * 1024 + c * 128: t * 1024 + (c + 1) * 128], identity=self.ident[:]),
                                 [("S4", t_) for t_ in range(nt4)] + ["ident"] if sig else (), ["psl2_%d" % lp] if sig else (), signal=sig)
                        sl = L[:, c * n:(c + 1) * n]
                        self.stt(sl, psl2[lp][:, 0:n], self.tabt[:, g2 + c:g2 + c + 1], sl, ALU.mult, ALU.add, ["psl2_%d" % lp, lk], [lk])
                    if fuse_final:
                        self.rstd(L[:, 0:8 * n], n, frs[:, 0:n], [lk], ["frs"], fsq, "fsq", fpsr, "fpsr", ftmp, "ftmp")
                        for c in range(8):
                            sl = L[:, c * n:(c + 1) * n]
                            self.stt(sl, sl, self.gains_t[:, 32 + c:33 + c], frs[:, 0:n], ALU.mult, ALU.mult, [lk, "frs"], [lk])
                        T.dma(self.v3(self.outT, 8)[:, :, off:off + n], self.v3(L[:, 0:8 * n], 8), reads=[lk])
                    else:
                        T.dma(self.v3(src_d, 8)[:, :, off:off + n], self.v3(L[:, 0:8 * n], 8), reads=[lk])
                T.flush()

    def phase_lru(self):
        nc, T = self.nc, self.T
        l = 1
        with ExitStack() as es:
            sb = lambda name, shape, dt=F32: es.enter_context(nc.sbuf_tensor(self.un(name), shape, dt))
            Lg = [sb("lLg%d" % i, [128, 4096]) for i in range(2)]
            sq = sb("lsq", [128, 4096]); tmp = sb("ltmp", [128, 512]); rs = sb("lrs", [128, 512])
            Ht = [sb("lHt%d" % i, [128, 512]) for i in range(2)]
            H16 = [sb("lH16_%d" % i, [128, 4096], BF16) for i in range(2)]
            win = [sb("win%d" % i, [128, 4096], BF16) for i in range(4)]
            xs = [sb("bxs%d" % i, [128, 512]) for i in range(2)]
            x2 = [sb("bx2%d" % i, [128, 512]) for i in range(2)]
            sg = [sb("bsg%d" % i, [128, 512]) for i in range(2)]
            GG = [sb("bGG%d" % i, [128, 4096]) for i in range(2)]
            UU = [sb("bUU%d" % i, [128, 4096]) for i in range(2)]
            psr = es.enter_context(nc.psum_tensor(self.un("lpsr"), [128, 512], F32))
            ps = [es.enter_context(nc.psum_tensor(self.un("bps%d" % i), [128, 512], F32)) for i in range(2)]
            for j in range(4):
                T.dma(Lg[j % 2][:], self.win_p[j * 128:(j + 1) * 128, :], writes=["lLg%d" % (j % 2)])
                self.actf(win[j][:], Lg[j % 2][:], AF.Copy, ["lLg%d" % (j % 2)], ["win%d" % j])
            gs = [("lat", g * 512, 512, g * 512) for g in range(8)] + [("ctx", 0, 256, T_LAT)]
            ucb = [0]

            def norm(gidx):
                (kind, off, n, hoff) = gs[gidx]
                who = 0 if kind == "lat" else 1
                src_d = self.lat_d if who == 0 else self.ctx_d
                L = Lg[gidx % 2]; key = "lLg%d" % (gidx % 2)
                T.dma(self.v3(L[:, 0:8 * n], 8), self.v3(src_d, 8)[:, :, off:off + n], writes=[key])
                self.rstd(L[:, 0:8 * n], n, rs[:, 0:n], [key], ["lrs"], sq, "lsq", psr, "lpsr", tmp, "ltmp")
                a = self.tab(l, who, 0, 0); b = self.tab(l, who, 0, 1)
                hh16 = H16[gidx % 2]; hk16 = "lH16_%d" % (gidx % 2)
                for c in range(8):
                    ht = Ht[c % 2]; htk = "lHt%d" % (c % 2)
                    self.tt("dve", ht[:, 0:n], L[:, c * n:(c + 1) * n], rs[:, 0:n], ALU.mult, [key, "lrs"], [htk])
                    self.ts("dve", hh16[:, c * n:(c + 1) * n], ht[:, 0:n], self.tabt[:, a + c:a + c + 1], self.tabt[:, b + c:b + c + 1], ALU.mult, ALU.add, [htk], [hk16])

            def proj(gidx):
                (kind, off, n, hoff) = gs[gidx]
                hg = H16[gidx % 2]; hk = "lH16_%d" % (gidx % 2)
                gg = GG[gidx % 2]; ggk = "bGG%d" % (gidx % 2)
                uu = UU[gidx % 2]; uuk = "bUU%d" % (gidx % 2)
                for oc in range(16):
                    if kind == "ctx" and oc < 8:
                        continue
                    par = ucb[0] % 2; ucb[0] += 1
                    p_ = ps[par]; pk = "bps%d" % par
                    w = win[oc // 4]; wk = "win%d" % (oc // 4)
                    ol = oc % 4
                    pairs = [(w[:, kc * 512 + ol * 128: kc * 512 + (ol + 1) * 128], hg[:, kc * n:(kc + 1) * n]) for kc in range(8)]
                    self.mm_group(p_[:, 0:n], pairs, [wk, hk], [pk])
                    if oc < 8:
                        k_ = "bx%d" % par
                        self.actf(xs[par][:, 0:n], p_[:, 0:n], AF.Copy, [pk], [k_ + "s"])
                        self.tt("dve", x2[par][:, 0:n], xs[par][:, 0:n], xs[par][:, 0:n], ALU.mult, [k_ + "s"], [k_ + "2"])
                        self.ts("dve", x2[par][:, 0:n], x2[par][:, 0:n], 0.044715, 1.0, ALU.mult, ALU.add, [k_ + "2"], [k_ + "2"])
                        self.tt("dve", x2[par][:, 0:n], x2[par][:, 0:n], xs[par][:, 0:n], ALU.mult, [k_ + "2", k_ + "s"], [k_ + "2"])
                        self.actf(sg[par][:, 0:n], x2[par][:, 0:n], AF.Sigmoid, [k_ + "2"], [k_ + "g"], scale=1.5957691216057308)
                        self.tt("dve", gg[:, oc * n:(oc + 1) * n], xs[par][:, 0:n], sg[par][:, 0:n], ALU.mult, [k_ + "s", k_ + "g"], [ggk])
                    else:
                        self.actf(uu[:, (oc - 8) * n:(oc - 7) * n], p_[:, 0:n], AF.Copy, [pk], [uuk])
                if kind == "lat":
                    T.dma(self.v3(self.gg_d, 8)[:, :, off:off + n], self.v3(gg[:, 0:8 * n], 8), reads=[ggk])
                    T.dma(self.v3(self.u_d, 8)[:, :, T_CTX + off:T_CTX + off + n], self.v3(uu[:, 0:8 * n], 8), reads=[uuk])
                else:
                    T.dma(self.v3(self.u_d, 8)[:, :, 0:T_CTX], self.v3(uu[:, 0:8 * n], 8), reads=[uuk])
            norm(0)
            for gidx in range(len(gs)):
                if gidx + 1 < len(gs):
                    norm(gidx + 1)
                proj(gidx)
            T.flush()
        with ExitStack() as es:
            sb = lambda name, shape, dt=F32: es.enter_context(nc.sbuf_tensor(self.un(name), shape, dt))
            cw = sb("cw", [128, 32]); lv = sb("lv", [128, 56]); cA = sb("cA", [128, 16]); tmp16 = sb("tmp16", [128, 16])
            w32 = sb("cw32", [128, 4096]); wr16 = sb("wr16", [128, 4096], BF16); wi16 = sb("wi16", [128, 4096], BF16)
            U = sb("cU", [128, NTOK])
            UC32 = [sb("UC32_%d" % i, [128, NTOK]) for i in range(2)]
            UC16 = [sb("UC16_%d" % i, [128, NTOK], BF16) for i in range(2)]
            Aa = sb("Aa", [128, NTOK]); Bb = sb("Bb", [128, NTOK]); Hs = sb("Hs", [128, NTOK])
            REC = sb("REC", [128, T_LAT]); Z16 = sb("Z16", [128, T_LAT], BF16)
            tr = [sb("ctr%d" % i, [128, 512]) for i in range(2)]
            ti = [sb("cti%d" % i, [128, 512]) for i in range(2)]
            tm = [sb("ctm%d" % i, [128, 512]) for i in range(2)]
            psr = [es.enter_context(nc.psum_tensor(self.un("cpr%d" % i), [128, 512], F32)) for i in range(2)]
            psi = [es.enter_context(nc.psum_tensor(self.un("cpi%d" % i), [128, 512], F32)) for i in range(2)]
            T.dma(cw[:], self.convw[:, :], writes=["cw"])
            T.dma(lv[:], self.lruv[:, :], writes=["lv"])
            T.dma(w32[:], self.wr_p[:, :], writes=["cw32"])
            self.actf(wr16[:], w32[:], AF.Copy, ["cw32"], ["wr16"])
            T.dma(w32[:], self.wi_p[:, :], writes=["cw32"])
            self.actf(wi16[:], w32[:], AF.Copy, ["cw32"], ["wi16"])
            self.actf(tmp16[:], lv[:, 40:56], AF.Exp, ["lv"], ["tmp16"], scale=-1.0)
            self.actf(tmp16[:], tmp16[:], AF.Ln, ["tmp16"], ["tmp16"], bias=1.0, scale=1.0)
            self.ts("dve", cA[:], tmp16[:], -8.0, None, ALU.mult, None, ["tmp16"], ["cA"])
            segs = [(0, T_CTX), (T_CTX, NTOK)]
            gs = [(s, min(512, NTOK - s)) for s in range(0, NTOK, 512)]
            uc = 0
            g1 = self.tab(1, 0, 0, 2)
            for nb in range(4):
                for ci in range(2):
                    c = 2 * nb + ci
                    T.dma(U[:], self.u_d[:, c * NTOK:(c + 1) * NTOK], writes=["cU"])
                    uc32 = UC32[ci]; k32 = "UC32_%d" % ci
                    for (s0, s1) in segs:
                        self.actf(uc32[:, s0:s1], U[:, s0:s1], AF.Identity, ["cU", "cw", "lv"], [k32], bias=lv[:, c:c + 1], scale=cw[:, 2 * 8 + c:2 * 8 + c + 1])
                        self.stt(uc32[:, s0 + 2:s1], U[:, s0:s1 - 2], cw[:, 0 * 8 + c:0 * 8 + c + 1], uc32[:, s0 + 2:s1], ALU.mult, ALU.add, ["cU", k32], [k32])
                        self.stt(uc32[:, s0 + 1:s1], U[:, s0:s1 - 1], cw[:, 1 * 8 + c:1 * 8 + c + 1], uc32[:, s0 + 1:s1], ALU.mult, ALU.add, ["cU", k32], [k32])
                        self.stt(uc32[:, s0:s1 - 1], U[:, s0 + 1:s1], cw[:, 3 * 8 + c:3 * 8 + c + 1], uc32[:, s0:s1 - 1], ALU.mult, ALU.add, ["cU", k32], [k32])
                    self.actf(UC16[ci][:], uc32[:], AF.Copy, [k32], ["UC16_%d" % ci])
                for oc in range(2):
                    c = 2 * nb + oc
                    for d in range(2):
                        for (s, n) in gs:
                            par = uc % 2; uc += 1
                            wbase = ((d * 4 + nb) * 2) * 256
                            pairs = [(wr16[:, wbase + kc * 256 + oc * 128: wbase + kc * 256 + (oc + 1) * 128], UC16[kc][:, s:s + n]) for kc in range(2)]
                            self.mm_group(psr[par][:, 0:n], pairs, ["wr16", "UC16_0", "UC16_1"], ["cpr%d" % par])
                            pairs = [(wi16[:, wbase + kc * 256 + oc * 128: wbase + kc * 256 + (oc + 1) * 128], UC16[kc][:, s:s + n]) for kc in range(2)]
                            self.mm_group(psi[par][:, 0:n], pairs, ["wi16", "UC16_0", "UC16_1"], ["cpi%d" % par])
                            self.actf(Aa[:, s:s + n], psr[par][:, 0:n], AF.Sigmoid, ["cpr%d" % par, "lv"], ["Aa"], bias=lv[:, 8 + d * 8 + c:8 + d * 8 + c + 1], scale=1.0)
                            self.actf(Bb[:, s:s + n], psi[par][:, 0:n], AF.Sigmoid, ["cpi%d" % par, "lv"], ["Bb"], bias=lv[:, 24 + d * 8 + c:24 + d * 8 + c + 1], scale=1.0)
                        self.actf(Aa[:, :], Aa[:, :], AF.Exp, ["Aa", "cA"], ["Aa"], scale=cA[:, d * 8 + c:d * 8 + c + 1])
                        self.actf(Hs[:, :], Aa[:, :], AF.Square, ["Aa"], ["Hs"])
                        self.actf(Hs[:, :], Hs[:, :], AF.Sqrt, ["Hs"], ["Hs"], bias=1.0, scale=-1.0)
                        self.tt("dve", Bb[:, :], Bb[:, :], UC32[oc][:, :], ALU.mult, ["Bb", "UC32_%d" % oc], ["Bb"])
                        self.tt("dve", Bb[:, :], Bb[:, :], Hs[:, :], ALU.mult, ["Bb", "Hs"], ["Bb"])
                        if d == 0:
                            T.op("dve", lambda e: e.tensor_tensor_scan(out=Hs[:, 0:T_CTX], data0=Aa[:, 0:T_CTX], data1=Bb[:, 0:T_CTX], initial=0.0, op0=ALU.mult, op1=ALU.add), ["Aa", "Bb"], ["Hs"])
                            T.op("dve", lambda e: e.tensor_tensor_scan(out=REC[:, :], data0=Aa[:, T_CTX:NTOK], data1=Bb[:, T_CTX:NTOK], initial=Hs[:, T_CTX - 1:T_CTX], op0=ALU.mult, op1=ALU.add), ["Aa", "Bb", "Hs"], ["REC"])
                        else:
                            def rev(t, a, b):
                                apx = t[:, a:b]
                                pstep = apx.ap[0][0]
                                return bass.AP(apx.tensor, apx.offset + (b - a - 1), [[pstep, 128], [-1, b - a]])
                            T.op("dve", lambda e: e.tensor_tensor_scan(out=rev(Hs, 0, T_CTX), data0=rev(Aa, 0, T_CTX), data1=rev(Bb, 0, T_CTX), initial=0.0, op0=ALU.mult, op1=ALU.add), ["Aa", "Bb"], ["Hs"])
                            T.op("dve", lambda e: e.tensor_tensor_scan(out=rev(Hs, T_CTX, NTOK), data0=rev(Aa, T_CTX, NTOK), data1=rev(Bb, T_CTX, NTOK), initial=Hs[:, 0:1], op0=ALU.mult, op1=ALU.add), ["Aa", "Bb", "Hs"], ["Hs"])
                            self.tt("dve", REC[:, :], REC[:, :], Hs[:, T_CTX:NTOK], ALU.add, ["REC", "Hs"], ["REC"])
                    T.dma(U[:, 0:T_LAT], self.gg_d[:, c * T_LAT:(c + 1) * T_LAT], writes=["cU"])
                    self.tt("dve", Z16[:, :], REC[:, :], U[:, 0:T_LAT], ALU.mult, ["REC", "cU"], ["Z16"])
                    T.dma(self.z_d[:, c * T_LAT:(c + 1) * T_LAT], Z16[:, :], reads=["Z16"])
            T.flush()
        with ExitStack() as es:
            sb = lambda name, shape, dt=F32: es.enter_context(nc.sbuf_tensor(self.un(name), shape, dt))
            stg = [sb("dstg%d" % i, [128, 4096]) for i in range(2)]
            wo = [sb("wo%d" % i, [128, 4096], BF16) for i in range(2)]
            Zg = [sb("dZ%d" % i, [128, 4096], BF16) for i in range(2)]
            Lg = [sb("dL%d" % i, [128, 4096]) for i in range(2)]
            ps = [es.enter_context(nc.psum_tensor(self.un("dps%d" % i), [128, 512], F32)) for i in range(2)]
            for j in range(2):
                T.dma(stg[j][:], self.wout_p[j * 128:(j + 1) * 128, :], writes=["dstg%d" % j])
                self.actf(wo[j][:], stg[j][:], AF.Copy, ["dstg%d" % j], ["wo%d" % j])
            g1 = self.tab(1, 0, 0, 2)
            uc = 0
            for g in range(8):
                z = Zg[g % 2]; zk = "dZ%d" % (g % 2)
                L = Lg[g % 2]; lk = "dL%d" % (g % 2)
                T.dma(self.v3(z[:, :], 8), self.v3(self.z_d, 8)[:, :, g * 512:(g + 1) * 512], writes=[zk])
                T.dma(self.v3(L[:, :], 8), self.v3(self.lat_d, 8)[:, :, g * 512:(g + 1) * 512], writes=[lk])
                for dc in range(8):
                    par = uc % 2; uc += 1
                    w = wo[dc // 4]; dl = dc % 4
                    pairs = [(w[:, kc * 512 + dl * 128: kc * 512 + (dl + 1) * 128], z[:, kc * 512:(kc + 1) * 512]) for kc in range(8)]
                    self.mm_group(ps[par][:, :], pairs, ["wo%d" % (dc // 4), zk], ["dps%d" % par])
                    sl = L[:, dc * 512:(dc + 1) * 512]
                    self.stt(sl, ps[par][:, :], self.tabt[:, g1 + dc:g1 + dc + 1], sl, ALU.mult, ALU.add, ["dps%d" % par, lk], [lk])
                T.dma(self.v3(self.lat_d, 8)[:, :, g * 512:(g + 1) * 512], self.v3(L[:, :], 8), reads=[lk])
            T.flush()

    def phase_final(self, do_norm):
        nc, T = self.nc, self.T
        with ExitStack() as es:
            sb = lambda name, shape, dt=F32: es.enter_context(nc.sbuf_tensor(self.un(name), shape, dt))
            Lg = [sb("fL%d" % i, [128, 4096]) for i in range(2)]
            sq = sb("fsq", [128, 4096]); tmp = sb("ftmp", [128, 512]); rs = sb("frs", [128, 512])
            psr = es.enter_context(nc.psum_tensor(self.un("fpsr"), [128, 512], F32))
            for g in range(8):
                L = Lg[g % 2]; key = "fL%d" % (g % 2)
                T.dma(self.v3(L[:, :], 8), self.v3(self.lat_d, 8)[:, :, g * 512:(g + 1) * 512], writes=[key])
                if do_norm:
                    self.rstd(L[:, :], 512, rs[:, :], [key], ["frs"], sq, "fsq", psr, "fpsr", tmp, "ftmp")
                    for c in range(8):
                        sl = L[:, c * 512:(c + 1) * 512]
                        self.stt(sl, sl, self.gains_t[:, 32 + c:33 + c], rs[:, :], ALU.mult, ALU.mult, [key, "frs"], [key])
                T.dma(self.v3(self.outT, 8)[:, :, g * 512:(g + 1) * 512], self.v3(L[:, :], 8), reads=[key])
            T.flush()


def _col(v, n):
    return np.ascontiguousarray(np.asarray(v, np.float32).reshape(n, 128).T)


def _pieces(w, ncol_pieces):
    w = np.asarray(w, np.float32)
    return np.ascontiguousarray(w.reshape(8, 128, ncol_pieces, 512).transpose(2, 1, 0, 3)).reshape(ncol_pieces, 128, 4096)


def _inv_counts():
    def bounds(n, w):
        idx = np.arange(n)
        return np.clip(idx - w // 2, 0, n), np.clip(idx + w // 2, 0, n)
    i2 = np.zeros((4, 4096), np.float32)
    i1 = np.zeros((4, 256), np.float32)
    for gi, w in enumerate((2, 4, 8, 16)):
        r0, r1 = bounds(64, w)
        cnt = ((r1 - r0)[:, None] * (r1 - r0)[None, :]).astype(np.float32)
        i2[gi] = (np.float32(1.0) / cnt).reshape(-1)
        l0, l1 = bounds(256, w)
        i1[gi] = np.float32(1.0) / (l1 - l0).astype(np.float32)
    return i2, i1


def prep_shared(inp, nexp=32):
    f = lambda k: np.asarray(inp[k], np.float32)
    sh = {}
    ada_w = f("ada_w")
    sh["ada_wp"] = np.concatenate([_pieces(ada_w[l], 12) for l in range(2)], 0).reshape(2 * 12 * 128, 4096)
    sh["ada_bp"] = np.concatenate([_col(f("ada_b")[l], 48) for l in range(2)], 1)
    sh["gains"] = np.concatenate([_col(f("norm_mix")[0], 8), _col(f("norm_ffn")[0], 8), _col(f("norm_mix")[1], 8),
                                  _col(f("norm_ffn")[1], 8), _col(f("final_norm"), 8)], 1)
    sh["pscale"] = _col(f("pool_scale")[0], 8)
    sh["poolw"] = np.ascontiguousarray(f("pool_w")[0].reshape(4, 2, 128, 256).transpose(2, 0, 1, 3)).reshape(128, 2048)
    sh["invc2"], sh["invc1"] = _inv_counts()
    sh["win_p"] = _pieces(f("lru_w_in")[0], 4).reshape(4 * 128, 4096)
    sh["wout_p"] = _pieces(f("lru_w_out")[0], 2).reshape(2 * 128, 4096)
    sh["convw"] = np.concatenate([_col(f("lru_conv_w")[0][k], 8) for k in range(4)], 1)
    sh["lruv"] = np.concatenate([_col(f("lru_conv_b")[0], 8)] + [_col(f("lru_b_r")[0][d], 8) for d in range(2)] +
                                [_col(f("lru_b_i")[0][d], 8) for d in range(2)] + [_col(f("lru_lam")[0][d], 8) for d in range(2)], 1)
    for nm, key in (("wr_p", "lru_w_r"), ("wi_p", "lru_w_i")):
        w = f(key)[0]
        sh[nm] = np.ascontiguousarray(w.reshape(2, 4, 2, 128, 256).transpose(3, 0, 1, 2, 4)).reshape(128, 4096)
    rw = f("router_w")
    sh["router_wp"] = np.ascontiguousarray(rw.reshape(2, 8, 128, 32).transpose(2, 0, 1, 3)).reshape(128, 512)
    sh["router_bp"] = np.ascontiguousarray(f("router_b"))
    wgu = f("exp_w_gu"); wdn = f("exp_w_down")
    wexp = np.empty((2, 32, 6, 128, 4096), np.float32)
    for l in range(2):
        for e in range(nexp):
            pg = _pieces(wgu[l, e], 4)
            pd = _pieces(wdn[l, e], 2)
            wexp[l, e, 0] = pg[0]; wexp[l, e, 1] = pg[2]; wexp[l, e, 2] = pg[1]; wexp[l, e, 3] = pg[3]
            wexp[l, e, 4] = pd[0]; wexp[l, e, 5] = pd[1]
    sh["wexp"] = wexp.reshape(2 * 32 * 6 * 128, 4096)
    bgu = f("exp_b_gu")
    sh["bgu"] = np.ascontiguousarray(bgu.reshape(2, 32, 16, 128).transpose(3, 0, 1, 2)).reshape(128, 1024)
    sh["bdn"] = np.ascontiguousarray(f("exp_b_down").reshape(64, 1024))
    sh["identd"] = np.eye(128, dtype=np.float32)
    import ml_dtypes
    sh["identbd"] = np.eye(128).astype(ml_dtypes.bfloat16)
    sh["utrid"] = np.triu(np.ones((128, 128), np.float32), 1).astype(ml_dtypes.bfloat16)
    sh["iotapd"] = np.arange(128, dtype=np.float32).reshape(128, 1)
    sh["jposd"] = (np.arange(66, dtype=np.float32) * 512.0).reshape(1, 66)
    sh["bgu_e"] = np.ascontiguousarray(bgu.reshape(2, 32, 16, 128).transpose(0, 1, 3, 2)).reshape(64 * 128, 16)
    return sh


def prep_core(inp, b):
    x = np.asarray(inp["x"][b], np.float32)
    ctx = np.asarray(inp["ctx"][b], np.float32)
    d = {}
    d["xT"] = np.ascontiguousarray(x.T.reshape(8, 128, T_LAT).transpose(1, 0, 2)).reshape(128, 8 * T_LAT)
    d["ctxT"] = np.ascontiguousarray(ctx.T.reshape(8, 128, T_CTX).transpose(1, 0, 2)).reshape(128, 8 * T_CTX)
    d["cs"] = np.concatenate([_col(inp["c"][b], 8), _col(inp["c_ctx"], 8)], 1)
    return d


def unpack_out(o):
    return np.ascontiguousarray(o.reshape(128, 8, T_LAT).transpose(1, 0, 2).reshape(1024, T_LAT).T)


_CACHE = {}


def kernel(**inputs):
    if "nc" not in _CACHE:
        _CACHE["nc"] = K().build()
    nc = _CACHE["nc"]
    sh = prep_shared(inputs)
    in_maps = []
    for b in range(8):
        m = dict(sh)
        m.update(prep_core(inputs, b))
        in_maps.append(m)
    res = run_bass_kernel_spmd(nc, in_maps, core_ids=list(range(8)))
    out = np.stack([unpack_out(res.results[b]["outT"]) for b in range(8)], 0)
    return out.astype(np.float32)
```

```python
import numpy as np
from contextlib import ExitStack
import concourse.bass as bass
import concourse.mybir as mybir
from concourse.bass_utils import run_bass_kernel_spmd

F32 = mybir.dt.float32
BF16 = mybir.dt.bfloat16
ALU = mybir.AluOpType
AF = mybir.ActivationFunctionType

T_LAT = 4096
T_CTX = 256
NTOK = T_LAT + T_CTX
NDS = 16
EPS = 1e-6


class Tr:
    ENG = ("pe", "act", "dve", "pool", "sp")

    def __init__(self, nc, es):
        self.nc = nc
        self.sem = {k: es.enter_context(nc.semaphore("s_" + k)) for k in self.ENG}
        self.dsem = [es.enter_context(nc.semaphore("d%d" % i)) for i in range(NDS)]
        self.cnt = {k: 0 for k in self.ENG}
        self.dcnt = [0] * NDS
        self.dnext = {"sp": 0, "pool": 0}
        self.seen = {k: {} for k in self.ENG}
        self.lastw = {}
        self.readers = {}
        self.ops = {k: [] for k in self.ENG}

    def _semh(self, key):
        return self.sem[key] if isinstance(key, str) else self.dsem[key[1]]

    def _collect(self, e, reads, writes, extra=()):
        need = {}

        def add(t):
            if t is None:
                return
            k, v = t
            if need.get(k, 0) < v:
                need[k] = v
        for r in reads:
            for k, v in self.lastw.get(r, {}).items():
                add((k, v))
        for w in writes:
            for k, v in self.lastw.get(w, {}).items():
                add((k, v))
            for k, v in self.readers.get(w, {}).items():
                add((k, v))
        for t in extra:
            add(t)
        wl = []
        for k, v in need.items():
            if k == e and e == "pe":
                continue
            if self.seen[e].get(k, 0) >= v:
                continue
            self.seen[e][k] = v
            wl.append((self._semh(k), v))
        return wl

    def _commit(self, ticket, reads, writes):
        k, v = ticket
        for r in reads:
            d = self.readers.setdefault(r, {})
            if d.get(k, 0) < v:
                d[k] = v
        for w in writes:
            d = self.lastw.setdefault(w, {})
            if d.get(k, 0) < v:
                d[k] = v
            self.readers[w] = {}

    def op(self, e, fn, reads=(), writes=(), signal=True):
        if not signal:
            self.ops[e].append(((), fn, None, 0))
            return None
        wl = self._collect(e, reads, writes)
        self.cnt[e] += 1
        ticket = (e, self.cnt[e])
        self.ops[e].append((wl, fn, self.sem[e], 1))
        self._commit(ticket, reads, writes)
        return ticket

    def dma(self, out, in_, reads=(), writes=(), q="sp", fn=None):
        half = NDS // 2
        j = self.dnext[q] + (0 if q == "sp" else half)
        self.dnext[q] = (self.dnext[q] + 1) % half
        extra = []
        if self.dcnt[j] > 0:
            extra.append((("d", j), self.dcnt[j]))
        wl = self._collect(q, reads, writes, extra)
        self.dcnt[j] += 16
        ticket = (("d", j), self.dcnt[j])
        if fn is None:
            fn = lambda eng, out=out, in_=in_: eng.dma_start(out=out, in_=in_)
        self.ops[q].append((wl, fn, self.dsem[j], 16))
        self._commit(ticket, reads, writes)
        return ticket

    def flush(self):
        nc = self.nc
        wl = []
        for j in range(NDS):
            if self.dcnt[j] > 0 and self.seen["sp"].get(("d", j), 0) < self.dcnt[j]:
                self.seen["sp"][("d", j)] = self.dcnt[j]
                wl.append((self.dsem[j], self.dcnt[j]))
        if wl:
            self.ops["sp"].append((wl, None, None, 0))
        ops = self.ops
        if not any(ops[k] for k in self.ENG):
            return
        self.ops = {k: [] for k in self.ENG}
        self.lastw = {}
        self.readers = {}

        def replay(eng, lst):
            for wl, fn, semh, inc in lst:
                for (sh, v) in wl:
                    eng.wait_ge(sh, v)
                if fn is not None:
                    ins = fn(eng)
                    if semh is not None:
                        ins.then_inc(semh, inc)

        with nc.Block() as block:
            if ops["sp"]:
                @block.sync
                def _(eng):
                    replay(eng, ops["sp"])
            if ops["pe"]:
                @block.tensor
                def _(eng):
                    replay(eng, ops["pe"])
            if ops["act"]:
                @block.scalar
                def _(eng):
                    replay(eng, ops["act"])
            if ops["dve"]:
                @block.vector
                def _(eng):
                    replay(eng, ops["dve"])
            if ops["pool"]:
                @block.gpsimd
                def _(eng):
                    replay(eng, ops["pool"])


class K:
    def __init__(self, nexp=32, stop_after=99, sparse=True):
        self.sparse = sparse
        self.nexp = nexp
        self.stop_after = stop_after
        nc = self.nc = bass.Bass("TRN2", target_bir_lowering=False)

        def din(name, shape, dt=F32):
            return nc.dram_tensor(name, shape, dt, kind="ExternalInput").ap()

        def dint(name, shape, dt=F32):
            return nc.dram_tensor(name, shape, dt, kind="Internal").ap()
        self.xT = din("xT", [128, 8 * T_LAT])
        self.ctxT = din("ctxT", [128, 8 * T_CTX])
        self.cs = din("cs", [128, 16])
        self.ada_wp = din("ada_wp", [2 * 12 * 128, 4096])
        self.ada_bp = din("ada_bp", [128, 96])
        self.gains = din("gains", [128, 40])
        self.pscale = din("pscale", [128, 8])
        self.poolw = din("poolw", [128, 2048])
        self.invc2 = din("invc2", [4, 4096])
        self.invc1 = din("invc1", [4, 256])
        self.win_p = din("win_p", [4 * 128, 4096])
        self.wout_p = din("wout_p", [2 * 128, 4096])
        self.convw = din("convw", [128, 32])
        self.lruv = din("lruv", [128, 56])
        self.wr_p = din("wr_p", [128, 4096])
        self.wi_p = din("wi_p", [128, 4096])
        self.router_wp = din("router_wp", [128, 512])
        self.router_bp = din("router_bp", [2, 32])
        self.wexp = din("wexp", [2 * 32 * 6 * 128, 4096])
        self.bgu = din("bgu", [128, 2 * 32 * 16])
        self.bdn = din("bdn", [64, 1024])
        self.identd = din("identd", [128, 128])
        self.identbd = din("identbd", [128, 128], BF16)
        self.utrid = din("utrid", [128, 128], BF16)
        self.iotapd = din("iotapd", [128, 1])
        self.jposd = din("jposd", [1, 66])
        self.bgu_e = din("bgu_e", [64 * 128, 16])
        self.xs_d = dint("xs_d", [66 * 512, 1024], BF16)
        self.ys_d = dint("ys_d", [66 * 512, 1024])
        self.outT = nc.dram_tensor("outT", [128, 8 * T_LAT], F32, kind="ExternalOutput").ap()
        self.lat_d = dint("lat_d", [128, 8 * T_LAT])
        self.ctx_d = dint("ctx_d", [128, 8 * T_CTX])
        self.h_d = dint("h_d", [128, 8 * NTOK], BF16)
        self.gt_d = dint("gt_d", [32, NTOK])
        self.u_d = dint("u_d", [128, 8 * NTOK])
        self.gg_d = dint("gg_d", [128, 8 * T_LAT])
        self.z_d = dint("z_d", [128, 8 * T_LAT], BF16)

    def zero_step(self, zt, n):
        if not self.sparse:
            return
        z = getattr(self, "_zrow", 0)
        for _ in range(n):
            if z < 66 * 512:
                self.T.dma(self.xs_d[z:z + 512, :].rearrange("(p r) c -> p (r c)", p=128), zt[:], reads=["zt"])
                z += 512
        self._zrow = z

    def un(self, name):
        self._uid = getattr(self, "_uid", 0) + 1
        return "%s_u%d" % (name, self._uid)

    def v3(self, ap2, c):
        return ap2.rearrange("p (c t) -> p c t", c=c)

    def tt(self, e, out, in0, in1, op, reads, writes):
        self.T.op(e, lambda g: g.tensor_tensor(out=out, in0=in0, in1=in1, op=op), reads, writes)

    def ts(self, e, out, in0, s1, s2, op0, op1, reads, writes):
        if s2 is None:
            self.T.op(e, lambda g: g.tensor_scalar(out=out, in0=in0, scalar1=s1, scalar2=None, op0=op0), reads, writes)
        else:
            self.T.op(e, lambda g: g.tensor_scalar(out=out, in0=in0, scalar1=s1, scalar2=s2, op0=op0, op1=op1), reads, writes)

    def stt(self, out, in0, scalar, in1, op0, op1, reads, writes):
        self.T.op("dve", lambda g: g.scalar_tensor_tensor(out=out, in0=in0, scalar=scalar, in1=in1, op0=op0, op1=op1), reads, writes)

    def actf(self, out, in_, func, reads, writes, bias=None, scale=None):
        kw = {}
        if bias is not None:
            kw["bias"] = bias
        if scale is not None:
            kw["scale"] = scale
        self.T.op("act", lambda g: g.activation(out=out, in_=in_, func=func, **kw), reads, writes)

    def mm(self, out, lhsT, rhs, start, stop, reads=(), writes=(), signal=True):
        self.T.op("pe", lambda g: g.matmul(out, lhsT=lhsT, rhs=rhs, start=start, stop=stop), reads, writes, signal=signal)

    def mm_group(self, out, pairs, reads, writes):
        n = len(pairs)
        for i, (l, r) in enumerate(pairs):
            sig = (i == 0) or (i == n - 1)
            self.mm(out, l, r, i == 0, i == n - 1, reads if sig else (), writes if sig else (), signal=sig)

    def tab(self, l, who, n, kind):
        i = ((l * 2 + who) * 2 + n) * 3 + kind
        return i * 8

    def build(self):
        nc = self.nc
        with ExitStack() as es:
            self.T = Tr(nc, es)
            self.tabt = es.enter_context(nc.sbuf_tensor("tabt", [128, 24 * 8], F32))
            self.ones = es.enter_context(nc.sbuf_tensor("ones", [128, 128], F32))
            self.ident = es.enter_context(nc.sbuf_tensor("ident", [128, 128], F32))
            self.gains_t = es.enter_context(nc.sbuf_tensor("gains_t", [128, 40], F32))
            self.epsc = es.enter_context(nc.sbuf_tensor("epsc", [128, 1], F32))
            with ExitStack() as es0:
                gen = self.phase_pool(es0)
                left = [9]

                def hook():
                    if left[0] > 0:
                        left[0] -= 1
                        next(gen, None)
                self.phase_adaln(es0, hook=hook)
                for _ in gen:
                    pass
                self.T.flush()
            if self.stop_after >= 2:
                if self.sparse:
                    self.phase_moe_sparse(0)
                else:
                    self.phase_norm_router(0)
                    self.T.flush()
                    self.phase_moe(0)
                self.T.flush()
            if self.stop_after >= 3:
                self.phase_lru()
                self.T.flush()
            if self.stop_after >= 4:
                if self.sparse:
                    self.phase_moe_sparse(1)
                else:
                    self.phase_norm_router(1)
                    self.T.flush()
                    self.phase_moe(1)
                self.T.flush()
            if not (self.sparse and self.stop_after >= 5):
                self.phase_final(self.stop_after >= 5)
            self.T.flush()
        return nc

    def phase_adaln(self, es, hook=None):
        nc, T = self.nc, self.T
        if True:
            sb = lambda name, shape, dt=F32: es.enter_context(nc.sbuf_tensor(self.un(name), shape, dt))
            cs_t = sb("cs_t", [128, 16]); sv = sb("sv", [128, 16])
            adab = sb("adab", [128, 96]); psc = sb("psc", [128, 8])
            modt = sb("modt", [128, 2 * 2 * 48])
            stg = [sb("a_stg%d" % i, [128, 4096]) for i in range(2)]
            tmp8 = sb("tmp8", [128, 8])
            psm = es.enter_context(nc.psum_tensor(self.un("psm"), [128, 192], F32))
            T.dma(cs_t[:], self.cs[:, :], writes=["cs_t"])
            T.dma(adab[:], self.ada_bp[:, :], writes=["adab"])
            T.dma(psc[:], self.pscale[:, :], writes=["psc"])
            T.dma(self.gains_t[:], self.gains[:, :], writes=["gains"])
            T.dma(self.ident[:], self.identd[:, :], writes=["ident"])
            T.op("pool", lambda g: g.memset(self.ones[:], 1.0), writes=["ones"])
            T.op("pool", lambda g: g.memset(self.epsc[:], EPS), writes=["epsc"])
            self.actf(sv[:], cs_t[:], AF.Silu, ["cs_t"], ["sv"])
            sv2 = sv[:].rearrange("p (two k) -> p k two", two=2)
            for l in range(2):
                for j in range(12):
                    s = stg[j % 2]
                    key = "a_stg%d" % (j % 2)
                    r0 = (l * 12 + j) * 128
                    T.dma(s[:], self.ada_wp[r0:r0 + 128, :], writes=[key])
                    for m in range(4):
                        jj = j * 4 + m
                        col = l * 96 + jj * 2
                        pairs = [(s[:, kc * 512 + m * 128: kc * 512 + (m + 1) * 128], sv2[:, kc, :]) for kc in range(8)]
                        self.mm_group(psm[:, col:col + 2], pairs, [key, "sv"], ["psm"])
                    if hook is not None and j % 2 == 1:
                        hook()
            for l in range(2):
                for who in range(2):
                    src = psm[:, l * 96:(l + 1) * 96].rearrange("p (j t) -> p j t", t=2)[:, :, who]
                    o = (l * 2 + who) * 48
                    self.tt("dve", modt[:, o:o + 48], src, adab[:, l * 48:(l + 1) * 48], ALU.add, ["psm", "adab"], ["modt"])
            for l in range(2):
                for who in range(2):
                    o = (l * 2 + who) * 48
                    for n in range(2):
                        gcol = (2 * l + n) * 8
                        a = self.tab(l, who, n, 0); b = self.tab(l, who, n, 1); g = self.tab(l, who, n, 2)
                        sc = modt[:, o + (1 + 3 * n) * 8: o + (2 + 3 * n) * 8]
                        sh = modt[:, o + (3 * n) * 8: o + (3 * n + 1) * 8]
                        gg = modt[:, o + (2 + 3 * n) * 8: o + (3 + 3 * n) * 8]
                        self.ts("dve", tmp8[:], sc, 1.0, None, ALU.add, None, ["modt"], ["tmp8"])
                        self.tt("dve", self.tabt[:, a:a + 8], tmp8[:], self.gains_t[:, gcol:gcol + 8], ALU.mult, ["tmp8", "gains"], ["tab"])
                        self.T.op("dve", lambda e, b=b, sh=sh: e.tensor_copy(out=self.tabt[:, b:b + 8], in_=sh), ["modt"], ["tab"])
                        if l == 0 and n == 0:
                            self.tt("dve", self.tabt[:, g:g + 8], gg, psc[:], ALU.mult, ["modt", "psc"], ["tab"])
                        else:
                            self.T.op("dve", lambda e, g=g, gg=gg: e.tensor_copy(out=self.tabt[:, g:g + 8], in_=gg), ["modt"], ["tab"])

    def rstd(self, src2, n, dst, rkeys, wkeys, sq, sqkey, ps, pskey, tmp, tmpkey):
        self.actf(sq[:, 0:8 * n], src2, AF.Square, rkeys, [sqkey])
        pairs = [(self.ones[:], sq[:, c * n:(c + 1) * n]) for c in range(8)]
        self.mm_group(ps[:, 0:n], pairs, [sqkey, "ones"], [pskey])
        self.actf(tmp[:, 0:n], ps[:, 0:n], AF.Sqrt, [pskey, "epsc"], [tmpkey], bias=self.epsc[:, 0:1], scale=1.0 / 1024.0)
        self.T.op("dve", lambda e: e.reciprocal(out=dst, in_=tmp[:, 0:n]), [tmpkey], wkeys)

    def phase_pool(self, es):
        nc, T = self.nc, self.T
        if True:
            sb = lambda name, shape, dt=F32: es.enter_context(nc.sbuf_tensor(self.un(name), shape, dt))
            rs_l = sb("rs_l", [128, T_LAT]); rs_c = sb("rs_c", [128, T_CTX])
            Lg = [sb("Lg%d" % i, [128, 4096]) for i in range(2)]
            tmp = sb("tmpr", [128, 512])
            Hc = sb("Hc", [128, 4096])
            sq = Hc
            ztp = sb("ztp", [128, 4096], BF16)
            T.op("pool", lambda e: e.memset(ztp[:], 0.0), writes=["zt"])
            P = sb("Pp", [128, 6400]); Q = sb("Qp", [128, 6400])
            d16 = [sb("d16_%d" % i, [128, 4096], BF16) for i in range(2)]
            invc = sb("invc", [128, 4096])
            pw32 = sb("pw32", [128, 2048]); pw16 = sb("pw16", [128, 2048], BF16)
            ps = [es.enter_context(nc.psum_tensor(self.un("pps%d" % i), [128, 512], F32)) for i in range(2)]
            T.dma(pw32[:], self.poolw[:, :], writes=["pw32"])
            self.actf(pw16[:], pw32[:], AF.Copy, ["pw32"], ["pw16"])
            for g in range(8):
                L = Lg[g % 2]; key = "Lg%d" % (g % 2)
                T.dma(self.v3(L[:, :], 8), self.v3(self.xT, 8)[:, :, g * 512:(g + 1) * 512], writes=[key])
                self.rstd(L[:, :], 512, rs_l[:, g * 512:(g + 1) * 512], [key], ["rs_l"], sq, "Hc", ps[g % 2], "pps%d" % (g % 2), tmp, "tmpr")
                yield
            L = Lg[0]
            T.dma(self.v3(L[:, 0:2048], 8), self.v3(self.ctxT, 8), writes=["Lg0"])
            self.rstd(L[:, 0:2048], 256, rs_c[:, :], ["Lg0"], ["rs_c"], sq, "Hc", ps[0], "pps0", tmp, "tmpr")
            yield
            for who in range(2):
                ntok = T_LAT if who == 0 else T_CTX
                R, Cw = (64, 64) if who == 0 else (1, 256)
                Ra, N = (R + 16, Cw + 16) if who == 0 else (1, Cw + 16)
                r_lo = 8 if who == 0 else 0
                src_d = self.xT if who == 0 else self.ctxT
                dst_d = self.lat_d if who == 0 else self.ctx_d
                rs = rs_l if who == 0 else rs_c
                invd = self.invc2 if who == 0 else self.invc1
                a0 = self.tab(0, who, 0, 0); b0 = self.tab(0, who, 0, 1); g0 = self.tab(0, who, 0, 2)
                Pv = P[:, 0:Ra * N].rearrange("p (r c) -> p r c", c=N)
                Qv = Q[:, 0:Ra * N].rearrange("p (r c) -> p r c", c=N)
                Pint = Pv[:, r_lo:r_lo + R, 8:8 + Cw]
                Qint = Qv[:, r_lo:r_lo + R, 8:8 + Cw]
                for gi in range(4):
                    k = gi + 1
                    T.dma(invc[:, 0:ntok], invd[gi:gi + 1, :].partition_broadcast(128), writes=["invc"])
                    for ci in range(2):
                        c = 2 * gi + ci
                        L = Lg[ci]; key = "Lg%d" % ci
                        T.dma(L[:, 0:ntok], src_d[:, c * ntok:(c + 1) * ntok], writes=[key])
                        self.tt("dve", Hc[:, 0:ntok], L[:, 0:ntok], rs[:, 0:ntok], ALU.mult, [key, "rs_l" if who == 0 else "rs_c"], ["Hc"])
                        self.ts("dve", Hc[:, 0:ntok], Hc[:, 0:ntok], self.tabt[:, a0 + c:a0 + c + 1], self.tabt[:, b0 + c:b0 + c + 1], ALU.mult, ALU.add, ["Hc", "tab"], ["Hc"])
                        T.op("pool", lambda e, ap=P[:, 0:Ra * N]: e.memset(ap, 0.0), writes=["P"])
                        self.actf(Pint, Hc[:, 0:ntok].rearrange("p (r c) -> p r c", c=Cw), AF.Copy, ["Hc"], ["P"])
                        bufs = [(Pv, "P"), (Qv, "Q")]
                        step = 0
                        passes = [2] if who == 1 else [2, 1]
                        for axis in passes:
                            Nn = N if axis == 2 else Ra
                            for s in range(1, k + 1):
                                (sv_, sk), (dv_, dk) = bufs[step % 2], bufs[(step + 1) % 2]
                                lo = [1, 2, 4, 8][s - 1]; hi = Nn - [0, 1, 3, 7][s - 1]
                                if s == 1:
                                    a_lo, a_hi, b_lo, b_hi = lo - 1, hi - 1, lo, hi
                                else:
                                    sh = 2 ** (s - 2)
                                    a_lo, a_hi, b_lo, b_hi = lo - sh, hi - sh, lo + sh, hi + sh
                                if axis == 2:
                                    o_ = dv_[:, :, lo:hi]; i0 = sv_[:, :, a_lo:a_hi]; i1 = sv_[:, :, b_lo:b_hi]
                                else:
                                    o_ = dv_[:, lo:hi, 8:8 + Cw]; i0 = sv_[:, a_lo:a_hi, 8:8 + Cw]; i1 = sv_[:, b_lo:b_hi, 8:8 + Cw]
                                self.tt("dve", o_, i0, i1, ALU.add, [sk], [dk])
                                step += 1
                        res_int, res_key, oth_int, oth_key = (Pint, "P", Qint, "Q") if step % 2 == 0 else (Qint, "Q", Pint, "P")
                        self.tt("dve", oth_int, res_int, invc[:, 0:ntok].rearrange("p (r c) -> p r c", c=Cw), ALU.mult, [res_key, "invc"], [oth_key])
                        self.tt("dve", d16[ci][:, 0:ntok].rearrange("p (r c) -> p r c", c=Cw), oth_int, Hc[:, 0:ntok].rearrange("p (r c) -> p r c", c=Cw), ALU.subtract, [oth_key, "Hc"], ["d16_%d" % ci])
                    ngrp = 8 if who == 0 else 1
                    gsz = 512 if who == 0 else 256
                    for oc in range(2):
                        c = 2 * gi + oc
                        for g in range(ngrp):
                            u = oc * ngrp + g
                            pst = ps[u % 2]; pk = "pps%d" % (u % 2)
                            pairs = [(pw16[:, (gi * 2 + kc) * 256 + oc * 128:(gi * 2 + kc) * 256 + (oc + 1) * 128],
                                      d16[kc][:, g * gsz:(g + 1) * gsz]) for kc in range(2)]
                            self.mm_group(pst[:, 0:gsz], pairs, ["pw16", "d16_0", "d16_1"], [pk])
                            self.stt(Lg[oc][:, g * gsz:(g + 1) * gsz], pst[:, 0:gsz], self.tabt[:, g0 + c:g0 + c + 1],
                                     Lg[oc][:, g * gsz:(g + 1) * gsz], ALU.mult, ALU.add, [pk, "Lg%d" % oc, "tab"], ["Lg%d" % oc])
                        T.dma(dst_d[:, c * ntok:(c + 1) * ntok], Lg[oc][:, 0:ntok], reads=["Lg%d" % oc])
                        self.zero_step(ztp, 4)

    def groups(self, l):
        gs = [("lat", g * 512, 512, g * 512) for g in range(8)]
        if l == 0:
            gs.append(("ctx", 0, 256, T_LAT))
        return gs

    def phase_norm_router(self, l, pers=None):
        nc, T = self.nc, self.T
        with ExitStack() as es:
            sb = lambda name, shape, dt=F32: es.enter_context(nc.sbuf_tensor(self.un(name), shape, dt))
            Lg = [sb("nLg%d" % i, [128, 4096]) for i in range(2)]
            sq = sb("nsq", [128, 4096]); tmp = sb("ntmp", [128, 512]); rs = sb("nrs", [128, 512])
            h32 = [sb("h32_%d" % i, [128, 4096]) for i in range(2)]
            h16 = [sb("h16_%d" % i, [128, 4096], BF16) for i in range(2)]
            rw = sb("rw", [128, 256]); rb = sb("rb", [128, 32])
            lg = sb("lg", [128, 32]); top8 = sb("top8", [128, 8]); nmx = sb("nmx", [128, 1])
            msk = sb("msk", [128, 32]); ex = sb("ex", [128, 32]); ssum = sb("ssum", [128, 1]); rsum = sb("rsum", [128, 1])
            G = sb("G", [128, 32]); GT = [sb("GT%d" % i, [32, 512]) for i in range(2)]
            psr = es.enter_context(nc.psum_tensor(self.un("psr"), [128, 512], F32))
            psl = [es.enter_context(nc.psum_tensor(self.un("psl%d" % i), [128, 128], F32)) for i in range(2)]
            pst = [es.enter_context(nc.psum_tensor(self.un("pst%d" % i), [32, 512], F32)) for i in range(2)]
            rb4 = sb("rb4", [128, 128]); msk4 = sb("msk4", [128, 128]); ex4 = sb("ex4", [128, 128]); ssum4 = sb("ssum4", [128, 4]); rsum4 = sb("rsum4", [128, 4])
            if pers is None:
                NTT_ = sum(n_ for (_, _, n_, _) in self.groups(l)) // 128
                pers = {"lg": sb("q_lg", [128, NTT_ * 32]), "top8": sb("q_top8", [128, NTT_ * 8]),
                        "G": sb("q_G", [128, NTT_ * 32]), "M": sb("q_M", [128, NTT_ * 32], BF16)}
            zt = None
            zrow = [0]
            if l == 0 and self.sparse:
                zt = sb("zt", [128, 4096], BF16)
                T.op("pool", lambda e: e.memset(zt[:], 0.0), writes=["zt"])
            T.dma(rw[:], self.router_wp[:, l * 256:(l + 1) * 256], writes=["rw"])
            for t_ in range(4):
                T.dma(rb4[:, t_ * 32:(t_ + 1) * 32], self.router_bp[l:l + 1, :].partition_broadcast(128), writes=["rb4"])
            tcount = 0
            for gidx, (kind, off, n, hoff) in enumerate(self.groups(l)):
                who = 0 if kind == "lat" else 1
                src_d = self.lat_d if who == 0 else self.ctx_d
                ntok = T_LAT if who == 0 else T_CTX
                L = Lg[gidx % 2]; key = "nLg%d" % (gidx % 2)
                T.dma(self.v3(L[:, 0:8 * n], 8), self.v3(src_d, 8)[:, :, off:off + n], writes=[key])
                self.rstd(L[:, 0:8 * n], n, rs[:, 0:n], [key], ["nrs"], sq, "nsq", psr, "psr", tmp, "ntmp")
                a = self.tab(l, who, 1, 0); b = self.tab(l, who, 1, 1)
                H = h32[gidx % 2]; hk = "h32_%d" % (gidx % 2)
                H16 = h16[gidx % 2]; hk16 = "h16_%d" % (gidx % 2)
                for c in range(8):
                    self.tt("dve", H[:, c * n:(c + 1) * n], L[:, c * n:(c + 1) * n], rs[:, 0:n], ALU.mult, [key, "nrs"], [hk])
                    self.ts("dve", H[:, c * n:(c + 1) * n], H[:, c * n:(c + 1) * n], self.tabt[:, a + c:a + c + 1], self.tabt[:, b + c:b + c + 1], ALU.mult, ALU.add, [hk], [hk])
                self.actf(H16[:, 0:8 * n], H[:, 0:8 * n], AF.Copy, [hk], [hk16])
                T.dma(self.v3(self.h_d, 8)[:, :, hoff:hoff + n], self.v3(H16[:, 0:8 * n], 8), reads=[hk16])
                gt = GT[gidx % 2]; gk = "GT%d" % (gidx % 2)
                nt4 = n // 128
                tti0 = hoff // 128
                pl_ = psl[gidx % 2]; plk = "psl%d" % (gidx % 2)
                ptt = pst[gidx % 2]; ptk = "pst%d" % (gidx % 2)
                for t in range(nt4):
                    pairs = [(H[:, c * n + t * 128:c * n + (t + 1) * 128], rw[:, c * 32:(c + 1) * 32]) for c in range(8)]
                    self.mm_group(pl_[:, t * 32:(t + 1) * 32], pairs, [hk, "rw"], [plk])
                W4 = nt4 * 32
                lg4 = pers["lg"][:, tti0 * 32: tti0 * 32 + W4]
                G4 = pers["G"][:, tti0 * 32: tti0 * 32 + W4]
                M4 = pers["M"][:, tti0 * 32: tti0 * 32 + W4]
                t8 = pers["top8"]

                def v4(ap2):
                    return ap2.rearrange("p (t e) -> p t e", e=32)

                def bc(ap2, col0, tstride):
                    pstp = ap2.ap[0][0]
                    return bass.AP(ap2.tensor, ap2.offset + col0, [[pstp, 128], [tstride, nt4], [0, 32]])
                self.tt("dve", lg4, pl_[:, 0:W4], rb4[:, 0:W4], ALU.add, [plk, "rb4"], ["lg"])
                for t in range(nt4):
                    T.op("dve", lambda e, t=t, tti0=tti0: e.max(out=t8[:, (tti0 + t) * 8:(tti0 + t + 1) * 8], in_=pers["lg"][:, (tti0 + t) * 32:(tti0 + t + 1) * 32]), ["lg"], ["top8"])
                t8b = t8[:, :]
                self.tt("dve", v4(msk4[:, 0:W4]), v4(lg4), bc(t8b, tti0 * 8 + 3, 8), ALU.is_ge, ["lg", "top8"], ["msk"])
                T.op("dve", lambda e, M4=M4, W4=W4: e.tensor_copy(out=M4, in_=msk4[:, 0:W4]), ["msk"], ["Mall"])
                self.tt("dve", v4(ex4[:, 0:W4]), v4(lg4), bc(t8b, tti0 * 8 + 0, 8), ALU.subtract, ["lg", "top8"], ["ex"])
                self.actf(ex4[:, 0:W4], ex4[:, 0:W4], AF.Exp, ["ex"], ["ex"])
                self.tt("dve", ex4[:, 0:W4], ex4[:, 0:W4], msk4[:, 0:W4], ALU.mult, ["ex", "msk"], ["ex"])
                T.op("dve", lambda e, W4=W4, nt4=nt4: e.reduce_sum(out=ssum4[:, 0:nt4], in_=v4(ex4[:, 0:W4]), axis=mybir.AxisListType.X), ["ex"], ["ssum"])
                T.op("dve", lambda e, nt4=nt4: e.reciprocal(out=rsum4[:, 0:nt4], in_=ssum4[:, 0:nt4]), ["ssum"], ["rsum"])
                self.tt("dve", v4(G4), v4(ex4[:, 0:W4]), bc(rsum4[:, :], 0, 1), ALU.mult, ["ex", "rsum"], ["G"])
                for t in range(nt4):
                    sig = t in (0, nt4 - 1)
                    T.op("pe", lambda e, t=t, ptt=ptt, tti0=tti0: e.transpose(out=ptt[:, t * 128:(t + 1) * 128], in_=pers["G"][:, (tti0 + t) * 32:(tti0 + t + 1) * 32], identity=self.ident[:]),
                         ["G", "ident"] if sig else (), [ptk] if sig else (), signal=sig)
                self.actf(gt[:, 0:n], ptt[:, 0:n], AF.Copy, [ptk], [gk])
                T.dma(self.gt_d[:, hoff:hoff + n], gt[:, 0:n], reads=[gk])
                if zt is not None:
                    self.zero_step(zt, 4 if gidx < 8 else 66)
            T.flush()

    def phase_moe(self, l):
        nc, T = self.nc, self.T
        gs = self.groups(l)
        blocks = [gs[0:3], gs[3:6], gs[6:]]
        TB = 1536
        with ExitStack() as es:
            sb = lambda name, shape, dt=F32: es.enter_context(nc.sbuf_tensor(self.un(name), shape, dt))
            Lb = sb("Lb", [128, 8 * TB]); Hb = sb("Hb", [128, 8 * TB], BF16); Ab = sb("Ab", [128, 8 * TB], BF16)
            stg = [sb("stg%d" % i, [128, 4096]) for i in range(2)]
            NW = 4
            w16 = [sb("w16_%d" % i, [128, 4096], BF16) for i in range(NW)]
            tsg = [sb("tsg%d" % i, [128, 512]) for i in range(2)]
            tr1 = [sb("tr1%d" % i, [128, 512]) for i in range(2)]
            tr2 = [sb("tr2%d" % i, [128, 512]) for i in range(2)]
            tq = [sb("tq%d" % i, [128, 512]) for i in range(2)]
            g2n = sb("g2n", [128, 16])
            Gb = sb("Gb", [128, TB]); GTs = sb("GTs", [32, TB])
            bg = sb("bg", [128, 512]); bgs = sb("bgs", [128, 512]); bd = sb("bd", [32, 1024])
            psg = [es.enter_context(nc.psum_tensor(self.un("psg%d" % i), [128, 512], F32)) for i in range(2)]
            psu = [es.enter_context(nc.psum_tensor(self.un("psu%d" % i), [128, 512], F32)) for i in range(2)]
            psy = [es.enter_context(nc.psum_tensor(self.un("psy%d" % i), [128, 512], F32)) for i in range(2)]
            T.dma(bg[:], self.bgu[:, l * 512:(l + 1) * 512], writes=["bg"])
            T.dma(bd[:], self.bdn[l * 32:(l + 1) * 32, :], writes=["bd"])
            for who in range(2):
                g2c = self.tab(l, who, 1, 2)
                self.ts("dve", g2n[:, who * 8:(who + 1) * 8], self.tabt[:, g2c:g2c + 8], -1.0 / 1.702, None, ALU.mult, None, ["tab"], ["g2n"])
            bg3 = bg[:].rearrange("p (e j) -> p e j", j=16)
            bgs3 = bgs[:].rearrange("p (e j) -> p e j", j=16)
            self.ts("dve", bgs3[:, :, 0:8], bg3[:, :, 0:8], 1.702, None, ALU.mult, None, ["bg"], ["bgs"])
            self.ts("dve", bgs3[:, :, 8:16], bg3[:, :, 8:16], 7.0, None, ALU.add, None, ["bg"], ["bgs"])
            q_issue = [0]
            ucount = [0, 0]
            pend = [None]
            C1 = 11.914 / (1.0 + float(np.exp(-11.914)))
            c14 = sb("c14", [128, 1])
            T.op("pool", lambda e: e.memset(c14[:], 14.0), writes=["c14"])
            for bi, blk in enumerate(blocks):
                offs = []
                o = 0
                for (kind, off, n, hoff) in blk:
                    offs.append(o)
                    o += n
                nb = o
                jobs = []
                for e in range(self.nexp):
                    for p in range(6):
                        jobs.append((e, p))

                def issue(idx):
                    e, p = jobs[idx]
                    q = q_issue[0]
                    q_issue[0] += 1
                    s = stg[q % 2]; sk = "stg%d" % (q % 2)
                    w = w16[q % NW]; wk = "w16_%d" % (q % NW)
                    r0 = ((l * 32 + e) * 6 + p) * 128
                    T.dma(s[:], self.wexp[r0:r0 + 128, :], writes=[sk])
                    self.actf(w[:], s[:], AF.Copy, [sk], [wk])
                    return (w, wk)
                issued = {}
                nxt = 0
                for _ in range(min(4, len(jobs))):
                    issued[nxt] = issue(nxt)
                    nxt += 1
                for gi, (kind, off, n, hoff) in enumerate(blk):
                    src_d = self.lat_d if kind == "lat" else self.ctx_d
                    bo = offs[gi]
                    T.dma(self.v3(Lb[:, :], 8)[:, :, bo:bo + n], self.v3(src_d, 8)[:, :, off:off + n], writes=[("Lb", gi)])
                    T.dma(self.v3(Hb[:, :], 8)[:, :, bo:bo + n], self.v3(self.h_d, 8)[:, :, hoff:hoff + n], writes=["Hb"])
                    T.dma(GTs[:, bo:bo + n], self.gt_d[:, hoff:hoff + n], writes=["GTs"])
                for gi, (kind, off, n, hoff) in enumerate(blk):
                    who = 0 if kind == "lat" else 1
                    g2 = self.tab(l, who, 1, 2)
                    bo = offs[gi]
                    for dc in range(8):
                        par = ucount[1] % 2; ucount[1] += 1
                        self.mm(psy[par][:, 0:n], bd[:, dc * 128:(dc + 1) * 128], GTs[:, bo:bo + n], True, True, ["bd", "GTs"], ["psy%d" % par])
                        lsl = Lb[:, dc * TB + bo: dc * TB + bo + n]
                        self.stt(lsl, psy[par][:, 0:n], self.tabt[:, g2 + dc:g2 + dc + 1], lsl, ALU.mult, ALU.add, ["psy%d" % par, ("Lb", gi)], [("Lb", gi)])
                for e in range(self.nexp):
                    for gi, (kind, off, n, hoff) in enumerate(blk):
                        bo = offs[gi]
                        T.dma(Gb[:, bo:bo + n], self.gt_d[e:e + 1, hoff:hoff + n].partition_broadcast(128), writes=["Gb"])
                    base = e * 6
                    for half in range(2):
                        (wg, wgk) = issued.pop(base + 2 * half)
                        (wu, wuk) = issued.pop(base + 2 * half + 1)
                        for fl in range(4):
                            f = half * 4 + fl
                            for gi, (kind, off, n, hoff) in enumerate(blk):
                                bo = offs[gi]
                                par = ucount[0] % 2; ucount[0] += 1
                                pg, pu = psg[par], psu[par]
                                pgk, puk = "psg%d" % par, "psu%d" % par
                                pairs = [(wg[:, kc * 512 + fl * 128: kc * 512 + (fl + 1) * 128], Hb[:, kc * TB + bo: kc * TB + bo + n]) for kc in range(8)]
                                self.mm_group(pg[:, 0:n], pairs, [wgk, "Hb"], [pgk])
                                pairs = [(wu[:, kc * 512 + fl * 128: kc * 512 + (fl + 1) * 128], Hb[:, kc * TB + bo: kc * TB + bo + n]) for kc in range(8)]
                                self.mm_group(pu[:, 0:n], pairs, [wuk, "Hb"], [puk])
                                ks = "t%d" % par
                                self.actf(tr1[par][:, 0:n], pu[:, 0:n], AF.Relu, [puk, "bgs"], [ks + "r1"], bias=bgs[:, e * 16 + 8 + f:e * 16 + 8 + f + 1], scale=1.0)
                                self.actf(tsg[par][:, 0:n], pg[:, 0:n], AF.Silu, [pgk, "bgs"], [ks + "s"], bias=bgs[:, e * 16 + f:e * 16 + f + 1], scale=1.702)
                                self.actf(tr2[par][:, 0:n], tr1[par][:, 0:n], AF.Relu, [ks + "r1", "c14"], [ks + "r2"], bias=c14[:, 0:1], scale=-1.0)
                                self.stt(tq[par][:, 0:n], tsg[par][:, 0:n], C1, Gb[:, bo:bo + n], ALU.min, ALU.mult, [ks + "s", "Gb"], [ks + "q"])
                                if pend[0] is not None:
                                    pend[0]()
                                def d2(par=par, n=n, f=f, bo=bo, gi=gi, ks=ks):
                                    self.stt(Ab[:, f * TB + bo: f * TB + bo + n], tr2[par][:, 0:n], 8.0, tq[par][:, 0:n], ALU.subtract, ALU.mult, [ks + "r2", ks + "q"], [("Ab", gi)])
                                pend[0] = d2
                        if pend[0] is not None:
                            pend[0]()
                            pend[0] = None
                        for _ in range(2):
                            if nxt < len(jobs):
                                issued[nxt] = issue(nxt)
                                nxt += 1
                    for half in range(2):
                        (wd, wdk) = issued.pop(base + 4 + half)
                        for dl in range(4):
                            dc = half * 4 + dl
                            for gi, (kind, off, n, hoff) in enumerate(blk):
                                who = 0 if kind == "lat" else 1
                                g2 = self.tab(l, who, 1, 2)
                                bo = offs[gi]
                                par = ucount[1] % 2; ucount[1] += 1
                                py = psy[par]; pyk = "psy%d" % par
                                pairs = [(wd[:, fc * 512 + dl * 128: fc * 512 + (dl + 1) * 128], Ab[:, fc * TB + bo: fc * TB + bo + n]) for fc in range(8)]
                                self.mm_group(py[:, 0:n], pairs, [wdk, ("Ab", gi)], [pyk])
                                lsl = Lb[:, dc * TB + bo: dc * TB + bo + n]
                                self.stt(lsl, py[:, 0:n], g2n[:, who * 8 + dc:who * 8 + dc + 1], lsl, ALU.mult, ALU.add, [pyk, ("Lb", gi), "g2n"], [("Lb", gi)])
                        if nxt < len(jobs):
                            issued[nxt] = issue(nxt)
                            nxt += 1
                for gi, (kind, off, n, hoff) in enumerate(blk):
                    dst_d = self.lat_d if kind == "lat" else self.ctx_d
                    bo = offs[gi]
                    T.dma(self.v3(dst_d, 8)[:, :, off:off + n], self.v3(Lb[:, :], 8)[:, :, bo:bo + n], reads=[("Lb", gi)])
            T.flush()


    def phase_moe_sparse(self, l):
        nc, T = self.nc, self.T
        I32 = mybir.dt.int32
        X = mybir.AxisListType.X
        gs = self.groups(l)
        NTT = sum(n for (_, _, n, _) in gs) // 128
        NT = (4 * NTT * 128 + 32 * 511) // 512
        TS = 512
        with ExitStack() as pes:
            psb_ = lambda name, shape, dt=F32: pes.enter_context(nc.sbuf_tensor(self.un(name), shape, dt))
            pers = {"lg": psb_("p_lg", [128, NTT * 32]), "top8": psb_("p_top8", [128, NTT * 8]),
                    "G": psb_("p_G", [128, NTT * 32]), "M": psb_("p_M", [128, NTT * 32], BF16)}
            dest_i = psb_("dest_i", [128, NTT * 4], I32)
            gk = psb_("gk", [128, NTT * 4])
            widx_i = psb_("widx_i", [128, NT * 12], I32)
            bidx_i = psb_("bidx_i", [128, NT], I32)
            identb = psb_("identb", [128, 128], BF16)
            self.phase_norm_router(l, pers)
            T.flush()
            with ExitStack() as es:
                sb = lambda name, shape, dt=F32: es.enter_context(nc.sbuf_tensor(self.un(name), shape, dt))
                onesb = sb("onesb", [128, 128], BF16); utri = sb("utri", [128, 128], BF16)
                iotap = sb("iotap", [128, 1]); jpos = sb("jpos", [128, 66]); one32 = sb("one32", [128, 32])
                cnt = sb("cnt", [128, 32]); ntl = sb("ntl", [128, 32]); pc = sb("pc", [128, 32]); pend = sb("pend", [128, 32]); pstart = sb("pstart", [128, 32])
                cmp3 = sb("cmp3", [128, 66 * 32]); ej = sb("ej", [128, 66]); wix = sb("wix", [128, 66]); wix6 = sb("wix6", [128, 66 * 6]); bix = sb("bix", [128, 66])
                Dt = sb("Dt", [128, 32]); oh = sb("oh", [128, 32]); pr = sb("pr", [128, 32]); destf = sb("destf", [128, NTT * 4]); gkf = sb("gkf", [128, NTT * 4])
                ps_c = es.enter_context(nc.psum_tensor(self.un("ps_c"), [128, 32], F32))
                ps_r = [es.enter_context(nc.psum_tensor(self.un("ps_r%d" % i), [128, 32], F32)) for i in range(2)]
                T.dma(utri[:], self.utrid[:, :], writes=["utri"])
                T.dma(identb[:], self.identbd[:, :], writes=["identb"])
                T.dma(iotap[:], self.iotapd[:, :], writes=["iotap"])
                T.dma(jpos[:], self.jposd[0:1, :].partition_broadcast(128), writes=["jpos"])
                T.op("pool", lambda e: e.memset(onesb[:], 1.0), writes=["onesb"])
                T.op("pool", lambda e: e.memset(one32[:], 1.0), writes=["one32"])
                M = pers["M"]
                pairs = [(onesb[:], M[:, tt * 32:(tt + 1) * 32]) for tt in range(NTT)]
                self.mm_group(ps_c[:, :], pairs, ["onesb", "Mall"], ["ps_c"])
                T.op("dve", lambda e: e.tensor_copy(out=cnt[:], in_=ps_c[:, :]), ["ps_c"], ["cnt"])
                self.ts("dve", ntl[:], cnt[:], 0.0, None, ALU.is_gt, None, ["cnt"], ["ntl"])
                for m in range(1, 9):
                    self.stt(ntl[:], cnt[:], 512.0 * m, ntl[:], ALU.is_gt, ALU.add, ["cnt", "ntl"], ["ntl"])
                self.ts("dve", pc[:], ntl[:], 512.0, None, ALU.mult, None, ["ntl"], ["pc"])
                T.op("dve", lambda e: e.tensor_tensor_scan(out=pend[:], data0=one32[:], data1=pc[:], initial=0.0, op0=ALU.mult, op1=ALU.add), ["one32", "pc"], ["pend"])
                self.tt("dve", pstart[:], pend[:], pc[:], ALU.subtract, ["pend", "pc"], ["pstart"])
                a0 = pend[:, :]; pstp = a0.ap[0][0]
                in0 = bass.AP(a0.tensor, a0.offset, [[pstp, 128], [0, NT], [1, 32]])
                a1 = jpos[:, :]; pstp1 = a1.ap[0][0]
                in1 = bass.AP(a1.tensor, a1.offset, [[pstp1, 128], [1, NT], [0, 32]])
                c3 = cmp3[:, 0:NT * 32].rearrange("p (j e) -> p j e", e=32)
                T.op("dve", lambda e: e.tensor_tensor(out=c3, in0=in0, in1=in1, op=ALU.is_le), ["pend", "jpos"], ["cmp3"])
                T.op("dve", lambda e: e.reduce_sum(out=ej[:, 0:NT], in_=c3, axis=X), ["cmp3"], ["ej"])
                tailf = sb("tailf", [128, 66])
                self.ts("dve", tailf[:, 0:NT], ej[:, 0:NT], 32.0, float(2 ** 20), ALU.is_ge, ALU.mult, ["ej"], ["tailf"])
                self.ts("dve", ej[:, 0:NT], ej[:, 0:NT], 31.0, 32.0 * l, ALU.min, ALU.add, ["ej"], ["ej"])
                self.ts("dve", wix[:, 0:NT], ej[:, 0:NT], 768.0, None, ALU.mult, None, ["ej"], ["wix"])
                self.tt("dve", wix[:, 0:NT], wix[:, 0:NT], tailf[:, 0:NT], ALU.add, ["wix", "tailf"], ["wix"])
                samef = sb("samef", [128, 66])
                self.tt("dve", samef[:, 1:NT], ej[:, 1:NT], ej[:, 0:NT - 1], ALU.is_equal, ["ej"], ["samef"])
                self.ts("dve", samef[:, 1:NT], samef[:, 1:NT], float(2 ** 20), None, ALU.mult, None, ["samef"], ["samef"])
                self.tt("dve", wix[:, 1:NT], wix[:, 1:NT], samef[:, 1:NT], ALU.add, ["wix", "samef"], ["wix"])
                w6 = wix6[:, 0:NT * 6].rearrange("p (j s) -> p j s", s=6)
                for p_ in range(6):
                    self.ts("dve", w6[:, :, p_], wix[:, 0:NT], iotap[:, 0:1], 128.0 * p_, ALU.add, ALU.add, ["wix", "iotap"], ["wix6"])
                wix12 = sb("wix12", [128, 66 * 12])
                w12 = wix12[:, 0:NT * 12].rearrange("p (q h) -> p q h", h=2)
                for h_ in range(2):
                    self.ts("dve", w12[:, :, h_], wix6[:, 0:NT * 6], 2.0, float(h_), ALU.mult, ALU.add, ["wix6"], ["wix12"])
                T.op("dve", lambda e: e.tensor_copy(out=widx_i[:, :], in_=wix12[:, 0:NT * 12]), ["wix12"], ["widx_i"])
                self.ts("dve", bix[:, 0:NT], ej[:, 0:NT], 128.0, iotap[:, 0:1], ALU.mult, ALU.add, ["ej", "iotap"], ["bix"])
                T.op("dve", lambda e: e.tensor_copy(out=bidx_i[:, :], in_=bix[:, 0:NT]), ["bix"], ["bidx_i"])
                D4 = sb("D4", [128, 128]); oh4 = sb("oh4", [128, 128]); pr4 = sb("pr4", [128, 128])
                ps_r4 = [es.enter_context(nc.psum_tensor(self.un("ps_r4_%d" % i), [128, 128], F32)) for i in range(2)]
                pstart4 = sb("pstart4", [128, 128])
                for t_ in range(4):
                    T.op("dve", lambda e, t_=t_: e.tensor_copy(out=pstart4[:, t_ * 32:(t_ + 1) * 32], in_=pstart[:]), ["pstart"], ["pstart4"])

                def v4(ap2):
                    return ap2.rearrange("p (t e) -> p t e", e=32)
                for b0 in range(0, NTT, 4):
                    nb4 = min(4, NTT - b0)
                    W4 = nb4 * 32
                    pr_ = ps_r4[(b0 // 4) % 2]; prk = "ps_r4_%d" % ((b0 // 4) % 2)
                    for t_ in range(nb4):
                        tt = b0 + t_
                        pairs = [(utri[:], M[:, tt * 32:(tt + 1) * 32])] + [(onesb[:], M[:, t2 * 32:(t2 + 1) * 32]) for t2 in range(tt)]
                        o_ = pr_[:, t_ * 32:(t_ + 1) * 32]
                        if len(pairs) == 1:
                            self.mm(o_, pairs[0][0], pairs[0][1], True, True, ["utri", "Mall", "onesb"], [prk])
                        else:
                            self.mm_group(o_, pairs, ["utri", "Mall", "onesb"], [prk])
                    self.tt("dve", D4[:, 0:W4], pr_[:, 0:W4], pstart4[:, 0:W4], ALU.add, [prk, "pstart4"], ["D4"])
                    lg4 = pers["lg"][:, b0 * 32: b0 * 32 + W4]; G4 = pers["G"][:, b0 * 32: b0 * 32 + W4]
                    t8b = pers["top8"][:, :]
                    for k in range(4):
                        pstp = t8b.ap[0][0]
                        bck = bass.AP(t8b.tensor, t8b.offset + b0 * 8 + k, [[pstp, 128], [8, nb4], [0, 32]])
                        self.tt("dve", v4(oh4[:, 0:W4]), v4(lg4), bck, ALU.is_equal, [], ["oh4"])
                        self.tt("dve", pr4[:, 0:W4], oh4[:, 0:W4], D4[:, 0:W4], ALU.mult, ["oh4", "D4"], ["pr4"])
                        c0 = b0 * 4 + k * nb4
                        T.op("dve", lambda e, c0=c0, nb4=nb4, W4=W4: e.reduce_sum(out=destf[:, c0:c0 + nb4], in_=v4(pr4[:, 0:W4]), axis=X), ["pr4"], ["destf"])
                        self.tt("dve", pr4[:, 0:W4], oh4[:, 0:W4], G4, ALU.mult, ["oh4"], ["pr4"])
                        T.op("dve", lambda e, c0=c0, nb4=nb4, W4=W4: e.reduce_sum(out=gkf[:, c0:c0 + nb4], in_=v4(pr4[:, 0:W4]), axis=X), ["pr4"], ["gkf"])
                T.op("dve", lambda e: e.tensor_copy(out=dest_i[:, :], in_=destf[:, :]), ["destf"], ["dest_i"])
                self.ts("dve", gk[:, :], gkf[:, :], -1.0 / 1.702, None, ALU.mult, None, ["gkf"], ["gk"])
                T.flush()
            def dcol(tt, k):
                b0 = (tt // 4) * 4
                nb4 = min(4, NTT - b0)
                return b0 * 4 + k * nb4 + (tt - b0)
            XW = 1024
            with ExitStack() as es:
                sb = lambda name, shape, dt=F32: es.enter_context(nc.sbuf_tensor(self.un(name), shape, dt))
                hf = [sb("hf%d" % i, [128, 1024], BF16) for i in range(4)]
                XT = [sb("XT_%d" % i, [128, XW], BF16) for i in range(4)]
                psX = [es.enter_context(nc.psum_tensor(self.un("psX%d" % i), [128, 1024], BF16)) for i in range(2)]
                for tt in range(NTT):
                    par = tt % 4
                    pp = tt % 2
                    T.dma(self.v3(hf[par][:, :], 8), self.v3(self.h_d, 8)[:, :, tt * 128:(tt + 1) * 128], writes=["hf%d" % par])
                    for c in range(8):
                        sig = c in (0, 7)
                        T.op("pe", lambda e, c=c, par=par, pp=pp: e.transpose(out=psX[pp][:, c * 128:(c + 1) * 128], in_=hf[par][:, c * 128:(c + 1) * 128], identity=identb[:]),
                             ["hf%d" % par, "identb"] if sig else (), ["psX%d" % pp] if sig else (), signal=sig)
                    if tt % 2 == 0:
                        self.actf(XT[par][:, :], psX[pp][:, :], AF.Copy, ["psX%d" % pp], [("XT", par)])
                    else:
                        T.op("dve", lambda e, par=par, pp=pp: e.tensor_copy(out=XT[par][:, :], in_=psX[pp][:, :]), ["psX%d" % pp], [("XT", par)])
                    for k in range(4):
                        def scat(eng, par=par, dc_=dcol(tt, k)):
                            return eng.indirect_dma_start(out=self.xs_d, out_offset=bass.IndirectOffsetOnAxis(ap=dest_i[:, dc_:dc_ + 1], axis=0),
                                                          in_=XT[par][:, :], in_offset=None)
                        T.dma(None, None, reads=[("XT", par)], writes=["xs_d"], q="pool", fn=scat)
                T.flush()
            with ExitStack() as es:
                sb = lambda name, shape, dt=F32: es.enter_context(nc.sbuf_tensor(self.un(name), shape, dt))
                NW = 6
                w16 = [sb("s_w16_%d" % i, [128, 4096], BF16) for i in range(NW)]
                Xs = [sb("Xs%d" % i, [128, XW], BF16) for i in range(4)]
                Hb = [sb("sHb%d" % i, [128, 8 * TS], BF16) for i in range(2)]
                Ab = [sb("sAb%d" % i, [128, 8 * TS], BF16) for i in range(2)]
                gc = [sb("gc%d" % i, [128, 4]) for i in range(2)]
                bgt = [sb("bgt%d" % i, [128, 16]) for i in range(2)]
                bgst = [sb("bgst%d" % i, [128, 16]) for i in range(2)]
                tsg = [sb("s_tsg%d" % i, [128, 512]) for i in range(2)]
                tr1 = [sb("s_tr1%d" % i, [128, 512]) for i in range(2)]
                tr2 = [sb("s_tr2%d" % i, [128, 512]) for i in range(2)]
                tq = [sb("s_tq%d" % i, [128, 512]) for i in range(2)]
                Ys = [sb("Ys%d" % i, [128, 1024]) for i in range(2)]
                c14 = sb("s_c14", [128, 1])
                psT = [es.enter_context(nc.psum_tensor(self.un("psT%d" % i), [128, 512], BF16)) for i in range(2)]
                psg = [es.enter_context(nc.psum_tensor(self.un("spsg%d" % i), [128, 512], F32)) for i in range(2)]
                psu = [es.enter_context(nc.psum_tensor(self.un("spsu%d" % i), [128, 512], F32)) for i in range(2)]
                psy = [es.enter_context(nc.psum_tensor(self.un("spsy%d" % i), [128, 512], F32)) for i in range(2)]
                T.op("pool", lambda e: e.memset(c14[:], 14.0), writes=["c14"])
                C1 = 11.914 / (1.0 + float(np.exp(-11.914)))
                q_issue = [0]
                jobs = [(j, p) for j in range(NT) for p in range(6)]

                wexp2 = self.wexp.rearrange("r (h c) -> (r h) c", h=2)
                bc_reg = {}

                def issue(idx):
                    j, p = jobs[idx]
                    q = q_issue[0]; q_issue[0] += 1
                    w = w16[q % NW]; wk = "s_w16_%d" % (q % NW)
                    for h_ in range(2):
                        def gat(eng, w=w, j=j, p=p, h_=h_):
                            col = (j * 6 + p) * 2 + h_
                            if "r" not in bc_reg:
                                bc_reg["r"] = eng.to_reg(2 * 2 * 32 * 6 * 128 - 1)
                            return eng.indirect_dma_start(out=w[:, h_ * 2048:(h_ + 1) * 2048], out_offset=None, in_=wexp2,
                                                          in_offset=bass.IndirectOffsetOnAxis(ap=widx_i[:, col:col + 1], axis=0),
                                                          bounds_check=bc_reg["r"], oob_is_err=False)
                        T.dma(None, None, reads=[], writes=[wk], q="pool", fn=gat)
                    return (w, wk)
                issued = {}
                nxt = 0
                for _ in range(NW):
                    issued[nxt] = issue(nxt); nxt += 1
                uc = [0, 0, 0]
                pend = [None]
                def prep_load(j):
                    par = j % 2
                    def bgat(eng, par=par, j=j):
                        return eng.indirect_dma_start(out=bgt[par][:], out_offset=None, in_=self.bgu_e,
                                                      in_offset=bass.IndirectOffsetOnAxis(ap=bidx_i[:, j:j + 1], axis=0))
                    T.dma(None, None, reads=[], writes=["bgt%d" % par], q="pool", fn=bgat)
                    self.ts("dve", bgst[par][:, 0:8], bgt[par][:, 0:8], 1.702, None, ALU.mult, None, ["bgt%d" % par], ["bgst%d" % par])
                    self.ts("dve", bgst[par][:, 8:16], bgt[par][:, 8:16], 7.0, None, ALU.add, None, ["bgt%d" % par], ["bgst%d" % par])
                    for s4 in range(4):
                        xs = Xs[s4]; xk = "Xs%d" % s4
                        r0 = j * TS + s4 * 128
                        T.dma(xs[:], self.xs_d[r0:r0 + 128, :], writes=[xk])

                def prep_tr(j):
                    par = j % 2
                    hb = Hb[par]; hbk = "sHb%d" % par
                    for c in range(8):
                        tp = uc[2] % 2; uc[2] += 1
                        for s4 in range(4):
                            sig = s4 in (0, 3)
                            T.op("pe", lambda e, c=c, s4=s4, tp=tp: e.transpose(out=psT[tp][:, s4 * 128:(s4 + 1) * 128], in_=Xs[s4][:, c * 128:(c + 1) * 128], identity=identb[:]),
                                 ["Xs0", "Xs1", "Xs2", "Xs3", "identb"] if sig else (), ["psT%d" % tp] if sig else (), signal=sig)
                        if c % 2 == 0:
                            self.actf(hb[:, c * TS:(c + 1) * TS], psT[tp][:, :], AF.Copy, ["psT%d" % tp], [hbk])
                        else:
                            T.op("dve", lambda e, c=c, tp=tp, hb=hb: e.tensor_copy(out=hb[:, c * TS:(c + 1) * TS], in_=psT[tp][:, :]), ["psT%d" % tp], [hbk])
                prep_load(0)
                prep_tr(0)
                for j in range(NT):
                    par = j % 2
                    hb = Hb[par]; hbk = "sHb%d" % par
                    ab = Ab[par]; abk = "sAb%d" % par
                    if j + 1 < NT:
                        prep_load(j + 1)
                    base = j * 6
                    for half in range(2):
                        (wg, wgk) = issued.pop(base + 2 * half)
                        (wu, wuk) = issued.pop(base + 2 * half + 1)
                        for fl in range(4):
                            f = half * 4 + fl
                            up = uc[0] % 2; uc[0] += 1
                            pg, pu = psg[up], psu[up]
                            pgk, puk = "spsg%d" % up, "spsu%d" % up
                            pairs = [(wg[:, kc * 512 + fl * 128: kc * 512 + (fl + 1) * 128], hb[:, kc * TS:(kc + 1) * TS]) for kc in range(8)]
                            self.mm_group(pg[:, :], pairs, [wgk, hbk], [pgk])
                            pairs = [(wu[:, kc * 512 + fl * 128: kc * 512 + (fl + 1) * 128], hb[:, kc * TS:(kc + 1) * TS]) for kc in range(8)]
                            self.mm_group(pu[:, :], pairs, [wuk, hbk], [puk])
                            ks = "st%d" % up
                            bk = "bgst%d" % par
                            self.actf(tr1[up][:, :], pu[:, :], AF.Relu, [puk, bk], [ks + "r1"], bias=bgst[par][:, 8 + f:9 + f], scale=1.0)
                            self.actf(tsg[up][:, :], pg[:, :], AF.Silu, [pgk, bk], [ks + "s"], bias=bgst[par][:, f:f + 1], scale=1.702)
                            self.actf(tr2[up][:, :], tr1[up][:, :], AF.Relu, [ks + "r1", "c14"], [ks + "r2"], bias=c14[:, 0:1], scale=-1.0)
                            self.ts("dve", tq[up][:, :], tsg[up][:, :], C1, None, ALU.min, None, [ks + "s"], [ks + "q"])
                            if pend[0] is not None:
                                pend[0]()
                            def d2(up=up, f=f, ab=ab, abk=abk, ks=ks):
                                self.stt(ab[:, f * TS:(f + 1) * TS], tr2[up][:, :], 8.0, tq[up][:, :], ALU.subtract, ALU.mult, [ks + "r2", ks + "q"], [abk])
                            pend[0] = d2
                        if pend[0] is not None:
                            pend[0](); pend[0] = None
                        if half == 0:
                            for _ in range(2):
                                if nxt < len(jobs):
                                    issued[nxt] = issue(nxt); nxt += 1
                    if j + 1 < NT:
                        prep_tr(j + 1)
                    for _ in range(2):
                        if nxt < len(jobs):
                            issued[nxt] = issue(nxt); nxt += 1
                    wd = [issued.pop(base + 4), issued.pop(base + 5)]
                    for s4 in range(4):
                        ysb = Ys[s4 % 2]; yk = "Ys%d" % (s4 % 2)
                        for half in range(2):
                            (w_, wk_) = wd[half]
                            yp = uc[1] % 2; uc[1] += 1
                            pairs = [(ab[:, fc * TS + s4 * 128: fc * TS + (s4 + 1) * 128], w_[:, fc * 512:(fc + 1) * 512]) for fc in range(8)]
                            self.mm_group(psy[yp][:, :], pairs, [wk_, abk], ["spsy%d" % yp])
                            self.actf(ysb[:, half * 512:(half + 1) * 512], psy[yp][:, :], AF.Copy, ["spsy%d" % yp], [yk])
                        r0 = j * TS + s4 * 128
                        T.dma(self.ys_d[r0:r0 + 128, :], ysb[:, :], reads=[yk], writes=["ys_d"])
                    for _ in range(2):
                        if nxt < len(jobs):
                            issued[nxt] = issue(nxt); nxt += 1
                T.flush()
            with ExitStack() as es:
                sb = lambda name, shape, dt=F32: es.enter_context(nc.sbuf_tensor(self.un(name), shape, dt))
                Y4 = [sb("Y4_%d" % i, [128, 4 * 1024]) for i in range(4)]
                S4 = sb("S4", [128, 4 * 1024])
                GTg = [sb("GTg%d" % i, [32, 512]) for i in range(2)]
                bd = sb("e_bd", [32, 1024])
                Lg = [sb("eL%d" % i, [128, 4096]) for i in range(2)]
                psb2 = [es.enter_context(nc.psum_tensor(self.un("psb2_%d" % i), [128, 512], F32)) for i in range(2)]
                psl2 = [es.enter_context(nc.psum_tensor(self.un("psl2_%d" % i), [128, 512], F32)) for i in range(2)]
                fuse_final = (l == 1 and self.stop_after >= 5)
                if fuse_final:
                    fsq = sb("fsq", [128, 4096]); ftmp = sb("ftmp", [128, 512]); frs = sb("frs", [128, 512])
                    fpsr = es.enter_context(nc.psum_tensor(self.un("fpsr"), [128, 512], F32))
                T.dma(bd[:], self.bdn[l * 32:(l + 1) * 32, :], writes=["e_bd"])
                uc2 = 0; uc3 = 0
                for gidx, (kind, off, n, hoff) in enumerate(gs):
                    who = 0 if kind == "lat" else 1
                    src_d = self.lat_d if who == 0 else self.ctx_d
                    g2 = self.tab(l, who, 1, 2)
                    L = Lg[gidx % 2]; lk = "eL%d" % (gidx % 2)
                    gt = GTg[gidx % 2]; gtk = "GTg%d" % (gidx % 2)
                    T.dma(self.v3(L[:, 0:8 * n], 8), self.v3(src_d, 8)[:, :, off:off + n], writes=[lk])
                    T.dma(gt[:, 0:n], self.gt_d[:, hoff:hoff + n], writes=[gtk])
                    nt4 = n // 128
                    for t in range(nt4):
                        tt = hoff // 128 + t
                        y4 = Y4[tt % 4]; yk = ("Y4", tt % 4)
                        for k in range(4):
                            def gat(eng, y4=y4, k=k, dc_=dcol(tt, k)):
                                return eng.indirect_dma_start(out=y4[:, k * 1024:(k + 1) * 1024], out_offset=None, in_=self.ys_d,
                                                              in_offset=bass.IndirectOffsetOnAxis(ap=dest_i[:, dc_:dc_ + 1], axis=0))
                            T.dma(None, None, reads=["ys_d"], writes=[yk], q="pool", fn=gat)
                        st = S4[:, t * 1024:(t + 1) * 1024]
                        g_ = [gk[:, dcol(tt, k):dcol(tt, k) + 1] for k in range(4)]
                        self.ts("dve", y4[:, 0:1024], y4[:, 0:1024], g_[0], None, ALU.mult, None, [yk], [yk])
                        self.stt(y4[:, 0:1024], y4[:, 1024:2048], g_[1], y4[:, 0:1024], ALU.mult, ALU.add, [yk], [yk])
                        self.stt(y4[:, 0:1024], y4[:, 2048:3072], g_[2], y4[:, 0:1024], ALU.mult, ALU.add, [yk], [yk])
                        self.stt(st, y4[:, 3072:4096], g_[3], y4[:, 0:1024], ALU.mult, ALU.add, [yk], [("S4", t)])
                        for half in range(2):
                            bp = uc2 % 2; uc2 += 1
                            self.mm(psb2[bp][:, :], gt[:, t * 128:(t + 1) * 128], bd[:, half * 512:(half + 1) * 512], True, True, [gtk, "e_bd"], ["psb2_%d" % bp])
                            sl = S4[:, t * 1024 + half * 512: t * 1024 + (half + 1) * 512]
                            self.tt("dve", sl, sl, psb2[bp][:, :], ALU.add, [("S4", t), "psb2_%d" % bp], [("S4", t)])
                    for c in range(8):
                        lp = uc3 % 2; uc3 += 1
                        for t in range(nt4):
                            sig = t in (0, nt4 - 1)
                            T.op("pe", lambda e, c=c, t=t, lp=lp: e.transpose(out=psl2[lp][:, t * 128:(t + 1) * 128], in_=S4[:, t * 1024 + c * 128: t * 1024 + (c + 1) * 128], identity=self.ident[:]),
                                 [("S4", t_) for t_ in range(nt4)] + ["ident"] if sig else (), ["psl2_%d" % lp] if sig else (), signal=sig)
                        sl = L[:, c * n:(c + 1) * n]
                        self.stt(sl, psl2[lp][:, 0:n], self.tabt[:, g2 + c:g2 + c + 1], sl, ALU.mult, ALU.add, ["psl2_%d" % lp, lk], [lk])
                    if fuse_final:
                        self.rstd(L[:, 0:8 * n], n, frs[:, 0:n], [lk], ["frs"], fsq, "fsq", fpsr, "fpsr", ftmp, "ftmp")
                        for c in range(8):
                            sl = L[:, c * n:(c + 1) * n]
                            self.stt(sl, sl, self.gains_t[:, 32 + c:33 + c], frs[:, 0:n], ALU.mult, ALU.mult, [lk, "frs"], [lk])
                        T.dma(self.v3(self.outT, 8)[:, :, off:off + n], self.v3(L[:, 0:8 * n], 8), reads=[lk])
                    else:
                        T.dma(self.v3(src_d, 8)[:, :, off:off + n], self.v3(L[:, 0:8 * n], 8), reads=[lk])
                T.flush()

    def phase_lru(self):
        nc, T = self.nc, self.T
        l = 1
        with ExitStack() as es:
            sb = lambda name, shape, dt=F32: es.enter_context(nc.sbuf_tensor(self.un(name), shape, dt))
            Lg = [sb("lLg%d" % i, [128, 4096]) for i in range(2)]
            sq = sb("lsq", [128, 4096]); tmp = sb("ltmp", [128, 512]); rs = sb("lrs", [128, 512])
            Ht = [sb("lHt%d" % i, [128, 512]) for i in range(2)]
            H16 = [sb("lH16_%d" % i, [128, 4096], BF16) for i in range(2)]
            win = [sb("win%d" % i, [128, 4096], BF16) for i in range(4)]
            xs = [sb("bxs%d" % i, [128, 512]) for i in range(2)]
            x2 = [sb("bx2%d" % i, [128, 512]) for i in range(2)]
            sg = [sb("bsg%d" % i, [128, 512]) for i in range(2)]
            GG = [sb("bGG%d" % i, [128, 4096]) for i in range(2)]
            UU = [sb("bUU%d" % i, [128, 4096]) for i in range(2)]
            psr = es.enter_context(nc.psum_tensor(self.un("lpsr"), [128, 512], F32))
            ps = [es.enter_context(nc.psum_tensor(self.un("bps%d" % i), [128, 512], F32)) for i in range(2)]
            for j in range(4):
                T.dma(Lg[j % 2][:], self.win_p[j * 128:(j + 1) * 128, :], writes=["lLg%d" % (j % 2)])
                self.actf(win[j][:], Lg[j % 2][:], AF.Copy, ["lLg%d" % (j % 2)], ["win%d" % j])
            gs = [("lat", g * 512, 512, g * 512) for g in range(8)] + [("ctx", 0, 256, T_LAT)]
            ucb = [0]

            def norm(gidx):
                (kind, off, n, hoff) = gs[gidx]
                who = 0 if kind == "lat" else 1
                src_d = self.lat_d if who == 0 else self.ctx_d
                L = Lg[gidx % 2]; key = "lLg%d" % (gidx % 2)
                T.dma(self.v3(L[:, 0:8 * n], 8), self.v3(src_d, 8)[:, :, off:off + n], writes=[key])
                self.rstd(L[:, 0:8 * n], n, rs[:, 0:n], [key], ["lrs"], sq, "lsq", psr, "lpsr", tmp, "ltmp")
                a = self.tab(l, who, 0, 0); b = self.tab(l, who, 0, 1)
                hh16 = H16[gidx % 2]; hk16 = "lH16_%d" % (gidx % 2)
                for c in range(8):
                    ht = Ht[c % 2]; htk = "lHt%d" % (c % 2)
                    self.tt("dve", ht[:, 0:n], L[:, c * n:(c + 1) * n], rs[:, 0:n], ALU.mult, [key, "lrs"], [htk])
                    self.ts("dve", hh16[:, c * n:(c + 1) * n], ht[:, 0:n], self.tabt[:, a + c:a + c + 1], self.tabt[:, b + c:b + c + 1], ALU.mult, ALU.add, [htk], [hk16])

            def proj(gidx):
                (kind, off, n, hoff) = gs[gidx]
                hg = H16[gidx % 2]; hk = "lH16_%d" % (gidx % 2)
                gg = GG[gidx % 2]; ggk = "bGG%d" % (gidx % 2)
                uu = UU[gidx % 2]; uuk = "bUU%d" % (gidx % 2)
                for oc in range(16):
                    if kind == "ctx" and oc < 8:
                        continue
                    par = ucb[0] % 2; ucb[0] += 1
                    p_ = ps[par]; pk = "bps%d" % par
                    w = win[oc // 4]; wk = "win%d" % (oc // 4)
                    ol = oc % 4
                    pairs = [(w[:, kc * 512 + ol * 128: kc * 512 + (ol + 1) * 128], hg[:, kc * n:(kc + 1) * n]) for kc in range(8)]
                    self.mm_group(p_[:, 0:n], pairs, [wk, hk], [pk])
                    if oc < 8:
                        k_ = "bx%d" % par
                        self.actf(xs[par][:, 0:n], p_[:, 0:n], AF.Copy, [pk], [k_ + "s"])
                        self.tt("dve", x2[par][:, 0:n], xs[par][:, 0:n], xs[par][:, 0:n], ALU.mult, [k_ + "s"], [k_ + "2"])
                        self.ts("dve", x2[par][:, 0:n], x2[par][:, 0:n], 0.044715, 1.0, ALU.mult, ALU.add, [k_ + "2"], [k_ + "2"])
                        self.tt("dve", x2[par][:, 0:n], x2[par][:, 0:n], xs[par][:, 0:n], ALU.mult, [k_ + "2", k_ + "s"], [k_ + "2"])
                        self.actf(sg[par][:, 0:n], x2[par][:, 0:n], AF.Sigmoid, [k_ + "2"], [k_ + "g"], scale=1.5957691216057308)
                        self.tt("dve", gg[:, oc * n:(oc + 1) * n], xs[par][:, 0:n], sg[par][:, 0:n], ALU.mult, [k_ + "s", k_ + "g"], [ggk])
                    else:
                        self.actf(uu[:, (oc - 8) * n:(oc - 7) * n], p_[:, 0:n], AF.Copy, [pk], [uuk])
                if kind == "lat":
                    T.dma(self.v3(self.gg_d, 8)[:, :, off:off + n], self.v3(gg[:, 0:8 * n], 8), reads=[ggk])
                    T.dma(self.v3(self.u_d, 8)[:, :, T_CTX + off:T_CTX + off + n], self.v3(uu[:, 0:8 * n], 8), reads=[uuk])
                else:
                    T.dma(self.v3(self.u_d, 8)[:, :, 0:T_CTX], self.v3(uu[:, 0:8 * n], 8), reads=[uuk])
            norm(0)
            for gidx in range(len(gs)):
                if gidx + 1 < len(gs):
                    norm(gidx + 1)
                proj(gidx)
            T.flush()
        with ExitStack() as es:
            sb = lambda name, shape, dt=F32: es.enter_context(nc.sbuf_tensor(self.un(name), shape, dt))
            cw = sb("cw", [128, 32]); lv = sb("lv", [128, 56]); cA = sb("cA", [128, 16]); tmp16 = sb("tmp16", [128, 16])
            w32 = sb("cw32", [128, 4096]); wr16 = sb("wr16", [128, 4096], BF16); wi16 = sb("wi16", [128, 4096], BF16)
            U = sb("cU", [128, NTOK])
            UC32 = [sb("UC32_%d" % i, [128, NTOK]) for i in range(2)]
            UC16 = [sb("UC16_%d" % i, [128, NTOK], BF16) for i in range(2)]
            Aa = sb("Aa", [128, NTOK]); Bb = sb("Bb", [128, NTOK]); Hs = sb("Hs", [128, NTOK])
            REC = sb("REC", [128, T_LAT]); Z16 = sb("Z16", [128, T_LAT], BF16)
            tr = [sb("ctr%d" % i, [128, 512]) for i in range(2)]
            ti = [sb("cti%d" % i, [128, 512]) for i in range(2)]
            tm = [sb("ctm%d" % i, [128, 512]) for i in range(2)]
            psr = [es.enter_context(nc.psum_tensor(self.un("cpr%d" % i), [128, 512], F32)) for i in range(2)]
            psi = [es.enter_context(nc.psum_tensor(self.un("cpi%d" % i), [128, 512], F32)) for i in range(2)]
            T.dma(cw[:], self.convw[:, :], writes=["cw"])
            T.dma(lv[:], self.lruv[:, :], writes=["lv"])
            T.dma(w32[:], self.wr_p[:, :], writes=["cw32"])
            self.actf(wr16[:], w32[:], AF.Copy, ["cw32"], ["wr16"])
            T.dma(w32[:], self.wi_p[:, :], writes=["cw32"])
            self.actf(wi16[:], w32[:], AF.Copy, ["cw32"], ["wi16"])
            self.actf(tmp16[:], lv[:, 40:56], AF.Exp, ["lv"], ["tmp16"], scale=-1.0)
            self.actf(tmp16[:], tmp16[:], AF.Ln, ["tmp16"], ["tmp16"], bias=1.0, scale=1.0)
            self.ts("dve", cA[:], tmp16[:], -8.0, None, ALU.mult, None, ["tmp16"], ["cA"])
            segs = [(0, T_CTX), (T_CTX, NTOK)]
            gs = [(s, min(512, NTOK - s)) for s in range(0, NTOK, 512)]
            uc = 0
            g1 = self.tab(1, 0, 0, 2)
            for nb in range(4):
                for ci in range(2):
                    c = 2 * nb + ci
                    T.dma(U[:], self.u_d[:, c * NTOK:(c + 1) * NTOK], writes=["cU"])
                    uc32 = UC32[ci]; k32 = "UC32_%d" % ci
                    for (s0, s1) in segs:
                        self.actf(uc32[:, s0:s1], U[:, s0:s1], AF.Identity, ["cU", "cw", "lv"], [k32], bias=lv[:, c:c + 1], scale=cw[:, 2 * 8 + c:2 * 8 + c + 1])
                        self.stt(uc32[:, s0 + 2:s1], U[:, s0:s1 - 2], cw[:, 0 * 8 + c:0 * 8 + c + 1], uc32[:, s0 + 2:s1], ALU.mult, ALU.add, ["cU", k32], [k32])
                        self.stt(uc32[:, s0 + 1:s1], U[:, s0:s1 - 1], cw[:, 1 * 8 + c:1 * 8 + c + 1], uc32[:, s0 + 1:s1], ALU.mult, ALU.add, ["cU", k32], [k32])
                        self.stt(uc32[:, s0:s1 - 1], U[:, s0 + 1:s1], cw[:, 3 * 8 + c:3 * 8 + c + 1], uc32[:, s0:s1 - 1], ALU.mult, ALU.add, ["cU", k32], [k32])
                    self.actf(UC16[ci][:], uc32[:], AF.Copy, [k32], ["UC16_%d" % ci])
                for oc in range(2):
                    c = 2 * nb + oc
                    for d in range(2):
                        for (s, n) in gs:
                            par = uc % 2; uc += 1
                            wbase = ((d * 4 + nb) * 2) * 256
                            pairs = [(wr16[:, wbase + kc * 256 + oc * 128: wbase + kc * 256 + (oc + 1) * 128], UC16[kc][:, s:s + n]) for kc in range(2)]
                            self.mm_group(psr[par][:, 0:n], pairs, ["wr16", "UC16_0", "UC16_1"], ["cpr%d" % par])
                            pairs = [(wi16[:, wbase + kc * 256 + oc * 128: wbase + kc * 256 + (oc + 1) * 128], UC16[kc][:, s:s + n]) for kc in range(2)]
                            self.mm_group(psi[par][:, 0:n], pairs, ["wi16", "UC16_0", "UC16_1"], ["cpi%d" % par])
                            self.actf(Aa[:, s:s + n], psr[par][:, 0:n], AF.Sigmoid, ["cpr%d" % par, "lv"], ["Aa"], bias=lv[:, 8 + d * 8 + c:8 + d * 8 + c + 1], scale=1.0)
                            self.actf(Bb[:, s:s + n], psi[par][:, 0:n], AF.Sigmoid, ["cpi%d" % par, "lv"], ["Bb"], bias=lv[:, 24 + d * 8 + c:24 + d * 8 + c + 1], scale=1.0)
                        self.actf(Aa[:, :], Aa[:, :], AF.Exp, ["Aa", "cA"], ["Aa"], scale=cA[:, d * 8 + c:d * 8 + c + 1])
                        self.actf(Hs[:, :], Aa[:, :], AF.Square, ["Aa"], ["Hs"])
                        self.actf(Hs[:, :], Hs[:, :], AF.Sqrt, ["Hs"], ["Hs"], bias=1.0, scale=-1.0)
                        self.tt("dve", Bb[:, :], Bb[:, :], UC32[oc][:, :], ALU.mult, ["Bb", "UC32_%d" % oc], ["Bb"])
                        self.tt("dve", Bb[:, :], Bb[:, :], Hs[:, :], ALU.mult, ["Bb", "Hs"], ["Bb"])
                        if d == 0:
                            T.op("dve", lambda e: e.tensor_tensor_scan(out=Hs[:, 0:T_CTX], data0=Aa[:, 0:T_CTX], data1=Bb[:, 0:T_CTX], initial=0.0, op0=ALU.mult, op1=ALU.add), ["Aa", "Bb"], ["Hs"])
                            T.op("dve", lambda e: e.tensor_tensor_scan(out=REC[:, :], data0=Aa[:, T_CTX:NTOK], data1=Bb[:, T_CTX:NTOK], initial=Hs[:, T_CTX - 1:T_CTX], op0=ALU.mult, op1=ALU.add), ["Aa", "Bb", "Hs"], ["REC"])
                        else:
                            def rev(t, a, b):
                                apx = t[:, a:b]
                                pstep = apx.ap[0][0]
                                return bass.AP(apx.tensor, apx.offset + (b - a - 1), [[pstep, 128], [-1, b - a]])
                            T.op("dve", lambda e: e.tensor_tensor_scan(out=rev(Hs, 0, T_CTX), data0=rev(Aa, 0, T_CTX), data1=rev(Bb, 0, T_CTX), initial=0.0, op0=ALU.mult, op1=ALU.add), ["Aa", "Bb"], ["Hs"])
                            T.op("dve", lambda e: e.tensor_tensor_scan(out=rev(Hs, T_CTX, NTOK), data0=rev(Aa, T_CTX, NTOK), data1=rev(Bb, T_CTX, NTOK), initial=Hs[:, 0:1], op0=ALU.mult, op1=ALU.add), ["Aa", "Bb", "Hs"], ["Hs"])
                            self.tt("dve", REC[:, :], REC[:, :], Hs[:, T_CTX:NTOK], ALU.add, ["REC", "Hs"], ["REC"])
                    T.dma(U[:, 0:T_LAT], self.gg_d[:, c * T_LAT:(c + 1) * T_LAT], writes=["cU"])
                    self.tt("dve", Z16[:, :], REC[:, :], U[:, 0:T_LAT], ALU.mult, ["REC", "cU"], ["Z16"])
                    T.dma(self.z_d[:, c * T_LAT:(c + 1) * T_LAT], Z16[:, :], reads=["Z16"])
            T.flush()
        with ExitStack() as es:
            sb = lambda name, shape, dt=F32: es.enter_context(nc.sbuf_tensor(self.un(name), shape, dt))
            stg = [sb("dstg%d" % i, [128, 4096]) for i in range(2)]
            wo = [sb("wo%d" % i, [128, 4096], BF16) for i in range(2)]
            Zg = [sb("dZ%d" % i, [128, 4096], BF16) for i in range(2)]
            Lg = [sb("dL%d" % i, [128, 4096]) for i in range(2)]
            ps = [es.enter_context(nc.psum_tensor(self.un("dps%d" % i), [128, 512], F32)) for i in range(2)]
            for j in range(2):
                T.dma(stg[j][:], self.wout_p[j * 128:(j + 1) * 128, :], writes=["dstg%d" % j])
                self.actf(wo[j][:], stg[j][:], AF.Copy, ["dstg%d" % j], ["wo%d" % j])
            g1 = self.tab(1, 0, 0, 2)
            uc = 0
            for g in range(8):
                z = Zg[g % 2]; zk = "dZ%d" % (g % 2)
                L = Lg[g % 2]; lk = "dL%d" % (g % 2)
                T.dma(self.v3(z[:, :], 8), self.v3(self.z_d, 8)[:, :, g * 512:(g + 1) * 512], writes=[zk])
                T.dma(self.v3(L[:, :], 8), self.v3(self.lat_d, 8)[:, :, g * 512:(g + 1) * 512], writes=[lk])
                for dc in range(8):
                    par = uc % 2; uc += 1
                    w = wo[dc // 4]; dl = dc % 4
                    pairs = [(w[:, kc * 512 + dl * 128: kc * 512 + (dl + 1) * 128], z[:, kc * 512:(kc + 1) * 512]) for kc in range(8)]
                    self.mm_group(ps[par][:, :], pairs, ["wo%d" % (dc // 4), zk], ["dps%d" % par])
                    sl = L[:, dc * 512:(dc + 1) * 512]
                    self.stt(sl, ps[par][:, :], self.tabt[:, g1 + dc:g1 + dc + 1], sl, ALU.mult, ALU.add, ["dps%d" % par, lk], [lk])
                T.dma(self.v3(self.lat_d, 8)[:, :, g * 512:(g + 1) * 512], self.v3(L[:, :], 8), reads=[lk])
            T.flush()

    def phase_final(self, do_norm):
        nc, T = self.nc, self.T
        with ExitStack() as es:
            sb = lambda name, shape, dt=F32: es.enter_context(nc.sbuf_tensor(self.un(name), shape, dt))
            Lg = [sb("fL%d" % i, [128, 4096]) for i in range(2)]
            sq = sb("fsq", [128, 4096]); tmp = sb("ftmp", [128, 512]); rs = sb("frs", [128, 512])
            psr = es.enter_context(nc.psum_tensor(self.un("fpsr"), [128, 512], F32))
            for g in range(8):
                L = Lg[g % 2]; key = "fL%d" % (g % 2)
                T.dma(self.v3(L[:, :], 8), self.v3(self.lat_d, 8)[:, :, g * 512:(g + 1) * 512], writes=[key])
                if do_norm:
                    self.rstd(L[:, :], 512, rs[:, :], [key], ["frs"], sq, "fsq", psr, "fpsr", tmp, "ftmp")
                    for c in range(8):
                        sl = L[:, c * 512:(c + 1) * 512]
                        self.stt(sl, sl, self.gains_t[:, 32 + c:33 + c], rs[:, :], ALU.mult, ALU.mult, [key, "frs"], [key])
                T.dma(self.v3(self.outT, 8)[:, :, g * 512:(g + 1) * 512], self.v3(L[:, :], 8), reads=[key])
            T.flush()


def _col(v, n):
    return np.ascontiguousarray(np.asarray(v, np.float32).reshape(n, 128).T)


def _pieces(w, ncol_pieces):
    w = np.asarray(w, np.float32)
    return np.ascontiguousarray(w.reshape(8, 128, ncol_pieces, 512).transpose(2, 1, 0, 3)).reshape(ncol_pieces, 128, 4096)


def _inv_counts():
    def bounds(n, w):
        idx = np.arange(n)
        return np.clip(idx - w // 2, 0, n), np.clip(idx + w // 2, 0, n)
    i2 = np.zeros((4, 4096), np.float32)
    i1 = np.zeros((4, 256), np.float32)
    for gi, w in enumerate((2, 4, 8, 16)):
        r0, r1 = bounds(64, w)
        cnt = ((r1 - r0)[:, None] * (r1 - r0)[None, :]).astype(np.float32)
        i2[gi] = (np.float32(1.0) / cnt).reshape(-1)
        l0, l1 = bounds(256, w)
        i1[gi] = np.float32(1.0) / (l1 - l0).astype(np.float32)
    return i2, i1


def prep_shared(inp, nexp=32):
    f = lambda k: np.asarray(inp[k], np.float32)
    sh = {}
    ada_w = f("ada_w")
    sh["ada_wp"] = np.concatenate([_pieces(ada_w[l], 12) for l in range(2)], 0).reshape(2 * 12 * 128, 4096)
    sh["ada_bp"] = np.concatenate([_col(f("ada_b")[l], 48) for l in range(2)], 1)
    sh["gains"] = np.concatenate([_col(f("norm_mix")[0], 8), _col(f("norm_ffn")[0], 8), _col(f("norm_mix")[1], 8),
                                  _col(f("norm_ffn")[1], 8), _col(f("final_norm"), 8)], 1)
    sh["pscale"] = _col(f("pool_scale")[0], 8)
    sh["poolw"] = np.ascontiguousarray(f("pool_w")[0].reshape(4, 2, 128, 256).transpose(2, 0, 1, 3)).reshape(128, 2048)
    sh["invc2"], sh["invc1"] = _inv_counts()
    sh["win_p"] = _pieces(f("lru_w_in")[0], 4).reshape(4 * 128, 4096)
    sh["wout_p"] = _pieces(f("lru_w_out")[0], 2).reshape(2 * 128, 4096)
    sh["convw"] = np.concatenate([_col(f("lru_conv_w")[0][k], 8) for k in range(4)], 1)
    sh["lruv"] = np.concatenate([_col(f("lru_conv_b")[0], 8)] + [_col(f("lru_b_r")[0][d], 8) for d in range(2)] +
                                [_col(f("lru_b_i")[0][d], 8) for d in range(2)] + [_col(f("lru_lam")[0][d], 8) for d in range(2)], 1)
    for nm, key in (("wr_p", "lru_w_r"), ("wi_p", "lru_w_i")):
        w = f(key)[0]
        sh[nm] = np.ascontiguousarray(w.reshape(2, 4, 2, 128, 256).transpose(3, 0, 1, 2, 4)).reshape(128, 4096)
    rw = f("router_w")
    sh["router_wp"] = np.ascontiguousarray(rw.reshape(2, 8, 128, 32).transpose(2, 0, 1, 3)).reshape(128, 512)
    sh["router_bp"] = np.ascontiguousarray(f("router_b"))
    wgu = f("exp_w_gu"); wdn = f("exp_w_down")
    wexp = np.empty((2, 32, 6, 128, 4096), np.float32)
    for l in range(2):
        for e in range(nexp):
            pg = _pieces(wgu[l, e], 4)
            pd = _pieces(wdn[l, e], 2)
            wexp[l, e, 0] = pg[0]; wexp[l, e, 1] = pg[2]; wexp[l, e, 2] = pg[1]; wexp[l, e, 3] = pg[3]
            wexp[l, e, 4] = pd[0]; wexp[l, e, 5] = pd[1]
    sh["wexp"] = wexp.reshape(2 * 32 * 6 * 128, 4096)
    bgu = f("exp_b_gu")
    sh["bgu"] = np.ascontiguousarray(bgu.reshape(2, 32, 16, 128).transpose(3, 0, 1, 2)).reshape(128, 1024)
    sh["bdn"] = np.ascontiguousarray(f("exp_b_down").reshape(64, 1024))
    sh["identd"] = np.eye(128, dtype=np.float32)
    import ml_dtypes
    sh["identbd"] = np.eye(128).astype(ml_dtypes.bfloat16)
    sh["utrid"] = np.triu(np.ones((128, 128), np.float32), 1).astype(ml_dtypes.bfloat16)
    sh["iotapd"] = np.arange(128, dtype=np.float32).reshape(128, 1)
    sh["jposd"] = (np.arange(66, dtype=np.float32) * 512.0).reshape(1, 66)
    sh["bgu_e"] = np.ascontiguousarray(bgu.reshape(2, 32, 16, 128).transpose(0, 1, 3, 2)).reshape(64 * 128, 16)
    return sh


def prep_core(inp, b):
    x = np.asarray(inp["x"][b], np.float32)
    ctx = np.asarray(inp["ctx"][b], np.float32)
    d = {}
    d["xT"] = np.ascontiguousarray(x.T.reshape(8, 128, T_LAT).transpose(1, 0, 2)).reshape(128, 8 * T_LAT)
    d["ctxT"] = np.ascontiguousarray(ctx.T.reshape(8, 128, T_CTX).transpose(1, 0, 2)).reshape(128, 8 * T_CTX)
    d["cs"] = np.concatenate([_col(inp["c"][b], 8), _col(inp["c_ctx"], 8)], 1)
    return d


def unpack_out(o):
    return np.ascontiguousarray(o.reshape(128, 8, T_LAT).transpose(1, 0, 2).reshape(1024, T_LAT).T)


_CACHE = {}


def kernel(**inputs):
    if "nc" not in _CACHE:
        _CACHE["nc"] = K().build()
    nc = _CACHE["nc"]
    sh = prep_shared(inputs)
    in_maps = []
    for b in range(8):
        m = dict(sh)
        m.update(prep_core(inputs, b))
        in_maps.append(m)
    res = run_bass_kernel_spmd(nc, in_maps, core_ids=list(range(8)))
    out = np.stack([unpack_out(res.results[b]["outT"]) for b in range(8)], 0)
    return out.astype(np.float32)
```
